# Optimizing a Trainium2 kernel written in Bass

```python
import math
import jax
import jax.numpy as jnp
from jax import lax
import numpy as np

D_MODEL = 1024
BATCH = 4
SEQ = 4096
DEPTH = 2

GRID_W = 64
CTX_LEN = 256
EPS = 1e-6
NEG_INF = -1e30
CONV_W = 4
F32 = jnp.float32

LRU_WIDTH = D_MODEL // 2
LRU_BLOCKS = 8
LRU_BLOCK = LRU_WIDTH // LRU_BLOCKS
LRU_C = 8.0
NA_HEADS = 8
NA_HEAD_DIM = (D_MODEL // 2) // NA_HEADS
NA_WIDTH = NA_HEADS * NA_HEAD_DIM
NA_WIN_R = 8
NA_WIN_C = 16
EVEN_SPLITS = (LRU_WIDTH, LRU_WIDTH, NA_WIDTH, NA_WIDTH, NA_WIDTH)
EVEN_IN = sum(EVEN_SPLITS)
EVEN_MIX = LRU_WIDTH + NA_WIDTH

SSD_HEADS = 8
SSD_HEAD_DIM = 64
SSD_WIDTH = SSD_HEADS * SSD_HEAD_DIM
SSD_GROUPS = 2
SSD_STATE = 128
SSD_CHUNK = 128
SSD_XBC = SSD_WIDTH + 2 * SSD_GROUPS * SSD_STATE
ML_HEADS = 4
ML_HEAD_DIM = 128
ML_WIDTH = ML_HEADS * ML_HEAD_DIM
ML_CHUNK = 128
ODD_SPLITS = (SSD_WIDTH, SSD_XBC, 2 * SSD_HEADS, ML_WIDTH, ML_WIDTH, ML_WIDTH, ML_WIDTH, 4 * ML_HEADS)
ODD_IN = sum(ODD_SPLITS)
ODD_MIX = SSD_WIDTH + ML_WIDTH

MOE_GROUPS = 4
MOE_EXPERTS_PER_GROUP = 4
MOE_EXPERTS = MOE_GROUPS * MOE_EXPERTS_PER_GROUP
MOE_TOP_K = 2
MOE_FF = 512

N_EVEN = (DEPTH + 1) // 2
N_ODD = DEPTH // 2

kernel_name = 'hybrid_prefix_diffusion_trunk'


def rms_norm(x, g):
    xf = x.astype(F32)
    y = xf * lax.rsqrt(jnp.mean(xf * xf, axis=-1, keepdims=True) + EPS)
    return (y * g.astype(F32)).astype(x.dtype)


def _split(t, sizes):
    return jnp.split(t, np.cumsum(sizes)[:-1].tolist(), axis=-1)


def _flip(t):
    return jnp.flip(t, axis=1)


def dw_conv(x, w, b):
    K = w.shape[0]
    y = lax.conv_general_dilated(x, w[:, None, :], window_strides=(1,), padding=[(K // 2, K - 1 - K // 2)],
                                 dimension_numbers=('NWC', 'WIO', 'NWC'), feature_group_count=x.shape[-1])
    return y + b


def block_diag_linear(x, w, b):
    bsz, L, _ = x.shape
    nb, bs, _ = w.shape
    y = jnp.einsum('blnd,nde->blne', x.reshape(bsz, L, nb, bs), w)
    return y.reshape(bsz, L, nb * bs) + b


def linear_scan(a, u, h0):
    def combine(left, right):
        al, ul = left
        ar, ur = right
        return al * ar, ar * ul + ur
    a_cum, h = lax.associative_scan(combine, (a, u), axis=1)
    return a_cum * h0[:, None] + h


def rglru_scan(xs, wa, ba, wx, bx, lam, h0):
    r = jax.nn.sigmoid(block_diag_linear(xs, wa, ba).astype(F32))
    i = jax.nn.sigmoid(block_diag_linear(xs, wx, bx).astype(F32))
    log_a = -LRU_C * r * jax.nn.softplus(-lam.astype(F32))
    u = jnp.sqrt(-jnp.expm1(2.0 * log_a)) * (i * xs.astype(F32))
    return linear_scan(jnp.exp(log_a), u, h0)


def prefix_bidirectional(run, ctx_in, lat_in, init, need_ctx):
    yc_f, sc_f = run(ctx_in, 0, init)
    yl_f, _ = run(lat_in, 0, sc_f)
    yc_b, sc_b = run(tuple(_flip(t) for t in ctx_in), 1, init)
    yl_b, _ = run(tuple(_flip(t) for t in lat_in), 1, sc_b)
    y_ctx = yc_f + _flip(yc_b) if need_ctx else None
    return yl_f + _flip(yl_b), y_ctx


def neighbourhood_attention(q, k, v, qc, kc, vc, q_g, k_g, rpb, need_ctx):
    bsz, S, H, hd = q.shape
    rows = S // GRID_W
    kr = min(NA_WIN_R, rows)
    scale = hd ** -0.5
    q, k, kc = rms_norm(q, q_g), rms_norm(k, k_g), rms_norm(kc, k_g)
    r = jnp.arange(rows)
    row_idx = jnp.clip(r - kr // 2, 0, rows - kr)[:, None] + jnp.arange(kr)[None, :]
    col = jnp.arange(GRID_W)
    col_start = jnp.clip(col - NA_WIN_C // 2, 0, GRID_W - NA_WIN_C)
    col_in = (col[None, :] >= col_start[:, None]) & (col[None, :] < col_start[:, None] + NA_WIN_C)
    d_row = row_idx - r[:, None] + (NA_WIN_R - 1)
    d_col = jnp.clip(col[None, :] - col[:, None] + (NA_WIN_C - 1), 0, 2 * NA_WIN_C - 2)
    bias = rpb.astype(F32)[:, d_row[:, None, :, None], d_col[None, :, None, :]]
    bias = jnp.where(col_in[None, None, :, None, :], bias, NEG_INF)

    def grid(t):
        return t.reshape(bsz, rows, GRID_W, H, hd).transpose(0, 3, 1, 2, 4)
    qg = grid(q)
    kg = grid(k)[:, :, row_idx]
    vg = grid(v)[:, :, row_idx]
    kch = kc.transpose(0, 2, 1, 3)
    vch = vc.transpose(0, 2, 1, 3)
    s_win = jnp.einsum('bhrqd,bhrjkd->bhrqjk', qg, kg, preferred_element_type=F32) * scale + bias[None]
    s_ctx = jnp.einsum('bhrqd,bhcd->bhrqc', qg, kch, preferred_element_type=F32) * scale
    n_win = kr * GRID_W
    p = jax.nn.softmax(jnp.concatenate([s_win.reshape(bsz, H, rows, GRID_W, n_win), s_ctx], axis=-1), axis=-1)
    p = p.astype(v.dtype)
    o = (jnp.einsum('bhrqjk,bhrjkd->bhrqd', p[..., :n_win].reshape(s_win.shape), vg)
         + jnp.einsum('bhrqc,bhcd->bhrqd', p[..., n_win:], vch))
    y = o.transpose(0, 2, 3, 1, 4).reshape(bsz, S, H * hd)
    if not need_ctx:
        return y, None
    qch = rms_norm(qc, q_g).transpose(0, 2, 1, 3)
    pc = jax.nn.softmax(jnp.einsum('bhqd,bhkd->bhqk', qch, kch, preferred_element_type=F32) * scale, axis=-1)
    yc = jnp.einsum('bhqk,bhkd->bhqd', pc.astype(v.dtype), vch).transpose(0, 2, 1, 3)
    return y, yc.reshape(bsz, -1, H * hd)


def ssd_chunked(xh, dt, A, Bm, Cm, h0):
    bsz, L, H, P = xh.shape
    G, N = Bm.shape[2], Bm.shape[3]
    R = H // G
    Q = SSD_CHUNK
    nc = L // Q
    x = xh.astype(F32).reshape(bsz, nc, Q, G, R, P)
    dtc = dt.reshape(bsz, nc, Q, G, R)
    Bc = Bm.astype(F32).reshape(bsz, nc, Q, G, N)
    Cc = Cm.astype(F32).reshape(bsz, nc, Q, G, N)
    dtx = x * dtc[..., None]
    cum = jnp.cumsum(dtc * A.reshape(G, R), axis=2)
    causal = jnp.tril(jnp.ones((Q, Q), bool))
    seg = cum[:, :, :, None] - cum[:, :, None, :]
    decay = jnp.exp(jnp.where(causal[None, None, :, :, None, None], seg, NEG_INF))
    cb = jnp.einsum('bcign,bcjgn->bcijg', Cc, Bc)
    y_diag = jnp.einsum('bcijg,bcijgr,bcjgrp->bcigrp', cb, decay, dtx)
    to_end = jnp.exp(cum[:, :, -1:] - cum)
    states = jnp.einsum('bcjgn,bcjgr,bcjgrp->bcgrpn', Bc, to_end, dtx)
    chunk_decay = jnp.exp(cum[:, :, -1])

    def step(h, inp):
        s_c, a_c = inp
        return a_c[..., None, None] * h + s_c, h
    h_last, h_start = lax.scan(step, h0, (states.swapaxes(0, 1), chunk_decay.swapaxes(0, 1)))
    h_start = h_start.swapaxes(0, 1)
    y_off = jnp.einsum('bcign,bcigr,bcgrpn->bcigrp', Cc, jnp.exp(cum), h_start)
    return (y_diag + y_off).reshape(bsz, L, H, P), h_last


def mlstm_chunked(q, k, v, log_i, log_f, state):
    bsz, L, H, dh = q.shape
    Q = ML_CHUNK
    nc = L // Q
    causal = jnp.tril(jnp.ones((Q, Q), bool))

    def chunks(t):
        return t.astype(F32).reshape(bsz, nc, Q, *t.shape[2:]).swapaxes(0, 1)

    def step(carry, inp):
        C, n, m = carry
        qc, kc, vc, ic, fc = inp
        b = jnp.cumsum(fc, axis=1)
        dlog = jnp.where(causal[None, :, :, None], b[:, :, None] - b[:, None] + ic[:, None], NEG_INF)
        inter = b + m[:, None]
        m_t = jnp.maximum(jnp.max(dlog, axis=2), inter)
        w = jnp.exp(dlog - m_t[:, :, None]) * jnp.einsum('bthd,bshd->btsh', qc, kc)
        g_in = jnp.exp(inter - m_t)
        num = jnp.einsum('btsh,bshd->bthd', w, vc) + g_in[..., None] * jnp.einsum('bhed,bthd->bthe', C, qc)
        den = jnp.sum(w, axis=2) + g_in * jnp.einsum('bhd,bthd->bth', n, qc)
        h = num / jnp.maximum(jnp.abs(den), jnp.exp(-m_t))[..., None]
        b_end = b[:, -1]
        g_s = b_end[:, None] - b + ic
        m_new = jnp.maximum(jnp.max(g_s, axis=1), b_end + m)
        w_s = jnp.exp(g_s - m_new[:, None])
        keep = jnp.exp(b_end + m - m_new)
        C_new = keep[..., None, None] * C + jnp.einsum('bsh,bshe,bshd->bhed', w_s, vc, kc)
        n_new = keep[..., None] * n + jnp.einsum('bsh,bshd->bhd', w_s, kc)
        return (C_new, n_new, m_new), h
    final, hs = lax.scan(step, state, tuple(chunks(t) for t in (q, k, v, log_i, log_f)))
    return hs.swapaxes(0, 1).reshape(bsz, L, H, dh), final


def even_mixer(h, hc, w_in, w_out, conv_w, conv_b, wa, ba, wx, bx, lam, q_g, k_g, rpb, need_ctx):
    bsz = h.shape[0]
    ux, ug, q, k, v = _split(h @ w_in, EVEN_SPLITS)
    uxc, ugc, qc, kc, vc = _split(hc @ w_in, EVEN_SPLITS)
    xl = dw_conv(ux, conv_w, conv_b)
    xcv = dw_conv(uxc, conv_w, conv_b)

    def run(inp, d, state):
        hs = rglru_scan(inp[0], wa[d], ba[d], wx[d], bx[d], lam[d], state)
        return hs, hs[:, -1]
    h0 = jnp.zeros((bsz, LRU_WIDTH), F32)
    r_lat, r_ctx = prefix_bidirectional(run, (xcv,), (xl,), h0, need_ctx)

    def heads(t):
        return t.reshape(t.shape[0], t.shape[1], NA_HEADS, NA_HEAD_DIM)
    a_lat, a_ctx = neighbourhood_attention(heads(q), heads(k), heads(v), heads(qc), heads(kc), heads(vc),
                                           q_g, k_g, rpb, need_ctx)
    y = jnp.concatenate([(r_lat * jax.nn.gelu(ug.astype(F32))).astype(h.dtype), a_lat], axis=-1) @ w_out
    if not need_ctx:
        return y, None
    yc = jnp.concatenate([(r_ctx * jax.nn.gelu(ugc.astype(F32))).astype(h.dtype), a_ctx], axis=-1) @ w_out
    return y, yc


def odd_mixer(h, hc, w_in, w_out, sconv_w, sconv_b, dt_bias, a_log, d_skip, snorm_g,
              mconv_w, mconv_b, gate_b, mnorm_g, need_ctx):
    bsz = h.shape[0]

    def prep(t):
        L = t.shape[1]
        z, xbc, dt_raw, mq, mk, mv, mo, mg = _split(t @ w_in, ODD_SPLITS)
        xbc = jax.nn.silu(dw_conv(xbc, sconv_w, sconv_b))
        xs, Bm, Cm = _split(xbc, (SSD_WIDTH, SSD_GROUPS * SSD_STATE, SSD_GROUPS * SSD_STATE))
        qk = jax.nn.silu(dw_conv(jnp.concatenate([mq, mk], axis=-1), mconv_w, mconv_b))
        mq, mk = _split(qk, (ML_WIDTH, ML_WIDTH))
        ssd_in = (xs.reshape(bsz, L, SSD_HEADS, SSD_HEAD_DIM), dt_raw.reshape(bsz, L, 2, SSD_HEADS),
                  Bm.reshape(bsz, L, SSD_GROUPS, SSD_STATE), Cm.reshape(bsz, L, SSD_GROUPS, SSD_STATE))
        ml_in = (mq.reshape(bsz, L, ML_HEADS, ML_HEAD_DIM),
                 mk.reshape(bsz, L, ML_HEADS, ML_HEAD_DIM) * (ML_HEAD_DIM ** -0.5),
                 mv.reshape(bsz, L, ML_HEADS, ML_HEAD_DIM),
                 mg.reshape(bsz, L, 2, 2, ML_HEADS) + gate_b)
        return z, mo, ssd_in, ml_in
    z, mo, s_in, m_in = prep(h)
    zc, moc, s_inc, m_inc = prep(hc)

    def ssd_run(inp, d, state):
        xh, dt_raw, Bm, Cm = inp
        dt = jax.nn.softplus(dt_raw[:, :, d].astype(F32) + dt_bias[d].astype(F32))
        return ssd_chunked(xh, dt, -jnp.exp(a_log[d].astype(F32)), Bm, Cm, state)
    s0 = jnp.zeros((bsz, SSD_GROUPS, SSD_HEADS // SSD_GROUPS, SSD_HEAD_DIM, SSD_STATE), F32)
    s_lat, s_ctx = prefix_bidirectional(ssd_run, s_inc, s_in, s0, need_ctx)

    def ml_run(inp, d, state):
        q, k, v, g = inp
        return mlstm_chunked(q, k, v, g[:, :, d, 0], jax.nn.log_sigmoid(g[:, :, d, 1].astype(F32)), state)
    m0 = (jnp.zeros((bsz, ML_HEADS, ML_HEAD_DIM, ML_HEAD_DIM), F32), jnp.zeros((bsz, ML_HEADS, ML_HEAD_DIM), F32),
          jnp.full((bsz, ML_HEADS), NEG_INF, F32))
    m_lat, m_ctx = prefix_bidirectional(ml_run, m_inc, m_in, m0, need_ctx)

    def merge(ys, xh, zz, hm, o):
        L = ys.shape[1]
        ys = ys + d_skip.astype(F32)[:, None] * xh.astype(F32)
        ys = ys.reshape(bsz, L, SSD_WIDTH) * jax.nn.silu(zz.astype(F32))
        ys = rms_norm(ys.reshape(bsz, L, SSD_GROUPS, -1), snorm_g.reshape(SSD_GROUPS, -1)).reshape(bsz, L, SSD_WIDTH)
        hm = rms_norm(hm, mnorm_g.reshape(ML_HEADS, ML_HEAD_DIM)).reshape(bsz, L, ML_WIDTH)
        hm = hm * jax.nn.sigmoid(o.astype(F32))
        return jnp.concatenate([ys, hm], axis=-1).astype(h.dtype) @ w_out
    y = merge(s_lat, s_in[0], z, m_lat, mo)
    if not need_ctx:
        return y, None
    return y, merge(s_ctx, s_inc[0], zc, m_ctx, moc)


def hierarchical_moe(t, router_g, router_e, w1, w3, w2):
    T = t.shape[0]
    tf = t.astype(F32)
    g_logits = tf @ router_g.astype(F32)
    g_sel = jnp.argmax(g_logits, axis=-1)
    g_w = jnp.max(jax.nn.softmax(g_logits, axis=-1), axis=-1, keepdims=True)
    e_logits = (tf @ router_e.astype(F32)).reshape(T, MOE_GROUPS, MOE_EXPERTS_PER_GROUP)
    e_logits = e_logits[jnp.arange(T), g_sel]
    top_v, top_i = lax.top_k(e_logits, MOE_TOP_K)
    top_w = jax.nn.softmax(top_v, axis=-1) * g_w
    expert_id = g_sel[:, None] * MOE_EXPERTS_PER_GROUP + top_i
    gate = jnp.einsum('tk,tke->te', top_w, jax.nn.one_hot(expert_id, MOE_EXPERTS, dtype=F32))
    out = jnp.zeros(t.shape, F32)
    for e in range(MOE_EXPERTS):
        hid = jax.nn.silu(t @ w1[e]) * (t @ w3[e])
        out = out + gate[:, e:e + 1] * (hid @ w2[e])
    return out.astype(t.dtype)


def setup_inputs(seed: int = 0) -> dict:
    key = jax.random.key(seed)
    ks = iter(jax.random.split(key, 64))
    D = D_MODEL

    def nrm(shape, s):
        return s * jax.random.normal(next(ks), shape, F32)
    lam_u = jax.random.uniform(next(ks), (N_EVEN, 2, LRU_WIDTH), F32, 0.9, 0.999) ** (1.0 / LRU_C)
    lru_lam = jnp.log(lam_u) - jnp.log1p(-lam_u)
    dt0 = jnp.exp(jax.random.uniform(next(ks), (N_ODD, 2, SSD_HEADS), F32, math.log(1e-3), math.log(1e-1)))
    ssd_dt_bias = dt0 + jnp.log(-jnp.expm1(-dt0))
    ssd_a_log = jnp.log(jax.random.uniform(next(ks), (N_ODD, 2, SSD_HEADS), F32, 1.0, 16.0))
    ig_b = nrm((N_ODD, 2, 1, ML_HEADS), 0.1)
    fg_b = jnp.broadcast_to(jnp.linspace(3.0, 6.0, ML_HEADS), (N_ODD, 2, 1, ML_HEADS)) + nrm((N_ODD, 2, 1, ML_HEADS), 0.1)
    ml_gate_b = jnp.concatenate([ig_b, fg_b], axis=2)
    return {
        'x': nrm((BATCH, SEQ, D), 1.0),
        'c': nrm((BATCH, D), 1.0),
        'ctx': nrm((BATCH, CTX_LEN, D), 1.0),
        'c_ctx': nrm((D,), 1.0),
        'mod_w': nrm((DEPTH, D, 6 * D), 0.5 * D ** -0.5),
        'mod_b': nrm((DEPTH, 6 * D), 0.02),
        'norm1_g': 1.0 + nrm((DEPTH, D), 0.1),
        'norm2_g': 1.0 + nrm((DEPTH, D), 0.1),
        'even_w_in': nrm((N_EVEN, D, EVEN_IN), D ** -0.5),
        'even_w_out': nrm((N_EVEN, EVEN_MIX, D), EVEN_MIX ** -0.5),
        'lru_conv_w': nrm((N_EVEN, CONV_W, LRU_WIDTH), CONV_W ** -0.5),
        'lru_conv_b': nrm((N_EVEN, LRU_WIDTH), 0.02),
        'lru_wa': nrm((N_EVEN, 2, LRU_BLOCKS, LRU_BLOCK, LRU_BLOCK), LRU_BLOCK ** -0.5),
        'lru_ba': nrm((N_EVEN, 2, LRU_WIDTH), 0.02),
        'lru_wx': nrm((N_EVEN, 2, LRU_BLOCKS, LRU_BLOCK, LRU_BLOCK), LRU_BLOCK ** -0.5),
        'lru_bx': nrm((N_EVEN, 2, LRU_WIDTH), 0.02),
        'lru_lam': lru_lam,
        'na_q_g': 1.0 + nrm((N_EVEN, NA_HEAD_DIM), 0.1),
        'na_k_g': 1.0 + nrm((N_EVEN, NA_HEAD_DIM), 0.1),
        'na_rpb': nrm((N_EVEN, NA_HEADS, 2 * NA_WIN_R - 1, 2 * NA_WIN_C - 1), 0.1),
        'odd_w_in': nrm((N_ODD, D, ODD_IN), D ** -0.5),
        'odd_w_out': nrm((N_ODD, ODD_MIX, D), ODD_MIX ** -0.5),
        'ssd_conv_w': nrm((N_ODD, CONV_W, SSD_XBC), CONV_W ** -0.5),
        'ssd_conv_b': nrm((N_ODD, SSD_XBC), 0.02),
        'ssd_dt_bias': ssd_dt_bias,
        'ssd_a_log': ssd_a_log,
        'ssd_d': 1.0 + nrm((N_ODD, SSD_HEADS), 0.1),
        'ssd_norm_g': 1.0 + nrm((N_ODD, SSD_WIDTH), 0.1),
        'ml_conv_w': nrm((N_ODD, CONV_W, 2 * ML_WIDTH), CONV_W ** -0.5),
        'ml_conv_b': nrm((N_ODD, 2 * ML_WIDTH), 0.02),
        'ml_gate_b': ml_gate_b,
        'ml_norm_g': 1.0 + nrm((N_ODD, ML_WIDTH), 0.1),
        'moe_router_g': nrm((DEPTH, D, MOE_GROUPS), D ** -0.5),
        'moe_router_e': nrm((DEPTH, D, MOE_EXPERTS), D ** -0.5),
        'moe_w1': nrm((DEPTH, MOE_EXPERTS, D, MOE_FF), D ** -0.5),
        'moe_w3': nrm((DEPTH, MOE_EXPERTS, D, MOE_FF), D ** -0.5),
        'moe_w2': nrm((DEPTH, MOE_EXPERTS, MOE_FF, D), MOE_FF ** -0.5),
    }


def reference(x, c, ctx, c_ctx, mod_w, mod_b, norm1_g, norm2_g,
              even_w_in, even_w_out, lru_conv_w, lru_conv_b, lru_wa, lru_ba, lru_wx, lru_bx, lru_lam,
              na_q_g, na_k_g, na_rpb,
              odd_w_in, odd_w_out, ssd_conv_w, ssd_conv_b, ssd_dt_bias, ssd_a_log, ssd_d, ssd_norm_g,
              ml_conv_w, ml_conv_b, ml_gate_b, ml_norm_g,
              moe_router_g, moe_router_e, moe_w1, moe_w3, moe_w2):
    bsz, S, D = x.shape
    xc = ctx
    for l in range(DEPTH):
        need_ctx = l < DEPTH - 1
        j = l // 2
        m = (jax.nn.silu(c) @ mod_w[l] + mod_b[l]).reshape(bsz, 1, 6, D)
        mc = (jax.nn.silu(c_ctx) @ mod_w[l] + mod_b[l]).reshape(1, 1, 6, D)
        h = rms_norm(x, norm1_g[l]) * (1 + m[:, :, 1]) + m[:, :, 0]
        hc = rms_norm(xc, norm1_g[l]) * (1 + mc[:, :, 1]) + mc[:, :, 0]
        if l % 2 == 0:
            y, yc = even_mixer(h, hc, even_w_in[j], even_w_out[j], lru_conv_w[j], lru_conv_b[j], lru_wa[j], lru_ba[j],
                               lru_wx[j], lru_bx[j], lru_lam[j], na_q_g[j], na_k_g[j], na_rpb[j], need_ctx)
        else:
            y, yc = odd_mixer(h, hc, odd_w_in[j], odd_w_out[j], ssd_conv_w[j], ssd_conv_b[j], ssd_dt_bias[j],
                              ssd_a_log[j], ssd_d[j], ssd_norm_g[j], ml_conv_w[j], ml_conv_b[j], ml_gate_b[j],
                              ml_norm_g[j], need_ctx)
        x = x + m[:, :, 2] * y
        h = rms_norm(x, norm2_g[l]) * (1 + m[:, :, 4]) + m[:, :, 3]
        if need_ctx:
            xc = xc + mc[:, :, 2] * yc
            hc = rms_norm(xc, norm2_g[l]) * (1 + mc[:, :, 4]) + mc[:, :, 3]
            tokens = jnp.concatenate([h.reshape(-1, D), hc.reshape(-1, D)], axis=0)
            out = hierarchical_moe(tokens, moe_router_g[l], moe_router_e[l], moe_w1[l], moe_w3[l], moe_w2[l])
            x = x + m[:, :, 5] * out[:bsz * S].reshape(bsz, S, D)
            xc = xc + mc[:, :, 5] * out[bsz * S:].reshape(xc.shape)
        else:
            out = hierarchical_moe(h.reshape(-1, D), moe_router_g[l], moe_router_e[l], moe_w1[l], moe_w3[l], moe_w2[l])
            x = x + m[:, :, 5] * out.reshape(bsz, S, D)
    return x
```

```python
import numpy as np
import concourse.bass as bass
import concourse.mybir as mybir
from concourse.bass_utils import run_bass_kernel_spmd
from contextlib import ExitStack

F32 = mybir.dt.float32
BF16 = mybir.dt.bfloat16
I32 = mybir.dt.int32
AF = mybir.ActivationFunctionType
ALU = mybir.AluOpType
AX = mybir.AxisListType
NDS = 40
SAME_ENGINE_WAIT = True


class Buf:
    def __init__(self, t, name=""):
        self.t = t
        self.name = name
        self.last_w = None
        self.reads = {}

    def __getitem__(self, k):
        return self.t[k]


class KB:
    ENG = ["pe", "dve", "act", "pool", "sp"]

    def __init__(self, nc, es):
        self.nc = nc
        self.es = es
        self.esem = {e: es.enter_context(nc.semaphore("s_" + e)) for e in self.ENG}
        self.ecnt = {e: 0 for e in self.ENG}
        self.dsem = [es.enter_context(nc.semaphore("d%d" % i)) for i in range(NDS)]
        self.dval = [0] * NDS
        self.dnext = 0
        self.seen = {e: {} for e in self.ENG}
        self.prog = {e: [] for e in self.ENG}
        self.nbuf = 0

    def sb(self, shape, dtype=F32, name=None):
        self.nbuf += 1
        name = name or "t%d" % self.nbuf
        t = self.es.enter_context(self.nc.sbuf_tensor(name, list(shape), dtype))
        return Buf(t, name)

    def ps(self, shape, dtype=F32, name=None):
        self.nbuf += 1
        name = name or "p%d" % self.nbuf
        t = self.es.enter_context(self.nc.psum_tensor(name, list(shape), dtype))
        return Buf(t, name)

    def view(self, t, name=""):
        return Buf(t, name)

    def _wait(self, e, ev):
        if ev is None:
            return
        sem, val, key = ev
        if key == e and (e == "pe" or not SAME_ENGINE_WAIT):
            return
        if self.seen[e].get(key, 0) >= val:
            return
        self.seen[e][key] = val
        self.prog[e].append(lambda eng, sem=sem, val=val: eng.wait_ge(sem, val))

    def _deps(self, e, reads, writes):
        for b in reads:
            self._wait(e, b.last_w)
        for b in writes:
            self._wait(e, b.last_w)
            for r in b.reads.values():
                self._wait(e, r)

    def _commit(self, ev, reads, writes):
        for b in reads:
            b.reads[ev[2]] = ev
        for b in writes:
            b.last_w = ev
            b.reads = {}

    def op(self, e, fn, reads=(), writes=()):
        self._deps(e, reads, writes)
        self.ecnt[e] += 1
        sem = self.esem[e]
        ev = (sem, self.ecnt[e], e)
        self.prog[e].append(lambda eng, fn=fn, sem=sem: fn(eng).then_inc(sem, 1))
        self._commit(ev, reads, writes)

    def dma(self, q, out_ap, in_ap, reads=(), writes=(), **kw):
        self._deps(q, reads, writes)
        i = self.dnext
        self.dnext = (i + 1) % NDS
        if self.dval[i] > 0:
            self._wait(q, (self.dsem[i], self.dval[i], ("d", i)))
        self.dval[i] += 16
        sem = self.dsem[i]
        ev = (sem, self.dval[i], ("d", i))
        self.prog[q].append(lambda eng, o=out_ap, a=in_ap, sem=sem, kw=kw: eng.dma_start(out=o, in_=a, **kw).then_inc(sem, 16))
        self._commit(ev, reads, writes)

    def barrier(self):
        for e in self.ENG:
            for e2 in self.ENG:
                if self.ecnt[e2] > 0 and not (e == "pe" and e2 == "pe"):
                    self._wait(e, (self.esem[e2], self.ecnt[e2], e2))
            for i in range(NDS):
                if self.dval[i] > 0:
                    self._wait(e, (self.dsem[i], self.dval[i], ("d", i)))
            if getattr(self, "ccn", 0) > 0:
                self._wait(e, (self.ccsem, self.ccn, "cc"))

    def scope(self):
        kb = self
        class _S:
            def __enter__(s_):
                s_.old = kb.es
                s_.st = ExitStack()
                s_.st.__enter__()
                kb.es = s_.st
                return s_
            def __exit__(s_, *a):
                kb.barrier()
                kb.es = s_.old
                return s_.st.__exit__(*a)
        return _S()

    def ident(self, dtype=F32):
        t = self.sb([128, 128], dtype)
        self.op("pool", lambda e: e.memset(t[:], 1.0), writes=[t])
        self.op("pool", lambda e: e.affine_select(t[:], t[:], [[-1, 128]], ALU.is_equal, 0.0, base=0, channel_multiplier=1), reads=[t], writes=[t])
        return t

    def finish(self):
        for i in range(NDS):
            if self.dval[i] > 0:
                self._wait("sp", (self.dsem[i], self.dval[i], ("d", i)))
        nc = self.nc
        with nc.Block() as block:
            def mk(e):
                def body(eng):
                    for f in self.prog[e]:
                        f(eng)
                return body
            block.tensor(mk("pe"))
            block.vector(mk("dve"))
            block.scalar(mk("act"))
            block.gpsimd(mk("pool"))
            block.sync(mk("sp"))


def load_cast_weight(kb, w_dram, K, M, wbf, stage_bufs, q="sp", cast_engs=("pool","dve")):
    KT = K // 128
    wv = w_dram.rearrange("(kt p) m -> p kt m", p=128)
    CH = stage_bufs[0].t.shape[-1]
    i = 0
    for kt in range(KT):
        for c0 in range(0, M, CH):
            c1 = min(M, c0 + CH)
            st = stage_bufs[i % len(stage_bufs)]
            kb.dma(q, st[:, 0:c1 - c0], wv[:, kt, c0:c1], writes=[st])
            ce = cast_engs[i % len(cast_engs)]
            kb.op(ce, lambda e, st=st, kt=kt, c0=c0, c1=c1: e.tensor_copy(wbf[:, kt, c0:c1], st[:, 0:c1 - c0]), reads=[st], writes=[wbf])
            i += 1


def lay8(v):
    return np.ascontiguousarray(v.reshape(8, 128).T)


TOT = 4352; NLAT = 4096; NCTX = 256
CH = [(i * 512, 512) for i in range(8)] + [(4096, 256)]

def emit_mixe(kb, ld, cw, wbd, lp, qkg, bias, st):
    if True:
        ident = kb.ident(F32)
        identb = kb.sb([128, 128], BF16)
        kb.op("dve", lambda e: e.tensor_copy(identb[:], ident[:]), reads=[ident], writes=[identb])
        cws = kb.sb([128, 2, 5]); lps = kb.sb([128, 2, 2, 3]); qkgs = kb.sb([128, 2])
        kb.dma("sp", cws[:], cw, writes=[cws]); kb.dma("sp", lps[:], lp, writes=[lps]); kb.dma("sp", qkgs[:], qkg, writes=[qkgs])
        with kb.scope():
            sp_ = kb.sb([128, 2, 2, 1])
            kb.op("act", lambda e: e.activation(sp_[:, :, :, 0], lps[:, :, :, 2], AF.Exp, scale=-1.0), reads=[lps], writes=[sp_])
            kb.op("act", lambda e: e.activation(sp_[:, :, :, 0], sp_[:, :, :, 0], AF.Ln, bias=1.0), reads=[sp_], writes=[sp_])
            kb.op("dve", lambda e: e.tensor_scalar(sp_[:, :, :, 0], sp_[:, :, :, 0], -8.0, None, ALU.mult), reads=[sp_], writes=[sp_])
            x = kb.sb([128, TOT]); xc = kb.sb([128, TOT]); ug = kb.sb([128, TOT])
            ra = [kb.sb([128, TOT]) for _ in range(2)]; iu = [kb.sb([128, TOT]) for _ in range(2)]
            s_ = kb.sb([128, TOT])
            wts = kb.sb([128, 2, 2, 128])
            pg = [kb.ps([128, 512]) for _ in range(4)]
            for j in range(2):
                ld(x, 0 + j)
                ld(ug, 2 + j)
                kb.dma("sp", wts[:], wbd[j].rearrange("d g k m -> k d g m"), writes=[wts])
                W = lambda i, j=j: cws[:, j, i:i+1]
                kb.op("dve", lambda e, W=W: e.tensor_scalar(xc[:], x[:], W(2), W(4), ALU.mult, ALU.add), reads=[x, cws], writes=[xc])
                for (a, b) in ((0, NLAT), (NLAT, TOT)):
                    kb.op("dve", lambda e, a=a, b=b, W=W: e.scalar_tensor_tensor(xc[:, a+2:b], x[:, a:b-2], W(0), xc[:, a+2:b], ALU.mult, ALU.add), reads=[x, xc, cws], writes=[xc])
                    kb.op("dve", lambda e, a=a, b=b, W=W: e.scalar_tensor_tensor(xc[:, a+1:b], x[:, a:b-1], W(1), xc[:, a+1:b], ALU.mult, ALU.add), reads=[x, xc, cws], writes=[xc])
                    kb.op("dve", lambda e, a=a, b=b, W=W: e.scalar_tensor_tensor(xc[:, a:b-1], x[:, a+1:b], W(3), xc[:, a:b-1], ALU.mult, ALU.add), reads=[x, xc, cws], writes=[xc])
                pi = 0
                for d in range(2):
                    for g in range(2):
                        dst = ra[d] if g == 0 else iu[d]
                        for (c0, n) in CH:
                            p_ = pg[pi % 4]; pi += 1
                            kb.op("pe", lambda e, d=d, g=g, p_=p_, c0=c0, n=n: e.matmul(p_[:, 0:n], wts[:, d, g, :], xc[:, c0:c0+n], start=True, stop=True), reads=[wts, xc], writes=[p_])
                            kb.op("act", lambda e, d=d, g=g, p_=p_, c0=c0, n=n, dst=dst, j=j: e.activation(dst[:, c0:c0+n], p_[:, 0:n], AF.Sigmoid, bias=lps[:, j, d, g:g+1]), reads=[p_, lps], writes=[dst])
                for d in range(2):
                    kb.op("act", lambda e, d=d, j=j: e.activation(ra[d][:], ra[d][:], AF.Exp, scale=sp_[:, j, d, :]), reads=[ra[d], sp_], writes=[ra[d]])
                for d in range(2):
                    kb.op("pool", lambda e, d=d: e.tensor_tensor(s_[:], ra[d][:], ra[d][:], ALU.mult), reads=[ra[d]], writes=[s_])
                    kb.op("act", lambda e: e.activation(s_[:], s_[:], AF.Sqrt, scale=-1.0, bias=1.0), reads=[s_], writes=[s_])
                    kb.op("pool", lambda e, d=d: e.tensor_tensor(iu[d][:], iu[d][:], xc[:], ALU.mult), reads=[iu[d], xc], writes=[iu[d]])
                    kb.op("dve", lambda e, d=d: e.tensor_tensor(iu[d][:], iu[d][:], s_[:], ALU.mult), reads=[iu[d], s_], writes=[iu[d]])
                hf = x; hb = xc
                kb.op("dve", lambda e: e.tensor_tensor_scan(hf[:, NLAT:TOT], ra[0][:, NLAT:TOT], iu[0][:, NLAT:TOT], 0.0, ALU.mult, ALU.add), reads=[ra[0], iu[0]], writes=[hf])
                kb.op("dve", lambda e: e.tensor_tensor_scan(hf[:, 0:NLAT], ra[0][:, 0:NLAT], iu[0][:, 0:NLAT], hf[:, TOT-1:TOT], ALU.mult, ALU.add), reads=[ra[0], iu[0], hf], writes=[hf])
                kb.op("dve", lambda e: e.tensor_tensor_scan(hb[:, TOT-1:NLAT-1:-1], ra[1][:, TOT-1:NLAT-1:-1], iu[1][:, TOT-1:NLAT-1:-1], 0.0, ALU.mult, ALU.add), reads=[ra[1], iu[1]], writes=[hb])
                kb.op("dve", lambda e: e.tensor_tensor_scan(hb[:, NLAT-1::-1], ra[1][:, NLAT-1::-1], iu[1][:, NLAT-1::-1], hb[:, NLAT:NLAT+1], ALU.mult, ALU.add), reads=[ra[1], iu[1], hb], writes=[hb])
                kb.op("pool", lambda e: e.tensor_tensor(hf[:], hf[:], hb[:], ALU.add), reads=[hf, hb], writes=[hf])
                t1 = ra[0]; t2 = ra[1]
                kb.op("pool", lambda e: e.tensor_tensor(t1[:], ug[:], ug[:], ALU.mult), reads=[ug], writes=[t1])
                kb.op("dve", lambda e: e.tensor_scalar(t1[:], t1[:], 0.044715, 1.0, ALU.mult, ALU.add), reads=[t1], writes=[t1])
                kb.op("pool", lambda e: e.tensor_tensor(t1[:], t1[:], ug[:], ALU.mult), reads=[t1, ug], writes=[t1])
                kb.op("act", lambda e: e.activation(t2[:], t1[:], AF.Sigmoid, scale=1.5957691216057308), reads=[t1], writes=[t2])
                kb.op("dve", lambda e: e.tensor_tensor(t2[:], t2[:], ug[:], ALU.mult), reads=[t2, ug], writes=[t2])
                kb.op("pool", lambda e: e.tensor_tensor(t2[:], t2[:], hf[:], ALU.mult), reads=[t2, hf], writes=[t2])
                st(j, t2)
        with kb.scope():
            bones = kb.sb([128, 128], BF16)
            kb.op("pool", lambda e: e.memset(bones[:], 0.0), writes=[bones])
            kb.op("pool", lambda e: e.memset(bones[0:64, 0:64], 1.0), writes=[bones])
            kb.op("pool", lambda e: e.memset(bones[64:128, 64:128], 1.0), writes=[bones])
            qf = kb.sb([128, TOT]); kf = kb.sb([128, TOT])
            sq = kb.sb([128, 512], BF16); rs_ = kb.sb([128, 512])
            Qbd = kb.sb([128, 68, 2, 64], BF16); Kn = kb.sb([128, TOT], BF16)
            vst = kb.sb([128, TOT]); V0 = kb.sb([128, 34, 128], BF16); V1 = kb.sb([128, 33, 128], BF16)
            bs = kb.sb([128, 8, 512])
            on = kb.sb([128, TOT])
            gsc = kb.sb([128, 2])
            kb.op("dve", lambda e: e.tensor_scalar(gsc[:, 0:1], qkgs[:, 0:1], 0.125, None, ALU.mult), reads=[qkgs], writes=[gsc])
            kb.op("dve", lambda e: e.tensor_copy(gsc[:, 1:2], qkgs[:, 1:2]), reads=[qkgs], writes=[gsc])
            pss = kb.ps([128, 512])
            psw = [kb.ps([128, 512]) for _ in range(2)]; psc = [kb.ps([128, 512]) for _ in range(2)]
            ppt = kb.ps([128, 1024], BF16) ; pso = kb.ps([128, 512]); pot = kb.ps([128, 512]); ptr = [pss, pso]
            sb = [kb.sb([128, 768]) for _ in range(2)]; P = [kb.sb([128, 768], BF16) for _ in range(2)]; Osb = [kb.sb([128, 128]) for _ in range(2)]
            ptb = [kb.sb([128, 768], BF16) for _ in range(2)]; Dm = [kb.sb([128, 128]) for _ in range(2)]
            sm = [kb.sb([128, 4]) for _ in range(3)]
            for j in range(2):
                ld(qf, 4 + j)
                ld(kf, 6 + j)
                kb.dma("sp", bs[:], bias[j], writes=[bs])
                ld(vst, 8 + j)
                for bi in range(34):
                    p_ = ptr[bi % 2]
                    kb.op("pe", lambda e, bi=bi, p_=p_: e.transpose(p_[:, 0:128], vst[:, bi*128:(bi+1)*128], ident[:]), reads=[vst, ident], writes=[p_])
                    kb.op("act", lambda e, bi=bi, p_=p_: e.activation(V0[:, bi, :], p_[:, 0:128], AF.Copy), reads=[p_], writes=[V0])
                for bi in range(33):
                    p_ = ptr[bi % 2]
                    kb.op("pe", lambda e, bi=bi, p_=p_: e.transpose(p_[:, 0:128], vst[:, 64+bi*128:64+(bi+1)*128], ident[:]), reads=[vst, ident], writes=[p_])
                    kb.op("dve", lambda e, bi=bi, p_=p_: e.tensor_copy(V1[:, bi, :], p_[:, 0:128]), reads=[p_], writes=[V1])
                kb.op("pool", lambda e: e.memset(Qbd[:], 0.0), writes=[Qbd])
                for which, src in ((0, qf), (1, kf)):
                    for (c0, n) in CH:
                        kb.op("act", lambda e, src=src, c0=c0, n=n: e.activation(sq[:, 0:n], src[:, c0:c0+n], AF.Square), reads=[src], writes=[sq])
                        kb.op("pe", lambda e, n=n: e.matmul(pss[:, 0:n], bones[:], sq[:, 0:n], start=True, stop=True), reads=[bones, sq], writes=[pss])
                        kb.op("act", lambda e, n=n: e.activation(rs_[:, 0:n], pss[:, 0:n], AF.Sqrt, scale=1.0 / 64, bias=1e-6), reads=[pss], writes=[rs_])
                        kb.op("dve", lambda e, n=n: e.reciprocal(rs_[:, 0:n], rs_[:, 0:n]), reads=[rs_], writes=[rs_])
                        if which == 1:
                            kb.op("dve", lambda e, c0=c0, n=n: e.scalar_tensor_tensor(Kn[:, c0:c0+n], kf[:, c0:c0+n], gsc[:, 1:2], rs_[:, 0:n], ALU.mult, ALU.mult), reads=[kf, gsc, rs_], writes=[Kn])
                        else:
                            r0 = c0 // 64; nr = n // 64
                            for hh in range(2):
                                pa, pb = hh * 64, hh * 64 + 64
                                kb.op("dve", lambda e, c0=c0, n=n, r0=r0, nr=nr, hh=hh, pa=pa, pb=pb: e.scalar_tensor_tensor(
                                    Qbd[pa:pb, r0:r0+nr, hh, :], qf[pa:pb, c0:c0+n].rearrange("p (r c) -> p r c", c=64), gsc[pa:pb, 0:1],
                                    rs_[pa:pb, 0:n].rearrange("p (r c) -> p r c", c=64), ALU.mult, ALU.mult), reads=[qf, gsc, rs_], writes=[Qbd])
                def stS(r):
                        b_ = r % 2
                        sb_, P_, ptb_, D_, sm_ = sb[b_], P[b_], ptb[b_], Dm[b_], sm[r % 3]
                        pw, pc = psw[b_], psc[b_]
                        lhs = Qbd[:, r, :, :].rearrange("p a c -> p (a c)")
                        if r < 64:
                            rs = min(max(r - 4, 0), 56)
                            cls = r if r < 4 else (4 if r <= 60 else r - 56)
                            kb.op("pe", lambda e, lhs=lhs, rs=rs, pw=pw: e.matmul(pw[:], lhs, Kn[:, rs*64:rs*64+512], start=True, stop=True), reads=[Qbd, Kn], writes=[pw])
                            kb.op("dve", lambda e, pw=pw, sb_=sb_, cls=cls: e.tensor_tensor(sb_[:, 0:512], pw[:], bs[:, cls, :], ALU.add), reads=[pw, bs], writes=[sb_])
                            w0 = 0; nb = 6
                        else:
                            w0 = 512; nb = 2
                        kb.op("pe", lambda e, lhs=lhs, pc=pc: e.matmul(pc[:, 0:256], lhs, Kn[:, NLAT:TOT], start=True, stop=True), reads=[Qbd, Kn], writes=[pc])
                        kb.op("act", lambda e, pc=pc, sb_=sb_: e.activation(sb_[:, 512:768], pc[:, 0:256], AF.Copy), reads=[pc], writes=[sb_])
                        kb.op("dve", lambda e, sb_=sb_, sm_=sm_, w0=w0: e.reduce_max(sm_[:, 0:1], sb_[:, w0:768], AX.X, negate=True), reads=[sb_], writes=[sm_])
                        kb.op("act", lambda e, sb_=sb_, sm_=sm_, P_=P_, w0=w0: e.activation(P_[:, w0:768], sb_[:, w0:768], AF.Exp, bias=sm_[:, 0:1], accum_out=sm_[:, 1:2]), reads=[sb_, sm_], writes=[P_, sm_])
                        kb.op("dve", lambda e, sm_=sm_: e.reciprocal(sm_[:, 2:3], sm_[:, 1:2]), reads=[sm_], writes=[sm_])
                def stB(r):
                        b_ = r % 2
                        sb_, P_, ptb_, D_, sm_ = sb[b_], P[b_], ptb[b_], Dm[b_], sm[r % 3]
                        if r < 64:
                            rs = min(max(r - 4, 0), 56)
                            w0 = 0; nb = 6
                        else:
                            w0 = 512; nb = 2
                        for bi in range(nb):
                            o0 = w0 + bi * 128
                            kb.op("pe", lambda e, o0=o0, P_=P_: e.transpose(ppt[:, o0:o0+128], P_[:, o0:o0+128], identb[:]), reads=[P_, identb], writes=[ppt])
                        if nb == 6:
                            kb.op("act", lambda e, ptb_=ptb_: e.activation(ptb_[:, 0:384], ppt[:, 0:384], AF.Copy), reads=[ppt], writes=[ptb_])
                            kb.op("dve", lambda e, ptb_=ptb_: e.tensor_copy(ptb_[:, 384:768], ppt[:, 384:768]), reads=[ppt], writes=[ptb_])
                        else:
                            kb.op("act", lambda e, ptb_=ptb_: e.activation(ptb_[:, 512:768], ppt[:, 512:768], AF.Copy), reads=[ppt], writes=[ptb_])
                def stC(r):
                        b_ = r % 2
                        sb_, P_, ptb_, D_, sm_ = sb[b_], P[b_], ptb[b_], Dm[b_], sm[r % 3]
                        if r < 64:
                            rs = min(max(r - 4, 0), 56)
                            w0 = 0; nb = 6
                        else:
                            w0 = 512; nb = 2
                        for bi in range(nb):
                            o0 = w0 + bi * 128
                            if o0 < 512:
                                Vt = (V0, rs // 2 + bi) if rs % 2 == 0 else (V1, (rs - 1) // 2 + bi)
                            else:
                                Vt = (V0, 32 + (o0 - 512) // 128)
                            kb.op("pe", lambda e, o0=o0, ptb_=ptb_, Vt=Vt, bi=bi, nb=nb: e.matmul(pso[:, 0:128], ptb_[:, o0:o0+128], Vt[0][:, Vt[1], :], start=(bi == 0), stop=(bi == nb - 1)), reads=[Vt[0], ptb_], writes=[pso])
                        O_ = Osb[b_]
                        kb.op("act", lambda e, O_=O_, sm_=sm_: e.activation(O_[:], pso[:, 0:128], AF.Copy, scale=sm_[:, 2:3]), reads=[pso, sm_], writes=[O_])
                        kb.op("pe", lambda e, O_=O_: e.transpose(pot[:, 0:128], O_[:], ident[:]), reads=[O_, ident], writes=[pot])
                        kb.op("act", lambda e, r=r: e.activation(on[0:64, r*64:(r+1)*64], pot[0:64, 0:64], AF.Copy), reads=[pot], writes=[on])
                        kb.op("dve", lambda e, r=r: e.tensor_copy(on[64:128, r*64:(r+1)*64], pot[64:128, 64:128]), reads=[pot], writes=[on])

                stS(0); stS(1); stB(0)
                for r in range(68):
                    if r + 2 < 68:
                        stS(r + 2)
                    stC(r)
                    if r + 1 < 68:
                        stB(r + 1)
                st(2 + j, on)

def na_bias(rpb):
    reps = [0, 1, 2, 3, 30, 61, 62, 63]
    cq = np.arange(64); ck = np.arange(64)
    cs = np.clip(cq - 8, 0, 48)
    valid = (ck[None, :] >= cs[:, None]) & (ck[None, :] < cs[:, None] + 16)
    dcol = np.clip(ck[None, :] - cq[:, None] + 15, 0, 30)
    out = np.full((8, 8, 64, 8, 64), -1e30, np.float32)
    for ci, r in enumerate(reps):
        rs = min(max(r - 4, 0), 56)
        for j in range(8):
            drow = rs + j - r + 7
            g = rpb[:, drow][:, dcol]
            out[:, ci, :, j, :] = np.where(valid[None], g, np.float32(-1e30))
    return out.reshape(8, 8, 64, 512)

def blockdiag2(wblk):
    o = np.zeros((128, 128), np.float32)
    o[0:64, 0:64] = wblk[0]; o[64:128, 64:128] = wblk[1]
    return o

def mixe_inputs(uT, hf, P):
    c0 = hf * 256
    ux = uT[c0:c0+256]; ug = uT[512+c0:512+c0+256]
    q = uT[1024+c0:1024+c0+256]; k = uT[1536+c0:1536+c0+256]; v = uT[2048+c0:2048+c0+256]
    cw = np.zeros((128, 2, 5), np.float32)
    for j in range(2):
        cw[:, j, 0:4] = P['lru_conv_w'][:, c0+j*128:c0+(j+1)*128].T
        cw[:, j, 4] = P['lru_conv_b'][c0+j*128:c0+(j+1)*128]
    wbd = np.zeros((2, 2, 2, 128, 128), np.float32)
    lp = np.zeros((128, 2, 2, 3), np.float32)
    for j in range(2):
        for d in range(2):
            b0 = hf * 4 + j * 2
            wbd[j, d, 0] = blockdiag2(P['lru_wa'][d, b0:b0+2]); wbd[j, d, 1] = blockdiag2(P['lru_wx'][d, b0:b0+2])
            sl = slice(c0+j*128, c0+(j+1)*128)
            lp[:, j, d, 0] = P['lru_ba'][d, sl]; lp[:, j, d, 1] = P['lru_bx'][d, sl]; lp[:, j, d, 2] = P['lru_lam'][d, sl]
    qkg = np.stack([np.tile(P['na_q_g'], 2), np.tile(P['na_k_g'], 2)], 1).astype(np.float32)
    nb = na_bias(P['na_rpb'])
    bias = np.zeros((2, 128, 8, 512), np.float32)
    for j in range(2):
        h0 = hf * 4 + j * 2
        bias[j, 0:64] = nb[h0].transpose(1, 0, 2); bias[j, 64:128] = nb[h0+1].transpose(1, 0, 2)
    C = np.ascontiguousarray
    return {"uxT": C(ux), "ugT": C(ug), "qT": C(q), "kT": C(k), "vT": C(v), "cw": cw, "wbd": wbd, "lp": lp, "qkg": qkg, "bias": bias}


TOT = 4352; NLAT = 4096
NCH = 34
ORDER = {0: [32, 33] + list(range(32)), 1: [33, 32] + list(range(31, -1, -1))}

def conv_silu(kb, src, dst, cp, ti, tmp, func=AF.Silu):
    W = lambda i: cp[:, ti, i:i+1]
    kb.op("dve", lambda e: e.tensor_scalar(tmp[:], src[:], W(2), W(4), ALU.mult, ALU.add), reads=[src, cp], writes=[tmp])
    for (a, b) in ((0, NLAT), (NLAT, TOT)):
        kb.op("dve", lambda e, a=a, b=b: e.scalar_tensor_tensor(tmp[:, a+2:b], src[:, a:b-2], W(0), tmp[:, a+2:b], ALU.mult, ALU.add), reads=[src, tmp, cp], writes=[tmp])
        kb.op("dve", lambda e, a=a, b=b: e.scalar_tensor_tensor(tmp[:, a+1:b], src[:, a:b-1], W(1), tmp[:, a+1:b], ALU.mult, ALU.add), reads=[src, tmp, cp], writes=[tmp])
        kb.op("dve", lambda e, a=a, b=b: e.scalar_tensor_tensor(tmp[:, a:b-1], src[:, a+1:b], W(3), tmp[:, a:b-1], ALU.mult, ALU.add), reads=[src, tmp, cp], writes=[tmp])
    kb.op("act", lambda e: e.activation(dst, tmp[:], func), reads=[tmp], writes=[dst.buf] if hasattr(dst, "buf") else [])

def emit_mixo(kb, ld, ld_rows, rowp, convp, chp, st):
    if True:
        ident = kb.ident(F32)
        identb = kb.sb([128, 128], BF16)
        kb.op("dve", lambda e: e.tensor_copy(identb[:], ident[:]), reads=[ident], writes=[identb])
        onesf = kb.sb([128, 128], F32); onesb = kb.sb([128, 128], BF16)
        kb.op("pool", lambda e: e.memset(onesf[:], 1.0), writes=[onesf])
        kb.op("pool", lambda e: e.memset(onesb[:], 1.0), writes=[onesb])
        maskf = kb.sb([128, 128], F32); maskb = kb.sb([128, 128], F32)
        kb.op("pool", lambda e: e.affine_select(maskf[:], onesf[:], [[1, 128]], ALU.is_ge, 0.0, base=0, channel_multiplier=-1), reads=[onesf], writes=[maskf])
        kb.op("pool", lambda e: e.affine_select(maskb[:], onesf[:], [[-1, 128]], ALU.is_ge, 0.0, base=0, channel_multiplier=1), reads=[onesf], writes=[maskb])
        masks = [maskf, maskb]
        cps = kb.sb([128, 8, 5]); chs = kb.sb([128, 2, 3]); rps = kb.sb([128, 2])
        kb.dma("sp", cps[:], convp, writes=[cps]); kb.dma("sp", chs[:], chp, writes=[chs]); kb.dma("sp", rps[:], rowp, writes=[rps])
        RC = kb.sb([48, TOT], F32)
        cols = kb.sb([128, NCH, 48], F32)
        DC = kb.sb([128, NCH, 4, 12], F32)
        sel = kb.sb([48, 12, 128], F32)
        with kb.scope():
            rs = kb.sb([128, TOT]); t1 = kb.sb([128, TOT]); t2 = kb.sb([128, TOT]); t3 = kb.sb([128, TOT])
            Tg = kb.sb([128, TOT]); Tl = kb.sb([128, TOT]); Gf = kb.sb([128, TOT]); Gb = kb.sb([128, TOT])
            ld_rows(rs)
            acol = kb.sb([128, 1])
            kb.op("act", lambda e: e.activation(acol[0:8, :], rps[0:8, 1:2], AF.Exp), reads=[rps], writes=[acol])
            kb.op("dve", lambda e: e.tensor_scalar(acol[0:8, :], acol[0:8, :], -1.0, None, ALU.mult), reads=[acol], writes=[acol])
            for (p0, p1) in ((0, 8), (32, 36)):
                S = slice(p0, p1)
                kb.op("dve", lambda e, S=S: e.tensor_scalar(t1[S, :], rs[S, :], rps[S, 0:1], None, ALU.add), reads=[rs, rps], writes=[t1])
                kb.op("dve", lambda e, S=S: e.scalar_tensor_tensor(t2[S, :], t1[S, :], -1.0, t1[S, :], ALU.mult, ALU.max), reads=[t1], writes=[t2])
                kb.op("act", lambda e, S=S: e.activation(t2[S, :], t2[S, :], AF.Exp, scale=-1.0), reads=[t2], writes=[t2])
                kb.op("act", lambda e, S=S: e.activation(t2[S, :], t2[S, :], AF.Ln, bias=1.0), reads=[t2], writes=[t2])
            S = slice(0, 8)
            kb.op("dve", lambda e: e.scalar_tensor_tensor(t3[S, :], t1[S, :], 0.0, t2[S, :], ALU.max, ALU.add), reads=[t1, t2], writes=[t3])
            kb.op("dve", lambda e: e.tensor_scalar(Tg[S, :], t3[S, :], acol[S, 0:1], None, ALU.mult), reads=[t3, acol], writes=[Tg])
            kb.op("act", lambda e: e.activation(Tl[S, :], t3[S, :], AF.Ln), reads=[t3], writes=[Tl])
            S2 = slice(32, 36)
            kb.op("dve", lambda e: e.scalar_tensor_tensor(Tg[S2, :], t1[S2, :], 0.0, t2[S2, :], ALU.min, ALU.subtract), reads=[t1, t2], writes=[Tg])
            S3 = slice(96, 100)
            kb.op("dve", lambda e: e.tensor_scalar(Tl[S3, :], rs[S3, :], rps[S3, 0:1], None, ALU.add), reads=[rs, rps], writes=[Tl])
            for S_ in (S, S2):
                for ci in range(NCH):
                    a, b = ci * 128, ci * 128 + 128
                    kb.op("dve", lambda e, S_=S_, a=a, b=b: e.tensor_tensor_scan(Gf[S_, a:b], onesf[S_, :], Tg[S_, a:b], 0.0, ALU.mult, ALU.add), reads=[onesf, Tg], writes=[Gf])
                    kb.op("dve", lambda e, S_=S_, a=a, b=b: e.tensor_tensor_scan(Gb[S_, b-1:(a-1 if a > 0 else None):-1], onesf[S_, :], Tg[S_, b-1:(a-1 if a > 0 else None):-1], 0.0, ALU.mult, ALU.add), reads=[onesf, Tg], writes=[Gb])
            for (dst0, src, p0, n) in ((0, Tg, 0, 8), (8, Tg, 32, 4), (12, Tl, 0, 8), (20, Tl, 96, 4), (24, Gf, 0, 8), (32, Gf, 32, 4), (36, Gb, 0, 8), (44, Gb, 32, 4)):
                kb.dma("sp", RC[dst0:dst0+n, :], src[p0:p0+n, :], reads=[src], writes=[RC])
            pt = [kb.ps([128, 512]) for _ in range(2)]
            for ci in range(NCH):
                p_ = pt[ci % 2]
                kb.op("pe", lambda e, ci=ci, p_=p_: e.transpose(p_[:, 0:48], RC[0:48, ci*128:(ci+1)*128], ident[0:48, 0:48]), reads=[RC, ident], writes=[p_])
                kb.op("act", lambda e, ci=ci, p_=p_: e.activation(cols[:, ci, :], p_[:, 0:48], AF.Copy), reads=[p_], writes=[cols])
            pg = kb.ps([128, 512])
            kb.op("pe", lambda e: e.matmul(pg[:, 0:NCH*12].rearrange("p (c j) -> p c j", j=12), onesf[:], cols[:, :, 0:12], start=True, stop=True), reads=[onesf, cols], writes=[pg])
            kb.op("act", lambda e: e.activation(DC[:, :, 3, :], pg[:, 0:NCH*12].rearrange("p (c j) -> p c j", j=12), AF.Copy), reads=[pg], writes=[DC])
            for (j0, j1, d) in ((0, 4, 0), (4, 8, 1), (8, 10, 0), (10, 12, 1)):
                g0 = 24 if d == 0 else 36
                kb.op("dve", lambda e, j0=j0, j1=j1, g0=g0: e.tensor_scalar(DC[:, :, 0, j0:j1], cols[:, :, g0+j0:g0+j1], -1.0, None, ALU.mult), reads=[cols], writes=[DC])
                kb.op("dve", lambda e, j0=j0, j1=j1: e.tensor_tensor(DC[:, :, 1, j0:j1], DC[:, :, 0, j0:j1], cols[:, :, 12+j0:12+j1], ALU.add), reads=[DC, cols], writes=[DC])
                kb.op("dve", lambda e, j0=j0, j1=j1: e.tensor_tensor(DC[:, :, 1, j0:j1], DC[:, :, 1, j0:j1], DC[:, :, 3, j0:j1], ALU.add), reads=[DC], writes=[DC])
            kb.op("act", lambda e: e.activation(DC[:, :, 1, :], DC[:, :, 1, :], AF.Exp), reads=[DC], writes=[DC])
            kb.op("act", lambda e: e.activation(DC[:, :, 2, :], DC[:, :, 3, :], AF.Exp), reads=[DC], writes=[DC])
            for j in range(12):
                d = 0 if (j < 4 or j in (8, 9)) else 1
                r = (24 if d == 0 else 36) + j
                kb.op("dve", lambda e, j=j, r=r: e.tensor_scalar(sel[:, j, :], onesf[0:48, :], ident[0:48, r:r+1], None, ALU.mult), reads=[onesf, ident], writes=[sel])

        def gla(units, Qf, Kf, Ktok, Vtok, dv, ysum, is_ml):
            pqk = kb.ps([128, 512]); pbc = [kb.ps([128, 512]) for _ in range(2)]
            pys = [kb.ps([128, 512]) for _ in range(2)]
            if is_ml:
                pden = kb.ps([128, 512]); pdss = [kb.ps([128, 512])]; pdn = kb.ps([128, 512])
            else:
                pdss = [kb.ps([128, 512]) for _ in range(2)]
            QKm2 = [kb.sb([128, 128]) for _ in range(2)]; dcl = [kb.sb([128, 128]) for _ in range(2)]; Wd = [kb.sb([128, 128]) for _ in range(2)]
            AT = [kb.sb([128, 128], BF16) for _ in range(2)]; Ebc = [kb.sb([128, 128]) for _ in range(2)]
            Qp = [kb.sb([128, 128], BF16) for _ in range(2)]; Kp = [kb.sb([128, 128], BF16) for _ in range(2)]
            rden = [kb.sb([128, 128]) for _ in range(2)]
            nu = len(units)
            Sst2 = [[kb.sb([128, dv]) for _ in range(nu)] for _ in range(2)]; Sb2 = [[kb.sb([128, dv], BF16) for _ in range(nu)] for _ in range(2)]
            if is_ml:
                nrep2 = [[kb.sb([128, 128]) for _ in range(nu)] for _ in range(2)]; nrb2 = [[kb.sb([128, 128], BF16) for _ in range(nu)] for _ in range(2)]
            it = 0
            touched = set()
            for d in range(2):
                for u in range(nu):
                    kb.op("pool", lambda e, u=u, d=d: e.memset(Sst2[d][u][:], 0.0), writes=[Sst2[d][u]])
                    kb.op("pool", lambda e, u=u, d=d: e.memset(Sb2[d][u][:], 0.0), writes=[Sb2[d][u]])
                    if is_ml:
                        kb.op("pool", lambda e, u=u, d=d: e.memset(nrep2[d][u][:], 0.0), writes=[nrep2[d][u]])
                        kb.op("pool", lambda e, u=u, d=d: e.memset(nrb2[d][u][:], 0.0), writes=[nrb2[d][u]])
            def mk_iter(d, ci, u, unit, b_, do_qk, first):
                jf, jb, qkg, yt, yp0 = unit
                j = jf if d == 0 else jb
                Sst = Sst2[d]; Sb = Sb2[d]; QKm = QKm2[d]
                nrep = nrep2[d] if is_ml else None; nrb = nrb2[d] if is_ml else None
                lat = ci < 32
                c0 = ci * 128
                py = pys[b_]; pds = pdss[b_ % len(pdss)]
                bc = pbc[b_]
                yo = py[yp0:yp0+dv, 0:128]
                ydst = yt[yp0:yp0+dv, c0:c0+128] if lat else None
                def P():
                    if lat:
                        if do_qk:
                            kb.op("pe", lambda e: e.matmul(pqk[:, 0:128], Kf[1](qkg, ci), Qf[1](qkg, ci), start=True, stop=True), reads=[Kf[0], Qf[0]], writes=[pqk])
                            kb.op("dve", lambda e: e.tensor_tensor(QKm[:], pqk[:, 0:128], masks[d][:], ALU.mult), reads=[pqk, masks[d]], writes=[QKm])
                        kb.op("pe", lambda e: e.matmul(bc[:, 0:128], sel[:, j, :], RC[0:48, c0:c0+128], start=True, stop=True), reads=[sel, RC], writes=[bc])
                        kb.op("dve", lambda e: e.tensor_scalar(dcl[b_][:], bc[:, 0:128], DC[:, ci, 0, j:j+1], 0.0, ALU.add, ALU.min), reads=[bc, DC], writes=[dcl[b_]])
                        kb.op("act", lambda e: e.activation(Wd[b_][:], dcl[b_][:], AF.Exp, bias=cols[:, ci, 12+j:13+j]), reads=[dcl[b_], cols], writes=[Wd[b_]])
                        kb.op("dve", lambda e: e.tensor_tensor(AT[b_][:], QKm[:], Wd[b_][:], ALU.mult), reads=[QKm, Wd[b_]], writes=[AT[b_]])
                        kb.op("act", lambda e: e.activation(Ebc[b_][:], bc[:, 0:128], AF.Exp), reads=[bc], writes=[Ebc[b_]])
                        kb.op("pool", lambda e: e.tensor_tensor(Qp[b_][:], Qf[1](qkg, ci), Ebc[b_][:], ALU.mult), reads=[Qf[0], Ebc[b_]], writes=[Qp[b_]])
                    kb.op("pool", lambda e: e.tensor_scalar(Kp[b_][:], Ktok[1](qkg, ci), DC[:, ci, 1, j:j+1], 1.0, ALU.mult, ALU.mult), reads=[Ktok[0], DC], writes=[Kp[b_]])
                def Q():
                    if lat:
                        kb.op("pe", lambda e: e.matmul(yo, Vtok[1](u, ci), AT[b_][:], start=True, stop=False), reads=[Vtok[0], AT[b_]], writes=[py])
                        kb.op("pe", lambda e: e.matmul(yo, Sb[u][:], Qp[b_][:], start=False, stop=True), reads=[Sb[u], Qp[b_]], writes=[py])
                        if is_ml:
                            kb.op("pe", lambda e: e.matmul(pden[:, 0:128], onesb[:], AT[b_][:], start=True, stop=False), reads=[onesb, AT[b_]], writes=[pden])
                            kb.op("pe", lambda e: e.matmul(pden[:, 0:128], nrb[u][:], Qp[b_][:], start=False, stop=True), reads=[nrb[u], Qp[b_]], writes=[pden])
                            kb.op("act", lambda e: e.activation(rden[b_][:], pden[:, 0:128], AF.Abs), reads=[pden], writes=[rden[b_]])
                            kb.op("dve", lambda e: e.tensor_scalar(rden[b_][:], rden[b_][:], 1.0, None, ALU.max), reads=[rden[b_]], writes=[rden[b_]])
                            kb.op("dve", lambda e: e.reciprocal(rden[b_][:], rden[b_][:]), reads=[rden[b_]], writes=[rden[b_]])
                            if first:
                                kb.op("dve", lambda e: e.tensor_tensor(ydst, yo, rden[b_][:], ALU.mult), reads=[py, rden[b_]], writes=[yt])
                            else:
                                kb.op("dve", lambda e: e.tensor_tensor(rden[b_][:], yo, rden[b_][:], ALU.mult), reads=[py, rden[b_]], writes=[rden[b_]])
                                kb.op("pool", lambda e: e.tensor_tensor(ydst, ydst, rden[b_][:], ALU.add), reads=[yt, rden[b_]], writes=[yt])
                        else:
                            if first:
                                kb.op("act", lambda e: e.activation(ydst, yo, AF.Copy), reads=[py], writes=[yt])
                            else:
                                kb.op("dve", lambda e: e.tensor_tensor(ydst, yo, ydst, ALU.add), reads=[py, yt], writes=[yt])
                    kb.op("pe", lambda e: e.matmul(pds[:, 0:dv], Kp[b_][:], Vtok[1](u, ci), start=True, stop=True), reads=[Kp[b_], Vtok[0]], writes=[pds])
                    kb.op("dve", lambda e: e.scalar_tensor_tensor(Sst[u][:], Sst[u][:], DC[:, ci, 2, j:j+1], pds[:, 0:dv], ALU.mult, ALU.add), reads=[Sst[u], DC, pds], writes=[Sst[u]])
                    kb.op("act", lambda e: e.activation(Sb[u][:], Sst[u][:], AF.Copy), reads=[Sst[u]], writes=[Sb[u]])
                    if is_ml:
                        kb.op("pe", lambda e: e.matmul(pdn[:, 0:128], Kp[b_][:], onesb[:], start=True, stop=True), reads=[Kp[b_], onesb], writes=[pdn])
                        kb.op("dve", lambda e: e.scalar_tensor_tensor(nrep[u][:], nrep[u][:], DC[:, ci, 2, j:j+1], pdn[:, 0:128], ALU.mult, ALU.add), reads=[nrep[u], DC, pdn], writes=[nrep[u]])
                        kb.op("act", lambda e: e.activation(nrb[u][:], nrep[u][:], AF.Copy), reads=[nrep[u]], writes=[nrb[u]])
                return P, Q
            its = []
            for step in range(NCH):
                for d in range(2):
                    ci = ORDER[d][step]
                    qk_done = {}
                    for u, unit in enumerate(units):
                        qkg = unit[2]; yt = unit[3]; yp0 = unit[4]
                        b_ = it % 2; it += 1
                        do_qk = (ci < 32) and (qkg not in qk_done)
                        qk_done[qkg] = 1
                        first = False
                        if ci < 32:
                            first = (id(yt), yp0, ci) not in touched; touched.add((id(yt), yp0, ci))
                        its.append(mk_iter(d, ci, u, unit, b_, do_qk, first))
            its[0][0]()
            for k in range(len(its)):
                if k + 1 < len(its):
                    its[k + 1][0]()
                its[k][1]()

        def to_tok(src_bf, dst_fn, ptr, idn=None):
            idn = idn or identb
            for ci in range(NCH):
                p_ = ptr[ci % 2]
                kb.op("pe", lambda e, ci=ci, p_=p_: e.transpose(p_[:, 0:128], src_bf[:, ci*128:(ci+1)*128], idn[:]), reads=[src_bf, idn], writes=[p_])
                dst, buf = dst_fn(ci)
                if ci % 2 == 0:
                    kb.op("act", lambda e, p_=p_, dst=dst: e.activation(dst, p_[:, 0:128], AF.Copy), reads=[p_], writes=[buf])
                else:
                    kb.op("dve", lambda e, p_=p_, dst=dst: e.tensor_copy(dst, p_[:, 0:128]), reads=[p_], writes=[buf])

        def norm_gate_out(ysrc, gate_src_dram, gfunc, gcol, row0, ntile_sum, stg, tmpb):
            pass

        with kb.scope():
            xsc = [kb.sb([128, TOT]) for _ in range(2)]
            Bf = kb.sb([128, TOT], BF16); Cf = kb.sb([128, TOT], BF16)
            Xtok = kb.sb([128, NCH, 256], BF16); Btok = kb.sb([128, NCH, 128], BF16)
            ys = [kb.sb([128, NLAT]) for _ in range(2)]
            with kb.scope():
                stg = kb.sb([128, TOT]); tmp = kb.sb([128, TOT])
                ptr = [kb.ps([128, 128], BF16) for _ in range(2)]
                ptrf = [kb.ps([128, 128], F32) for _ in range(2)]
                for t in range(2):
                    ld(stg, 2 + t)
                    W = lambda i, t=t: cps[:, t, i:i+1]
                    conv_silu_(kb, stg, xsc[t], cps, t, tmp)
                    to_tok(xsc[t], lambda ci, t=t: (Xtok[:, ci, t*128:(t+1)*128], Xtok), ptrf, idn=ident)
                ld(stg, 4)
                conv_silu_(kb, stg, Bf, cps, 2, tmp)
                to_tok(Bf, lambda ci: (Btok[:, ci, :], Btok), ptr)
                ld(stg, 5)
                conv_silu_(kb, stg, Cf, cps, 3, tmp)
            with kb.scope():
                units = [(hl, 4 + hl, 0, ys[hl // 2], (hl % 2) * 64) for hl in range(4)]
                gla(units, (Cf, lambda g, ci: Cf[:, ci*128:(ci+1)*128]), (Bf, lambda g, ci: Bf[:, ci*128:(ci+1)*128]),
                    (Btok, lambda g, ci: Btok[:, ci, :]), (Xtok, lambda u, ci: Xtok[:, ci, u*64:(u+1)*64]), 64, None, False)
            with kb.scope():
                stgm = kb.sb([128, TOT]); tmpm = kb.sb([128, TOT])
                pss = kb.ps([128, 512])
                sqb = [kb.sb([128, 512], BF16) for _ in range(2)]
                rstd = kb.sb([128, 512])
                for t in range(2):
                    ld(stgm, 0 + t)
                    kb.op("act", lambda e: e.activation(tmpm[:, 0:NLAT], stgm[:, 0:NLAT], AF.Silu), reads=[stgm], writes=[tmpm])
                    kb.op("dve", lambda e, t=t: e.scalar_tensor_tensor(ys[t][:], xsc[t][:, 0:NLAT], chs[:, t, 0:1], ys[t][:], ALU.mult, ALU.add), reads=[xsc[t], chs, ys[t]], writes=[ys[t]])
                    kb.op("pool", lambda e, t=t: e.tensor_tensor(ys[t][:], ys[t][:], tmpm[:, 0:NLAT], ALU.mult), reads=[ys[t], tmpm], writes=[ys[t]])
                for c in range(8):
                    a, b = c * 512, c * 512 + 512
                    for t in range(2):
                        kb.op("act", lambda e, t=t, a=a, b=b: e.activation(sqb[t][:], ys[t][:, a:b], AF.Square), reads=[ys[t]], writes=[sqb[t]])
                        kb.op("pe", lambda e, t=t: e.matmul(pss[:], onesb[:], sqb[t][:], start=(t == 0), stop=(t == 1)), reads=[onesb, sqb[t]], writes=[pss])
                    kb.op("act", lambda e: e.activation(rstd[:], pss[:], AF.Sqrt, scale=1.0 / 256, bias=1e-6), reads=[pss], writes=[rstd])
                    kb.op("dve", lambda e: e.reciprocal(rstd[:], rstd[:]), reads=[rstd], writes=[rstd])
                    for t in range(2):
                        kb.op("dve", lambda e, t=t, a=a, b=b: e.scalar_tensor_tensor(ys[t][:, a:b], ys[t][:, a:b], chs[:, t, 1:2], rstd[:], ALU.mult, ALU.mult), reads=[ys[t], chs, rstd], writes=[ys[t]])
                for t in range(2):
                    st(t, ys[t])
        with kb.scope():
            Qf = kb.sb([128, 2, TOT], BF16); Kf = kb.sb([128, 2, TOT], BF16); vb = kb.sb([128, TOT], BF16)
            Ktok = kb.sb([128, NCH, 2, 128], BF16); Vtok = kb.sb([128, NCH, 2, 128], BF16)
            hs = [kb.sb([128, NLAT]) for _ in range(2)]
            with kb.scope():
                stg2 = kb.sb([128, TOT]); tmp2 = kb.sb([128, TOT])
                ptr2 = [kb.ps([128, 128], BF16) for _ in range(2)]
                for t in range(2):
                    ld(stg2, 6 + t)
                    conv_silu_(kb, stg2, kb.view(Qf.t, "Qf"), cps, 4 + t, tmp2, dst_ap=Qf[:, t, :], dst_buf=Qf)
                    ld(stg2, 8 + t)
                    conv_silu_(kb, stg2, None, cps, 6 + t, tmp2, dst_ap=stg2[:], dst_buf=stg2)
                    kb.op("dve", lambda e, t=t: e.tensor_scalar(Kf[:, t, :], stg2[:], float(128 ** -0.5), None, ALU.mult), reads=[stg2], writes=[Kf])
                    kf_t = kb.view(Kf.t, "kf")
                    for ci in range(NCH):
                        p_ = ptr2[ci % 2]
                        kb.op("pe", lambda e, ci=ci, p_=p_, t=t: e.transpose(p_[:, 0:128], Kf[:, t, ci*128:(ci+1)*128], identb[:]), reads=[Kf, identb], writes=[p_])
                        kb.op("act", lambda e, ci=ci, p_=p_, t=t: e.activation(Ktok[:, ci, t, :], p_[:, 0:128], AF.Copy), reads=[p_], writes=[Ktok])
                    ld(stg2, 10 + t)
                    kb.op("pool", lambda e: e.tensor_copy(vb[:], stg2[:]), reads=[stg2], writes=[vb])
                    to_tok(vb, lambda ci, t=t: (Vtok[:, ci, t, :], Vtok), ptr2)
            with kb.scope():
                units2 = [(8 + hl, 10 + hl, hl, hs[hl], 0) for hl in range(2)]
                gla(units2, (Qf, lambda g, ci: Qf[:, g, ci*128:(ci+1)*128]), (Kf, lambda g, ci: Kf[:, g, ci*128:(ci+1)*128]),
                    (Ktok, lambda g, ci: Ktok[:, ci, g, :]), (Vtok, lambda u, ci: Vtok[:, ci, u, :]), 128, None, True)
            with kb.scope():
                stg3 = kb.sb([128, TOT]); tmp3 = kb.sb([128, TOT])
                pss2 = kb.ps([128, 512]); sqb2 = kb.sb([128, 512], BF16); rstd2 = kb.sb([128, 512])
                for t in range(2):
                    ld(stg3, 12 + t)
                    kb.op("act", lambda e: e.activation(tmp3[:, 0:NLAT], stg3[:, 0:NLAT], AF.Sigmoid), reads=[stg3], writes=[tmp3])
                    for c in range(8):
                        a, b = c * 512, c * 512 + 512
                        kb.op("act", lambda e, t=t, a=a, b=b: e.activation(sqb2[:], hs[t][:, a:b], AF.Square), reads=[hs[t]], writes=[sqb2])
                        kb.op("pe", lambda e: e.matmul(pss2[:], onesb[:], sqb2[:], start=True, stop=True), reads=[onesb, sqb2], writes=[pss2])
                        kb.op("act", lambda e: e.activation(rstd2[:], pss2[:], AF.Sqrt, scale=1.0 / 128, bias=1e-6), reads=[pss2], writes=[rstd2])
                        kb.op("dve", lambda e: e.reciprocal(rstd2[:], rstd2[:]), reads=[rstd2], writes=[rstd2])
                        kb.op("dve", lambda e, t=t, a=a, b=b: e.scalar_tensor_tensor(hs[t][:, a:b], hs[t][:, a:b], chs[:, t, 2:3], rstd2[:], ALU.mult, ALU.mult), reads=[hs[t], chs, rstd2], writes=[hs[t]])
                    kb.op("pool", lambda e, t=t: e.tensor_tensor(hs[t][:], hs[t][:], tmp3[:, 0:NLAT], ALU.mult), reads=[hs[t], tmp3], writes=[hs[t]])
                    st(2 + t, hs[t])

def conv_silu_(kb, src, dst, cp, ti, tmp, dst_ap=None, dst_buf=None, func=AF.Silu):
    if dst_ap is None:
        dst_ap = dst[:]; dst_buf = dst
    W = lambda i: cp[:, ti, i:i+1]
    kb.op("dve", lambda e: e.tensor_scalar(tmp[:], src[:], W(2), W(4), ALU.mult, ALU.add), reads=[src, cp], writes=[tmp])
    for (a, b) in ((0, NLAT), (NLAT, TOT)):
        kb.op("dve", lambda e, a=a, b=b: e.scalar_tensor_tensor(tmp[:, a+2:b], src[:, a:b-2], W(0), tmp[:, a+2:b], ALU.mult, ALU.add), reads=[src, tmp, cp], writes=[tmp])
        kb.op("dve", lambda e, a=a, b=b: e.scalar_tensor_tensor(tmp[:, a+1:b], src[:, a:b-1], W(1), tmp[:, a+1:b], ALU.mult, ALU.add), reads=[src, tmp, cp], writes=[tmp])
        kb.op("dve", lambda e, a=a, b=b: e.scalar_tensor_tensor(tmp[:, a:b-1], src[:, a+1:b], W(3), tmp[:, a:b-1], ALU.mult, ALU.add), reads=[src, tmp, cp], writes=[tmp])
    kb.op("act", lambda e: e.activation(dst_ap, tmp[:], func), reads=[tmp], writes=[dst_buf])

def mixo_inputs(uT, hf, P):
    C = np.ascontiguousarray
    c0 = hf * 256
    z = uT[c0:c0+256]; xs = uT[512+c0:512+c0+256]
    Bm = uT[512+512+hf*128:512+512+hf*128+128]; Cm = uT[512+768+hf*128:512+768+hf*128+128]
    dtr = uT[1536:1552]; mq = uT[1552+c0:1552+c0+256]; mk = uT[2064+c0:2064+c0+256]; mv = uT[2576+c0:2576+c0+256]; mo = uT[3088+c0:3088+c0+256]
    mg = uT[3600:3616]
    rows = np.zeros((128, TOT), np.float32); rowp = np.zeros((128, 2), np.float32)
    for d in range(2):
        for hl in range(4):
            h = hf * 4 + hl
            rows[d*4+hl] = dtr[d*8+h]; rowp[d*4+hl, 0] = P['ssd_dt_bias'][d, h]; rowp[d*4+hl, 1] = P['ssd_a_log'][d, h]
        for hl in range(2):
            h = hf * 2 + hl
            rows[32+d*2+hl] = mg[d*8+4+h]; rowp[32+d*2+hl, 0] = P['ml_gate_b'][d, 1, h]
            rows[96+d*2+hl] = mg[d*8+h]; rowp[96+d*2+hl, 0] = P['ml_gate_b'][d, 0, h]
    convp = np.zeros((128, 8, 5), np.float32)
    def cv(w, b, ch0):
        o = np.zeros((128, 5), np.float32); o[:, 0:4] = w[:, ch0:ch0+128].T; o[:, 4] = b[ch0:ch0+128]; return o
    convp[:, 0] = cv(P['ssd_conv_w'], P['ssd_conv_b'], c0); convp[:, 1] = cv(P['ssd_conv_w'], P['ssd_conv_b'], c0+128)
    convp[:, 2] = cv(P['ssd_conv_w'], P['ssd_conv_b'], 512+hf*128); convp[:, 3] = cv(P['ssd_conv_w'], P['ssd_conv_b'], 768+hf*128)
    convp[:, 4] = cv(P['ml_conv_w'], P['ml_conv_b'], c0); convp[:, 5] = cv(P['ml_conv_w'], P['ml_conv_b'], c0+128)
    convp[:, 6] = cv(P['ml_conv_w'], P['ml_conv_b'], 512+c0); convp[:, 7] = cv(P['ml_conv_w'], P['ml_conv_b'], 512+c0+128)
    chp = np.zeros((128, 2, 3), np.float32)
    for t in range(2):
        chp[:, t, 0] = np.repeat(P['ssd_d'][hf*4+t*2:hf*4+t*2+2], 64)
        chp[:, t, 1] = P['ssd_norm_g'][c0+t*128:c0+(t+1)*128]
        chp[:, t, 2] = P['ml_norm_g'][c0+t*128:c0+(t+1)*128]
    return {"zT": C(z), "xsT": C(xs), "BT": C(Bm), "CT": C(Cm), "mqT": C(mq), "mkT": C(mk), "mvT": C(mv), "moT": C(mo),
            "rows": rows, "rowp": rowp, "convp": convp, "chp": chp}


PAIRS = [[0, 1], [2, 3], [4, 5], [6, 7]]

def emit_cc(kb, in_ap, out_ap, reads, writes):
    e = "pool"
    kb._deps(e, reads, writes)
    kb.ccn += 1
    sem = kb.ccsem
    ev = (sem, kb.ccn, "cc")
    kb.prog[e].append(lambda eng, i=in_ap, o=out_ap, sem=sem: eng.collective_compute("AllGather", ALU.bypass, replica_groups=PAIRS, ins=[i], outs=[o]).then_inc(sem))
    kb._commit(ev, reads, writes)

def emit_mod(kb, cT, mod_w, mod_bl, mods):
    with kb.scope():
        cs = kb.sb([128, 8, 2]); sc = kb.sb([128, 8, 2]); bs = kb.sb([128, 2, 48])
        kb.dma("sp", cs[:], cT, writes=[cs]); kb.dma("sp", bs[:], mod_bl, writes=[bs])
        kb.op("act", lambda e: e.activation(sc[:], cs[:], AF.Silu), reads=[cs], writes=[sc])
        ws = [kb.sb([128, 8, 1536]) for _ in range(2)]
        ps = [kb.ps([128, 512]) for _ in range(2)]
        ci = 0
        for l in range(2):
            wv = mod_w[l].rearrange("(kt p) m -> p kt m", p=128)
            for q in range(4):
                w_ = ws[ci % 2]; ci += 1
                for kt in range(8):
                    kb.dma("sp", w_[:, kt, :], wv[:, kt, q*1536:(q+1)*1536], writes=[w_])
                for mm in range(12):
                    m = q * 12 + mm
                    j, kto = divmod(m, 8)
                    p_ = ps[m % 2]
                    for kt in range(8):
                        kb.op("pe", lambda e, mm=mm, kt=kt, p_=p_, w_=w_: e.matmul(p_[:, 0:2], w_[:, kt, mm*128:(mm+1)*128], sc[:, kt, :], start=(kt == 0), stop=(kt == 7)), reads=[w_, sc], writes=[p_])
                    kb.op("dve", lambda e, l=l, m=m, j=j, kto=kto, p_=p_: e.tensor_scalar(mods[l][:, kto, j:12:6], p_[:, 0:2], bs[:, l, m:m+1], None, ALU.add), reads=[p_, bs], writes=[mods[l]])

def emit_pre(kb, xT, xbuf, w, NOUT, g1col, g1buf, mod, U_loc, ubuf, NTOK, NLAT, ones, stages, after_tile=None):
    MT = (NOUT + 127) // 128
    with kb.scope():
        stages = [kb.sb([128, 2048], F32) for _ in range(2)]
        wbf = kb.sb([128, 8, NOUT], BF16)
        A = kb.sb([128, 8, 2], F32); Bc = kb.sb([128, 8, 2], F32)
        for s in range(2):
            kb.op("dve", lambda e, s=s: e.scalar_tensor_tensor(A[:, :, s:s+1], mod[:, :, s*6+1:s*6+2], 1.0, g1col, ALU.add, ALU.mult), reads=[mod, g1buf], writes=[A])
            kb.op("dve", lambda e, s=s: e.tensor_copy(Bc[:, :, s:s+1], mod[:, :, s*6:s*6+1]), reads=[mod], writes=[Bc])
        wv_ = w.rearrange("(kt p) m -> p kt m", p=128)
        wch = [(kt, c0, min(NOUT, c0 + 2048)) for kt in range(8) for c0 in range(0, NOUT, 2048)]
        wp = {"dma": 0, "cast": 0}
        def w_dma():
            i = wp["dma"]
            if i >= len(wch): return
            kt, c0, c1 = wch[i]; st_ = stages[i % 2]
            kb.dma("sp", st_[:, 0:c1 - c0], wv_[:, kt, c0:c1], writes=[st_])
            wp["dma"] += 1
        def w_cast():
            i = wp["cast"]
            if i >= wp["dma"]: return
            kt, c0, c1 = wch[i]; st_ = stages[i % 2]
            eng = "pool" if i % 2 == 0 else "dve"
            kb.op(eng, lambda e, st_=st_, kt=kt, c0=c0, c1=c1: e.tensor_copy(wbf[:, kt, c0:c1], st_[:, 0:c1 - c0]), reads=[st_], writes=[wbf])
            wp["cast"] += 1
        def w_step(n):
            for _ in range(n):
                w_cast(); w_dma()
        xs = [kb.sb([128, 8, 512], F32) for _ in range(2)]
        sq = [kb.sb([128, 8, 512], BF16) for _ in range(2)]
        hall = kb.sb([128, 8, NTOK], BF16)
        tmp = [kb.sb([128, 512], F32) for _ in range(2)]
        rstd = [kb.sb([128, 512], F32) for _ in range(2)]
        ss = kb.ps([128, 512], F32)
        pso = [kb.ps([128, 512], F32) for _ in range(4)]
        ob = [kb.sb([128, 512], F32) for _ in range(4)]
        xv = xT.rearrange("(kt p) n -> p kt n", p=128)
        tiles = [(i * 512, 512, 0) for i in range(NLAT // 512)] + ([(NLAT, NTOK - NLAT, 1)] if NTOK > NLAT else [])
        hv = [kb.view(hall.t, "hall%d" % ti) for ti in range(len(tiles))]
        per_t = -(-len(wch) // len(tiles))
        for ti, (t0, n, s) in enumerate(tiles):
            x_ = xs[ti % 2]; sq_ = sq[ti % 2]; rs_ = rstd[ti % 2]; h_ = hv[ti]
            kb.dma("sp", x_[:, :, 0:n], xv[:, :, t0:t0 + n], reads=[xbuf], writes=[x_])
            if ti == 0:
                w_dma(); w_dma()
            kb.op("act", lambda e, x_=x_, sq_=sq_, n=n: e.activation(sq_[:, :, 0:n], x_[:, :, 0:n], AF.Square), reads=[x_], writes=[sq_])
            for kt in range(8):
                kb.op("pe", lambda e, kt=kt, sq_=sq_, n=n: e.matmul(ss[:, 0:n], ones[:], sq_[:, kt, 0:n], start=(kt == 0), stop=(kt == 7)), reads=[ones, sq_], writes=[ss])
            kb.op("act", lambda e, rs_=rs_, n=n: e.activation(rs_[:, 0:n], ss[:, 0:n], AF.Sqrt, scale=1.0 / 1024, bias=1e-6), reads=[ss], writes=[rs_])
            kb.op("dve", lambda e, rs_=rs_, n=n: e.reciprocal(rs_[:, 0:n], rs_[:, 0:n]), reads=[rs_], writes=[rs_])
            for kt in range(8):
                t_ = tmp[kt % 2]
                kb.op("dve", lambda e, kt=kt, t_=t_, x_=x_, rs_=rs_, n=n, s=s: e.scalar_tensor_tensor(t_[:, 0:n], x_[:, kt, 0:n], A[:, kt, s:s+1], rs_[:, 0:n], ALU.mult, ALU.mult), reads=[x_, A, rs_], writes=[t_])
                kb.op("pool", lambda e, kt=kt, t_=t_, n=n, s=s, t0=t0: e.tensor_scalar(hall[:, kt, t0:t0+n], t_[:, 0:n], Bc[:, kt, s:s+1], 1.0, ALU.add, ALU.mult), reads=[t_, Bc], writes=[h_])
            w_step(per_t)
        w_step(len(wch) + 2)
        oi = 0
        for m in range(MT):
            mw = min(128, NOUT - m * 128)
            for ti, (t0, n, s) in enumerate(tiles):
                p_ = pso[oi % 4]; o_ = ob[oi % 4]; h_ = hv[ti]
                for kt in range(8):
                    kb.op("pe", lambda e, kt=kt, m=m, mw=mw, p_=p_, n=n, t0=t0: e.matmul(p_[0:mw, 0:n], wbf[:, kt, m*128:m*128+mw], hall[:, kt, t0:t0+n], start=(kt == 0), stop=(kt == 7)), reads=[wbf, h_], writes=[p_])
                if oi % 2 == 0:
                    kb.op("act", lambda e, p_=p_, o_=o_, mw=mw, n=n: e.activation(o_[0:mw, 0:n], p_[0:mw, 0:n], AF.Copy), reads=[p_], writes=[o_])
                else:
                    kb.op("dve", lambda e, p_=p_, o_=o_, mw=mw, n=n: e.tensor_copy(o_[0:mw, 0:n], p_[0:mw, 0:n]), reads=[p_], writes=[o_])
                kb.dma("sp", U_loc[m*128:m*128+mw, t0:t0+n], o_[0:mw, 0:n], reads=[o_], writes=[ubuf[m]])
                oi += 1
            if after_tile is not None:
                after_tile(m)

def emit_post(kb, xT, xbuf, mk_ldmix, w_out, g2col, g2buf, mod, wr, w1, w3, w2, xoT, xo_buf, NTOK, NLAT, ones, ident, stages, NEXP=16):
    tiles = [(i * 512, 512, 0) for i in range(NLAT // 512)] + ([(NLAT, NTOK - NLAT, 1)] if NTOK > NLAT else [])
    xv = xT.rearrange("(kt p) n -> p kt n", p=128)
    ov = xoT.rearrange("(kt p) n -> p kt n", p=128)
    with kb.scope():
        stages = [kb.sb([128, 2048], F32) for _ in range(2)]
        A = kb.sb([128, 8, 2], F32); Bc = kb.sb([128, 8, 2], F32)
        G1 = kb.sb([128, 8, 2], F32); G5 = kb.sb([128, 8, 2], F32)
        for s in range(2):
            kb.op("dve", lambda e, s=s: e.scalar_tensor_tensor(A[:, :, s:s+1], mod[:, :, s*6+4:s*6+5], 1.0, g2col, ALU.add, ALU.mult), reads=[mod, g2buf], writes=[A])
            kb.op("dve", lambda e, s=s: e.tensor_copy(Bc[:, :, s:s+1], mod[:, :, s*6+3:s*6+4]), reads=[mod], writes=[Bc])
            kb.op("dve", lambda e, s=s: e.tensor_copy(G1[:, :, s:s+1], mod[:, :, s*6+2:s*6+3]), reads=[mod], writes=[G1])
            kb.op("dve", lambda e, s=s: e.tensor_copy(G5[:, :, s:s+1], mod[:, :, s*6+5:s*6+6]), reads=[mod], writes=[G5])
        h2all = kb.sb([128, 8, NTOK], BF16)
        gateT = kb.sb([16, NTOK], F32)
        wrs = kb.sb([128, 8, 20], F32)
        kb.dma("sp", wrs[:], wr.rearrange("(kt p) m -> p kt m", p=128), writes=[wrs])
        with kb.scope():
            wob = kb.sb([128, 8, 1024], BF16)
            load_cast_weight(kb, w_out, 1024, 1024, wob, stages)
            xs = [kb.sb([128, 8, 512], F32) for _ in range(2)]
            ms = [kb.sb([128, 8, 512], F32) for _ in range(2)]
            mb = kb.sb([128, 8, 512], BF16)
            sq = kb.sb([128, 8, 512], BF16)
            h2f = kb.sb([128, 8, 512], F32)
            tmp = [kb.sb([128, 512], F32) for _ in range(2)]
            rstd = kb.sb([128, 512], F32)
            ss = kb.ps([128, 512], F32)
            psy = [kb.ps([128, 512], F32) for _ in range(2)]
            psl = kb.ps([128, 512], F32)
            pst = kb.ps([128, 512], F32)
            L = kb.sb([128, 4, 20], F32)
            sm = kb.sb([128, 4, 16], F32)
            oh = kb.sb([128, 4, 4], F32); pen = kb.sb([128, 4, 4], F32)
            em = kb.sb([128, 4, 16], F32); em2 = kb.sb([128, 4, 16], F32)
            eq1 = kb.sb([128, 4, 16], F32); eq2 = kb.sb([128, 4, 16], F32); gate = kb.sb([128, 4, 16], F32)
            junk = kb.sb([128, 4, 4], F32)
            def route(t0, n):
                ns = n // 128
                for c in range(ns):
                    c0 = c * 128
                    for kt in range(8):
                        kb.op("pe", lambda e, kt=kt, c0=c0, c=c: e.matmul(psl[:, c*20:(c+1)*20], h2f[:, kt, c0:c0+128], wrs[:, kt, :], start=(kt == 0), stop=(kt == 7)), reads=[h2f, wrs], writes=[psl])
                kb.op("act", lambda e, ns=ns: e.activation(L[:, 0:ns, :], psl[:, 0:ns*20].rearrange("p (s c) -> p s c", c=20), AF.Copy), reads=[psl], writes=[L])
                D = lambda fn, r, w: kb.op("dve", fn, reads=r, writes=w)
                S_ = slice(0, ns)
                Lg = L[:, S_, 0:4]; Le = L[:, S_, 4:20]
                bc = lambda col, w: sm[:, S_, col:col+1].broadcast_to([128, ns, w])
                D(lambda e: e.tensor_reduce(sm[:, S_, 10:11], Lg, AX.X, ALU.max), [L], [sm])
                D(lambda e: e.tensor_tensor(oh[:, S_, :], Lg, bc(10, 4), ALU.is_equal), [L, sm], [oh])
                D(lambda e: e.tensor_tensor(junk[:, S_, :], Lg, bc(10, 4), ALU.subtract), [L, sm], [junk])
                kb.op("act", lambda e: e.activation(junk[:, S_, :], junk[:, S_, :], AF.Exp), reads=[junk], writes=[junk])
                D(lambda e: e.tensor_reduce(sm[:, S_, 1:2], junk[:, S_, :], AX.X, ALU.add), [junk], [sm])
                D(lambda e: e.reciprocal(sm[:, S_, 2:3], sm[:, S_, 1:2]), [sm], [sm])
                D(lambda e: e.tensor_scalar(pen[:, S_, :], oh[:, S_, :], 1e30, -1e30, ALU.mult, ALU.add), [oh], [pen])
                D(lambda e: e.tensor_tensor(em[:, S_, :].rearrange("p s (g e) -> p s g e", e=4), Le.rearrange("p s (g e) -> p s g e", e=4),
                                            pen[:, S_, :].rearrange("p s (g o) -> p s g o", o=1).broadcast_to([128, ns, 4, 4]), ALU.add), [L, pen], [em])
                D(lambda e: e.tensor_reduce(sm[:, S_, 3:4], em[:, S_, :], AX.X, ALU.max), [em], [sm])
                D(lambda e: e.tensor_tensor(eq1[:, S_, :], em[:, S_, :], bc(3, 16), ALU.is_equal), [em, sm], [eq1])
                D(lambda e: e.scalar_tensor_tensor(em2[:, S_, :], eq1[:, S_, :], -1e30, em[:, S_, :], ALU.mult, ALU.add), [eq1, em], [em2])
                D(lambda e: e.tensor_reduce(sm[:, S_, 4:5], em2[:, S_, :], AX.X, ALU.max), [em2], [sm])
                D(lambda e: e.tensor_tensor(eq2[:, S_, :], em2[:, S_, :], bc(4, 16), ALU.is_equal), [em2, sm], [eq2])
                D(lambda e: e.tensor_tensor(sm[:, S_, 5:6], sm[:, S_, 3:4], sm[:, S_, 4:5], ALU.subtract), [sm], [sm])
                kb.op("act", lambda e: e.activation(sm[:, S_, 6:7], sm[:, S_, 5:6], AF.Sigmoid), reads=[sm], writes=[sm])
                D(lambda e: e.tensor_scalar(sm[:, S_, 7:8], sm[:, S_, 6:7], -1.0, 1.0, ALU.mult, ALU.add), [sm], [sm])
                D(lambda e: e.tensor_tensor(sm[:, S_, 8:10], sm[:, S_, 6:8], bc(2, 2), ALU.mult), [sm], [sm])
                D(lambda e: e.tensor_tensor(gate[:, S_, :], eq1[:, S_, :], bc(8, 16), ALU.mult), [eq1, sm], [gate])
                D(lambda e: e.tensor_tensor(eq2[:, S_, :], eq2[:, S_, :], bc(9, 16), ALU.mult), [eq2, sm], [eq2])
                D(lambda e: e.tensor_tensor(gate[:, S_, :], gate[:, S_, :], eq2[:, S_, :], ALU.add), [gate, eq2], [gate])
                for c in range(ns):
                    kb.op("pe", lambda e, c=c: e.transpose(pst[0:16, c*128:(c+1)*128], gate[:, c, :], ident[:]), reads=[gate, ident], writes=[pst])
                kb.op("act", lambda e, t0=t0, n=n: e.activation(gateT[:, t0:t0+n], pst[0:16, 0:n], AF.Copy), reads=[pst], writes=[gateT])

            ldmix = mk_ldmix()
            def load(ti):
                t0, n, s = tiles[ti]
                kb.dma("sp", xs[ti % 2][:, :, 0:n], xv[:, :, t0:t0+n], reads=[xbuf], writes=[xs[ti % 2]])
                ldmix(ms[ti % 2], t0, n)
            mbs = [mb, kb.sb([128, 8, 512], BF16)]
            def partA(ti):
                t0, n, s = tiles[ti]
                x_ = xs[ti % 2]; m_ = ms[ti % 2]; mb_ = mbs[ti % 2]
                kb.op("pool", lambda e, m_=m_, n=n, mb_=mb_: e.tensor_copy(mb_[:, :, 0:n], m_[:, :, 0:n]), reads=[m_], writes=[mb_])
                for m in range(8):
                    p_ = psy[m % 2]
                    for kt in range(8):
                        kb.op("pe", lambda e, kt=kt, m=m, p_=p_, n=n, mb_=mb_: e.matmul(p_[:, 0:n], wob[:, kt, m*128:(m+1)*128], mb_[:, kt, 0:n], start=(kt == 0), stop=(kt == 7)), reads=[wob, mb_], writes=[p_])
                    kb.op("dve", lambda e, m=m, p_=p_, x_=x_, n=n, s=s: e.scalar_tensor_tensor(x_[:, m, 0:n], p_[:, 0:n], G1[:, m, s:s+1], x_[:, m, 0:n], ALU.mult, ALU.add), reads=[p_, G1, x_], writes=[x_])
                kb.dma("sp", ov[:, :, t0:t0+n], x_[:, :, 0:n], reads=[x_], writes=[xo_buf])
            def partB(ti):
                t0, n, s = tiles[ti]
                x_ = xs[ti % 2]
                kb.op("act", lambda e, x_=x_, n=n: e.activation(sq[:, :, 0:n], x_[:, :, 0:n], AF.Square), reads=[x_], writes=[sq])
                for kt in range(8):
                    kb.op("pe", lambda e, kt=kt, n=n: e.matmul(ss[:, 0:n], ones[:], sq[:, kt, 0:n], start=(kt == 0), stop=(kt == 7)), reads=[ones, sq], writes=[ss])
                kb.op("act", lambda e, n=n: e.activation(rstd[:, 0:n], ss[:, 0:n], AF.Sqrt, scale=1.0 / 1024, bias=1e-6), reads=[ss], writes=[rstd])
                kb.op("dve", lambda e, n=n: e.reciprocal(rstd[:, 0:n], rstd[:, 0:n]), reads=[rstd], writes=[rstd])
                for kt in range(8):
                    t_ = tmp[kt % 2]
                    kb.op("dve", lambda e, kt=kt, t_=t_, x_=x_, n=n, s=s: e.scalar_tensor_tensor(t_[:, 0:n], x_[:, kt, 0:n], A[:, kt, s:s+1], rstd[:, 0:n], ALU.mult, ALU.mult), reads=[x_, A, rstd], writes=[t_])
                    kb.op("pool", lambda e, kt=kt, t_=t_, n=n, s=s: e.tensor_scalar(h2f[:, kt, 0:n], t_[:, 0:n], Bc[:, kt, s:s+1], 1.0, ALU.add, ALU.mult), reads=[t_, Bc], writes=[h2f])
                kb.op("act", lambda e, n=n, t0=t0: e.activation(h2all[:, :, t0:t0+n], h2f[:, :, 0:n], AF.Copy), reads=[h2f], writes=[h2all])
                route(t0, n)
            load(0)
            if len(tiles) > 1:
                load(1)
            partA(0)
            for ti in range(len(tiles)):
                if ti + 1 < len(tiles):
                    partA(ti + 1)
                partB(ti)
                if ti + 2 < len(tiles):
                    load(ti + 2)
        with kb.scope():
            yacc = kb.sb([128, 8, NTOK], F32)
            es2 = kb.scope(); es2.__enter__()
            sel = kb.sb([16, NEXP, 128], F32)
            kb.op("pool", lambda e: e.memset(sel[:], 1.0), writes=[sel])
            kb.op("pool", lambda e: e.affine_select(sel[:], sel[:], [[-1, NEXP], [0, 128]], ALU.is_equal, 0.0, base=0, channel_multiplier=1), reads=[sel], writes=[sel])
            wb = [dict(w1=kb.sb([128, 8, 512], BF16), w3=kb.sb([128, 8, 512], BF16), w2=kb.sb([128, 4, 1024], BF16)) for _ in range(2)]
            gb_ps = kb.ps([128, 512], F32)
            gb = kb.sb([128, 512], F32)
            pa = [kb.ps([128, 512], F32) for _ in range(2)]
            pb = [kb.ps([128, 512], F32) for _ in range(2)]
            po = [kb.ps([128, 512], F32) for _ in range(2)]
            sa = [kb.sb([128, 512], F32) for _ in range(2)]
            tt = [kb.sb([128, 512], F32) for _ in range(2)]
            hid = [kb.sb([128, 4, 512], BF16) for _ in range(2)]
            st4 = [kb.view(stages[i // 2].t[:, (i % 2) * 1024:(i % 2 + 1) * 1024], "st4_%d" % i) for i in range(4)]
            def chunks(e_):
                d_ = wb[e_ % 2]
                out = []
                for nm, src, K, M in (("w1", w1[e_], 1024, 512), ("w3", w3[e_], 1024, 512), ("w2", w2[e_], 512, 1024)):
                    wv = src.rearrange("(kt p) m -> p kt m", p=128)
                    for kt in range(K // 128):
                        out.append((wv[:, kt, :], d_[nm], kt, M))
                return out
            allch = [c for e_ in range(NEXP) for c in chunks(e_)]
            NCHK = 20
            ptr_ = {"dma": 0, "cast": 0}
            def emit_dma():
                i = ptr_["dma"]
                if i >= len(allch): return
                src, dbuf, kt, M = allch[i]
                st = st4[i % 4]
                kb.dma("sp", st[:, 0:M], src, writes=[st])
                ptr_["dma"] += 1
            def emit_cast():
                i = ptr_["cast"]
                if i >= ptr_["dma"]: return
                src, dbuf, kt, M = allch[i]
                st = st4[i % 4]
                kb.op("pool", lambda e, st=st, dbuf=dbuf, kt=kt, M=M: e.tensor_copy(dbuf[:, kt, 0:M], st[:, 0:M]), reads=[st], writes=[dbuf])
                ptr_["cast"] += 1
            def advance(upto):
                while ptr_["dma"] < min(upto, len(allch)):
                    emit_dma()
                    if ptr_["dma"] - ptr_["cast"] > 2:
                        emit_cast()
            def flush(upto):
                while ptr_["cast"] < min(upto, ptr_["dma"]):
                    emit_cast()
            gbs = [gb, kb.sb([128, 512], F32)]
            items = [(e_, ti) for e_ in range(NEXP) for ti in range(len(tiles))]
            def stageA(k):
                e_, ti = items[k]
                t0, n, s = tiles[ti]
                d_ = wb[e_ % 2]; h_ = hid[k % 2]; g_ = gbs[k % 2]
                kb.op("pe", lambda e, e_=e_, t0=t0, n=n: e.matmul(gb_ps[:, 0:n], sel[:, e_, :], gateT[:, t0:t0+n], start=True, stop=True), reads=[sel, gateT], writes=[gb_ps])
                kb.op("act", lambda e, n=n, g_=g_: e.activation(g_[:, 0:n], gb_ps[:, 0:n], AF.Copy), reads=[gb_ps], writes=[g_])
                for f in range(4):
                    a_ = pa[f % 2]; b_ = pb[f % 2]; sa_ = sa[f % 2]; t_ = tt[f % 2]
                    for kt in range(8):
                        kb.op("pe", lambda e, kt=kt, f=f, a_=a_, d_=d_, t0=t0, n=n: e.matmul(a_[:, 0:n], d_["w1"][:, kt, f*128:(f+1)*128], h2all[:, kt, t0:t0+n], start=(kt == 0), stop=(kt == 7)), reads=[d_["w1"], h2all], writes=[a_])
                    for kt in range(8):
                        kb.op("pe", lambda e, kt=kt, f=f, b_=b_, d_=d_, t0=t0, n=n: e.matmul(b_[:, 0:n], d_["w3"][:, kt, f*128:(f+1)*128], h2all[:, kt, t0:t0+n], start=(kt == 0), stop=(kt == 7)), reads=[d_["w3"], h2all], writes=[b_])
                    kb.op("act", lambda e, a_=a_, sa_=sa_, n=n: e.activation(sa_[:, 0:n], a_[:, 0:n], AF.Silu), reads=[a_], writes=[sa_])
                    kb.op("dve", lambda e, b_=b_, sa_=sa_, t_=t_, n=n: e.tensor_tensor(t_[:, 0:n], b_[:, 0:n], sa_[:, 0:n], ALU.mult), reads=[b_, sa_], writes=[t_])
                    kb.op("pool", lambda e, t_=t_, h_=h_, f=f, n=n, g_=g_: e.tensor_tensor(h_[:, f, 0:n], t_[:, 0:n], g_[:, 0:n], ALU.mult), reads=[t_, g_], writes=[h_])
            def stageB(k):
                e_, ti = items[k]
                t0, n, s = tiles[ti]
                d_ = wb[e_ % 2]; h_ = hid[k % 2]
                for m in range(8):
                    o_ = po[m % 2]
                    for f in range(4):
                        kb.op("pe", lambda e, f=f, m=m, o_=o_, h_=h_, d_=d_, n=n: e.matmul(o_[:, 0:n], d_["w2"][:, f, m*128:(m+1)*128], h_[:, f, 0:n], start=(f == 0), stop=(f == 3)), reads=[d_["w2"], h_], writes=[o_])
                    if e_ == 0:
                        kb.op("dve", lambda e, m=m, o_=o_, t0=t0, n=n: e.tensor_copy(yacc[:, m, t0:t0+n], o_[:, 0:n]), reads=[o_], writes=[yacc])
                    else:
                        kb.op("dve", lambda e, m=m, o_=o_, t0=t0, n=n: e.tensor_tensor(yacc[:, m, t0:t0+n], o_[:, 0:n], yacc[:, m, t0:t0+n], ALU.add), reads=[o_, yacc], writes=[yacc])
            advance(2 * NCHK); flush(2 * NCHK)
            nt_ = len(tiles)
            per_it = -(-NCHK // max(1, nt_ - 1))
            stageA(0)
            for k in range(len(items)):
                if k + 1 < len(items):
                    e_n, ti_n = items[k + 1]
                    if ti_n == 0:
                        flush((e_n + 1) * NCHK)
                    stageA(k + 1)
                stageB(k)
                e_k, ti_k = items[k]
                if e_k >= 1 and e_k + 1 < NEXP and ti_k < nt_ - 1:
                    advance((e_k + 1) * NCHK + min(NCHK, (ti_k + 1) * per_it))
            es2.__exit__(None, None, None)
            xm = [kb.sb([128, 8, 512], F32) for _ in range(2)]
            for ti, (t0, n, s) in enumerate(tiles):
                x_ = xm[ti % 2]
                kb.dma("sp", x_[:, :, 0:n], ov[:, :, t0:t0+n], reads=[xo_buf], writes=[x_])
                for m in range(8):
                    kb.op("dve", lambda e, m=m, x_=x_, t0=t0, n=n, s=s: e.scalar_tensor_tensor(x_[:, m, 0:n], yacc[:, m, t0:t0+n], G5[:, m, s:s+1], x_[:, m, 0:n], ALU.mult, ALU.add), reads=[yacc, G5, x_], writes=[x_])
                kb.dma("sp", ov[:, :, t0:t0+n], x_[:, :, 0:n], reads=[x_], writes=[xo_buf])

TOT = 4352
def build_fused():
    nc = bass.Bass("TRN2", target_bir_lowering=False)
    I = lambda n, s: nc.dram_tensor(n, s, F32, kind="ExternalInput").ap()
    N = lambda n, s: nc.dram_tensor(n, s, F32, kind="Internal").ap()
    xT0 = I("xT0", [1024, 2176]); cT = I("cT", [128, 8, 2]); mod_w = I("mod_w", [2, 1024, 6144]); mod_bl = I("mod_bl", [128, 2, 48])
    g12 = I("g12", [128, 4, 8])
    rsel_d = I("rsel", [128, 2])
    w_in = [I("w_in0", [1024, 2560]), I("w_in1", [1024, 3616])]
    w_out = [I("w_out0", [1024, 1024]), I("w_out1", [1024, 1024])]
    cw = I("cw", [128, 2, 5]); wbd = I("wbd", [2, 2, 2, 128, 128]); lp = I("lp", [128, 2, 2, 3]); qkg = I("qkg", [128, 2]); bias = I("bias", [2, 128, 8, 512])
    rowp = I("rowp", [128, 2]); convp = I("convp", [128, 8, 5]); chp = I("chp", [128, 2, 3])
    wr = I("wr", [2, 1024, 20]); w1 = I("w1", [2, 16, 1024, 512]); w3 = I("w3", [2, 16, 1024, 512]); w2 = I("w2", [2, 16, 512, 1024])
    xoT = nc.dram_tensor("xoT", [1024, 2048], F32, kind="ExternalOutput").ap()
    NOUT = [2560, 3616]; T = [10, 14]; NT = [2176, 2048]
    U_loc = [N("U_loc%d" % l, [((NOUT[l] + 127) // 128) * 128, 2176]) for l in range(2)]
    U_g = [[N("U_g%d_%d" % (l, i), [256, 2176]) for i in range(T[l])] for l in range(2)]
    G_g = N("G_g", [64, 2176])
    M_send = [[N("M_send%d_%d" % (l, t), [128, NT[l]]) for t in range(4)] for l in range(2)]
    M_loc = [[N("M_loc%d_%d" % (l, t), [128, NT[l]]) for t in range(4)] for l in range(2)]
    M_g = [[N("M_g%d_%d" % (l, t), [256, NT[l]]) for t in range(4)] for l in range(2)]
    X1 = N("X1", [1024, 2176])
    with ExitStack() as es:
        kb = KB(nc, es)
        kb.ccsem = es.enter_context(nc.semaphore("ccsem")); kb.ccn = 0
        V = kb.view
        b_x0 = V(xT0); b_X1 = V(X1); b_xo = V(xoT)
        b_Ul = [V(U_loc[l]) for l in range(2)]; b_Ug = [[V(a) for a in U_g[l]] for l in range(2)]; b_Gg = V(G_g)
        b_Ms = [[V(a) for a in M_send[l]] for l in range(2)]; b_Ml = [[V(a) for a in M_loc[l]] for l in range(2)]; b_Mg = [[V(a) for a in M_g[l]] for l in range(2)]
        ones = kb.sb([128, 128], BF16)
        kb.op("pool", lambda e: e.memset(ones[:], 1.0), writes=[ones])
        ident = kb.ident(F32)
        gs = kb.sb([128, 4, 8]); rsel = kb.sb([128, 2])
        kb.dma("sp", gs[:], g12, writes=[gs]); kb.dma("sp", rsel[:], rsel_d, writes=[rsel])
        R0 = rsel[:, 0:1]; R1 = rsel[:, 1:2]
        mods = [kb.sb([128, 8, 12]) for _ in range(2)]
        stages = None
        emit_mod(kb, cT, mod_w, mod_bl, mods)

        def blend(dst_ap, a_ap, b_ap, ca, cb, t_ap, rd, wr_):
            kb.op("pool", lambda e: e.tensor_scalar(t_ap, a_ap, ca, 1.0, ALU.mult, ALU.mult), reads=rd + [rsel], writes=[wr_[1]])
            kb.op("dve", lambda e: e.scalar_tensor_tensor(dst_ap, b_ap, cb, t_ap, ALU.mult, ALU.add), reads=rd + [rsel, wr_[1]], writes=[wr_[0]])

        for l in range(2):
            xin, xbuf = (xT0, b_x0) if l == 0 else (X1, b_X1)
            xout, xobuf = (X1, b_X1) if l == 0 else (xoT, b_xo)
            NTOKp = 2176
            MTl = (NOUT[l] + 127) // 128
            ub_t = [V(U_loc[l], "ul%d_%d" % (l, m)) for m in range(MTl)]
            def after_tile(m, l=l, ub_t=ub_t):
                if m < T[l]:
                    emit_cc(kb, U_loc[l][m*128:(m+1)*128, :], U_g[l][m], [ub_t[m]], [b_Ug[l][m]])
                elif l == 1 and m == 28:
                    emit_cc(kb, U_loc[1][28*128:28*128+32, :], G_g, [ub_t[m]], [b_Gg])
            emit_pre(kb, xin, xbuf, w_in[l], NOUT[l], gs[:, l, :].rearrange("p (k o) -> p k o", o=1), gs, mods[l], U_loc[l], ub_t, NTOKp, 2048, ones, stages, after_tile)
            if l == 1:
                b_Gg.last_w = (kb.ccsem, kb.ccn, "cc")
            for i in range(T[l]):
                b_Ug[l][i].last_w = (kb.ccsem, kb.ccn, "cc")
            with kb.scope():
                CHK = [(0, 512), (512, 512), (1024, 512), (1536, 512), (2048, 128)]
                sets = [tuple(kb.sb([128, 512]) for _ in range(5)) for _ in range(2)]
                cnt = {"i": 0}
                def nxt():
                    cnt["i"] += 1
                    return sets[cnt["i"] % 2]
                def dcols(h, c0, cn):
                    return (h * 2048 + c0, cn) if c0 < 2048 else (4096 + h * 128, cn)
                def ld(dst, i, l=l):
                    for (c0, cn) in CHK:
                        sl, s0, s1, ta, tb = nxt()
                        kb.dma("sp", sl[:, 0:cn], U_loc[l][(T[l]+i)*128:(T[l]+i+1)*128, c0:c0+cn], reads=[b_Ul[l]], writes=[sl])
                        kb.dma("sp", s0[:, 0:cn], U_g[l][i][0:128, c0:c0+cn], reads=[b_Ug[l][i]], writes=[s0])
                        kb.dma("sp", s1[:, 0:cn], U_g[l][i][128:256, c0:c0+cn], reads=[b_Ug[l][i]], writes=[s1])
                        d0, _ = dcols(0, c0, cn); d1, _ = dcols(1, c0, cn)
                        blend(dst[:, d0:d0+cn], sl[:, 0:cn], s0[:, 0:cn], R0, R1, ta[:, 0:cn], [sl, s0], (dst, ta))
                        blend(dst[:, d1:d1+cn], sl[:, 0:cn], s1[:, 0:cn], R1, R0, tb[:, 0:cn], [sl, s1], (dst, tb))
                def st(t, src, l=l):
                    pieces = [(0, 512), (512, 512), (1024, 512), (1536, 512)] + ([(2048, 128)] if l == 0 else [])
                    for (c0, cn) in pieces:
                        sl, s0, s1, ta, tb = nxt()
                        if c0 < 2048:
                            a0 = src[:, c0:c0+cn]; a1 = src[:, 2048+c0:2048+c0+cn]
                        else:
                            a0 = src[:, 4096:4224]; a1 = src[:, 4224:4352]
                        blend(s0[:, 0:cn], a0, a1, R0, R1, ta[:, 0:cn], [src], (s0, ta))
                        kb.dma("sp", M_loc[l][t][:, c0:c0+cn], s0[:, 0:cn], reads=[s0], writes=[b_Ml[l][t]])
                        blend(s1[:, 0:cn], a0, a1, R1, R0, tb[:, 0:cn], [src], (s1, tb))
                        kb.dma("sp", M_send[l][t][:, c0:c0+cn], s1[:, 0:cn], reads=[s1], writes=[b_Ms[l][t]])
                    emit_cc(kb, M_send[l][t], M_g[l][t], [b_Ms[l][t]], [b_Mg[l][t]])
                if l == 0:
                    emit_mixe(kb, ld, cw, wbd, lp, qkg, bias, st)
                else:
                    def ld_rows(dst):
                        kb.op("pool", lambda e: e.memset(dst[:], 0.0), writes=[dst])
                        for (c0, cn) in CHK:
                            for h in range(2):
                                sA, sB, _s1, ta, _tb = nxt()
                                kb.op("pool", lambda e, sA=sA: e.memset(sA[:], 0.0), writes=[sA])
                                kb.op("pool", lambda e, sB=sB: e.memset(sB[:], 0.0), writes=[sB])
                                a_r0 = 0 if h == 0 else 16
                                b_r0 = 16 if h == 0 else 0
                                for (pb, ro, nr) in ((0, 0, 8), (32, 8, 4), (96, 12, 4)):
                                    kb.dma("sp", sA[pb:pb+nr, 0:cn], G_g[h*32+a_r0+ro:h*32+a_r0+ro+nr, c0:c0+cn], reads=[b_Gg], writes=[sA])
                                    kb.dma("sp", sB[pb:pb+nr, 0:cn], G_g[h*32+b_r0+ro:h*32+b_r0+ro+nr, c0:c0+cn], reads=[b_Gg], writes=[sB])
                                d0, _ = dcols(h, c0, cn)
                                blend(dst[:, d0:d0+cn], sA[:, 0:cn], sB[:, 0:cn], R0, R1, ta[:, 0:cn], [sA, sB], (dst, ta))
                    emit_mixo(kb, ld, ld_rows, rowp, convp, chp, st)
            NTOK = NT[l]
            if True:
              def mk_ldmix(l=l):
                sb0 = kb.sb([128, 4, 512]); sb1 = kb.sb([128, 4, 512])
                def ldmix(dst, t0, n, l=l):
                    for kt in range(4):
                        kb.dma("sp", dst[:, kt, 0:n], M_loc[l][kt][:, t0:t0+n], reads=[b_Ml[l][kt]], writes=[dst])
                        kb.dma("sp", sb0[:, kt, 0:n], M_g[l][kt][0:128, t0:t0+n], reads=[b_Mg[l][kt]], writes=[sb0])
                        kb.dma("sp", sb1[:, kt, 0:n], M_g[l][kt][128:256, t0:t0+n], reads=[b_Mg[l][kt]], writes=[sb1])
                    blend(dst[:, 4:8, 0:n], sb0[:, :, 0:n], sb1[:, :, 0:n], R1, R0, sb0[:, :, 0:n], [sb0, sb1], (dst, sb0))
                return ldmix
              emit_post(kb, xin, xbuf, mk_ldmix, w_out[l], gs[:, 2 + l, :].rearrange("p (k o) -> p k o", o=1), gs, mods[l], wr[l], w1[l], w3[l], w2[l],
                        xout if l == 1 else X1, xobuf, NTOK, 2048, ones, ident, stages)
        kb._wait("sp", (kb.ccsem, kb.ccn, "cc"))
        kb.finish()
    return nc


def needs_even(hf):
    c0 = hf * 256
    return np.concatenate([np.arange(b + c0, b + c0 + 256) for b in (0, 512, 1024, 1536, 2048)])

def needs_odd(hf):
    c0 = hf * 256
    return np.concatenate([np.arange(c0, c0 + 256), np.arange(512 + c0, 512 + c0 + 256), np.arange(1024 + hf*128, 1024 + hf*128 + 128),
                           np.arange(1280 + hf*128, 1280 + hf*128 + 128), np.arange(1552 + c0, 1552 + c0 + 256), np.arange(2064 + c0, 2064 + c0 + 256),
                           np.arange(2576 + c0, 2576 + c0 + 256), np.arange(3088 + c0, 3088 + c0 + 256)])

def gates_odd(hf):
    dt = [1536 + d*8 + hf*4 + hl for d in range(2) for hl in range(4)]
    fg = [3600 + d*8 + 4 + hf*2 + hl for d in range(2) for hl in range(2)]
    ig = [3600 + d*8 + hf*2 + hl for d in range(2) for hl in range(2)]
    return np.array(dt + fg + ig)

def mixset(hf):
    return np.concatenate([np.arange(hf*256, hf*256 + 256), np.arange(512 + hf*256, 512 + hf*256 + 256)])

def fused_inputs(b, hf, a):
    C = np.ascontiguousarray
    f = lambda k: np.asarray(a[k], dtype=np.float32)
    x = f('x'); ctx = f('ctx')
    d = {}
    d["xT0"] = C(np.concatenate([x[b, hf*2048:(hf+1)*2048], ctx[b, hf*128:(hf+1)*128]], 0).T)
    cc = np.stack([f('c')[b], f('c_ctx')], 0)
    d["cT"] = C(cc.T.reshape(8, 128, 2).transpose(1, 0, 2))
    d["mod_w"] = f('mod_w')
    d["mod_bl"] = C(f('mod_b').reshape(2, 48, 128).transpose(2, 0, 1))
    d["g12"] = C(np.stack([f('norm1_g')[0], f('norm1_g')[1], f('norm2_g')[0], f('norm2_g')[1]], 0).reshape(4, 8, 128).transpose(2, 0, 1))
    d["rsel"] = np.tile(np.array([[1.0 - hf, float(hf)]], np.float32), (128, 1))
    pe = np.concatenate([needs_even(1 - hf), needs_even(hf)])
    po = np.concatenate([needs_odd(1 - hf), needs_odd(hf), gates_odd(hf), gates_odd(1 - hf)])
    d["w_in0"] = C(f('even_w_in')[0][:, pe]); d["w_in1"] = C(f('odd_w_in')[0][:, po])
    pm = np.concatenate([mixset(hf), mixset(1 - hf)])
    d["w_out0"] = C(f('even_w_out')[0][pm]); d["w_out1"] = C(f('odd_w_out')[0][pm])
    Pe = {k: f(k)[0] for k in ['lru_conv_w','lru_conv_b','lru_wa','lru_ba','lru_wx','lru_bx','lru_lam','na_q_g','na_k_g','na_rpb']}
    Po = {k: f(k)[0] for k in ['ssd_conv_w','ssd_conv_b','ssd_dt_bias','ssd_a_log','ssd_d','ssd_norm_g','ml_conv_w','ml_conv_b','ml_gate_b','ml_norm_g']}
    me = mixe_inputs(np.zeros((2560, 1), np.float32), hf, Pe)
    for k in ("cw", "wbd", "lp", "qkg", "bias"):
        d[k] = me[k]
    mo = mixo_inputs(np.zeros((3616, TOT), np.float32), hf, Po)
    for k in ("rowp", "convp", "chp"):
        d[k] = mo[k]
    d["wr"] = C(np.concatenate([f('moe_router_g'), f('moe_router_e')], 2))
    d["w1"] = f('moe_w1'); d["w3"] = f('moe_w3'); d["w2"] = f('moe_w2')
    return d

_NC = {}
def kernel(**a):
    if "nc" not in _NC:
        _NC["nc"] = build_fused()
    cores = [(b, hf) for b in range(4) for hf in range(2)]
    maps = [fused_inputs(b, hf, a) for (b, hf) in cores]
    res = run_bass_kernel_spmd(_NC["nc"], maps, core_ids=list(range(8))).results
    out = np.zeros((4, 4096, 1024), np.float32)
    for i, (b, hf) in enumerate(cores):
        out[b, hf*2048:(hf+1)*2048] = res[i]["xoT"].T
    return out
```

```python
import numpy as np
import concourse.bass as bass
import concourse.mybir as mybir
from concourse.bass_utils import run_bass_kernel_spmd
from contextlib import ExitStack

F32 = mybir.dt.float32
BF16 = mybir.dt.bfloat16
I32 = mybir.dt.int32
AF = mybir.ActivationFunctionType
ALU = mybir.AluOpType
AX = mybir.AxisListType
NDS = 40
SAME_ENGINE_WAIT = True


class Buf:
    def __init__(self, t, name=""):
        self.t = t
        self.name = name
        self.last_w = None
        self.reads = {}

    def __getitem__(self, k):
        return self.t[k]


class KB:
    ENG = ["pe", "dve", "act", "pool", "sp"]

    def __init__(self, nc, es):
        self.nc = nc
        self.es = es
        self.esem = {e: es.enter_context(nc.semaphore("s_" + e)) for e in self.ENG}
        self.ecnt = {e: 0 for e in self.ENG}
        self.dsem = [es.enter_context(nc.semaphore("d%d" % i)) for i in range(NDS)]
        self.dval = [0] * NDS
        self.dnext = 0
        self.seen = {e: {} for e in self.ENG}
        self.prog = {e: [] for e in self.ENG}
        self.nbuf = 0

    def sb(self, shape, dtype=F32, name=None):
        self.nbuf += 1
        name = name or "t%d" % self.nbuf
        t = self.es.enter_context(self.nc.sbuf_tensor(name, list(shape), dtype))
        return Buf(t, name)

    def ps(self, shape, dtype=F32, name=None):
        self.nbuf += 1
        name = name or "p%d" % self.nbuf
        t = self.es.enter_context(self.nc.psum_tensor(name, list(shape), dtype))
        return Buf(t, name)

    def view(self, t, name=""):
        return Buf(t, name)

    def _wait(self, e, ev):
        if ev is None:
            return
        sem, val, key = ev
        if key == e and (e == "pe" or not SAME_ENGINE_WAIT):
            return
        if self.seen[e].get(key, 0) >= val:
            return
        self.seen[e][key] = val
        self.prog[e].append(lambda eng, sem=sem, val=val: eng.wait_ge(sem, val))

    def _deps(self, e, reads, writes):
        for b in reads:
            self._wait(e, b.last_w)
        for b in writes:
            self._wait(e, b.last_w)
            for r in b.reads.values():
                self._wait(e, r)

    def _commit(self, ev, reads, writes):
        for b in reads:
            b.reads[ev[2]] = ev
        for b in writes:
            b.last_w = ev
            b.reads = {}

    def op(self, e, fn, reads=(), writes=()):
        self._deps(e, reads, writes)
        self.ecnt[e] += 1
        sem = self.esem[e]
        ev = (sem, self.ecnt[e], e)
        self.prog[e].append(lambda eng, fn=fn, sem=sem: fn(eng).then_inc(sem, 1))
        self._commit(ev, reads, writes)

    def dma(self, q, out_ap, in_ap, reads=(), writes=(), **kw):
        self._deps(q, reads, writes)
        i = self.dnext
        self.dnext = (i + 1) % NDS
        if self.dval[i] > 0:
            self._wait(q, (self.dsem[i], self.dval[i], ("d", i)))
        self.dval[i] += 16
        sem = self.dsem[i]
        ev = (sem, self.dval[i], ("d", i))
        self.prog[q].append(lambda eng, o=out_ap, a=in_ap, sem=sem, kw=kw: eng.dma_start(out=o, in_=a, **kw).then_inc(sem, 16))
        self._commit(ev, reads, writes)

    def barrier(self):
        for e in self.ENG:
            for e2 in self.ENG:
                if self.ecnt[e2] > 0 and not (e == "pe" and e2 == "pe"):
                    self._wait(e, (self.esem[e2], self.ecnt[e2], e2))
            for i in range(NDS):
                if self.dval[i] > 0:
                    self._wait(e, (self.dsem[i], self.dval[i], ("d", i)))
            if getattr(self, "ccn", 0) > 0:
                self._wait(e, (self.ccsem, self.ccn, "cc"))

    def scope(self):
        kb = self
        class _S:
            def __enter__(s_):
                s_.old = kb.es
                s_.st = ExitStack()
                s_.st.__enter__()
                kb.es = s_.st
                return s_
            def __exit__(s_, *a):
                kb.barrier()
                kb.es = s_.old
                return s_.st.__exit__(*a)
        return _S()

    def ident(self, dtype=F32):
        t = self.sb([128, 128], dtype)
        self.op("pool", lambda e: e.memset(t[:], 1.0), writes=[t])
        self.op("pool", lambda e: e.affine_select(t[:], t[:], [[-1, 128]], ALU.is_equal, 0.0, base=0, channel_multiplier=1), reads=[t], writes=[t])
        return t

    def finish(self):
        for i in range(NDS):
            if self.dval[i] > 0:
                self._wait("sp", (self.dsem[i], self.dval[i], ("d", i)))
        nc = self.nc
        with nc.Block() as block:
            def mk(e):
                def body(eng):
                    for f in self.prog[e]:
                        f(eng)
                return body
            block.tensor(mk("pe"))
            block.vector(mk("dve"))
            block.scalar(mk("act"))
            block.gpsimd(mk("pool"))
            block.sync(mk("sp"))


def load_cast_weight(kb, w_dram, K, M, wbf, stage_bufs, q="sp", cast_engs=("pool","dve")):
    KT = K // 128
    wv = w_dram.rearrange("(kt p) m -> p kt m", p=128)
    CH = stage_bufs[0].t.shape[-1]
    i = 0
    for kt in range(KT):
        for c0 in range(0, M, CH):
            c1 = min(M, c0 + CH)
            st = stage_bufs[i % len(stage_bufs)]
            kb.dma(q, st[:, 0:c1 - c0], wv[:, kt, c0:c1], writes=[st])
            ce = cast_engs[i % len(cast_engs)]
            kb.op(ce, lambda e, st=st, kt=kt, c0=c0, c1=c1: e.tensor_copy(wbf[:, kt, c0:c1], st[:, 0:c1 - c0]), reads=[st], writes=[wbf])
            i += 1


def lay8(v):
    return np.ascontiguousarray(v.reshape(8, 128).T)


TOT = 4352; NLAT = 4096; NCTX = 256
CH = [(i * 512, 512) for i in range(8)] + [(4096, 256)]

def emit_mixe(kb, ld, cw, wbd, lp, qkg, bias, st):
    if True:
        ident = kb.ident(F32)
        identb = kb.sb([128, 128], BF16)
        kb.op("dve", lambda e: e.tensor_copy(identb[:], ident[:]), reads=[ident], writes=[identb])
        cws = kb.sb([128, 2, 5]); lps = kb.sb([128, 2, 2, 3]); qkgs = kb.sb([128, 2])
        kb.dma("sp", cws[:], cw, writes=[cws]); kb.dma("sp", lps[:], lp, writes=[lps]); kb.dma("sp", qkgs[:], qkg, writes=[qkgs])
        with kb.scope():
            sp_ = kb.sb([128, 2, 2, 1])
            kb.op("act", lambda e: e.activation(sp_[:, :, :, 0], lps[:, :, :, 2], AF.Exp, scale=-1.0), reads=[lps], writes=[sp_])
            kb.op("act", lambda e: e.activation(sp_[:, :, :, 0], sp_[:, :, :, 0], AF.Ln, bias=1.0), reads=[sp_], writes=[sp_])
            kb.op("dve", lambda e: e.tensor_scalar(sp_[:, :, :, 0], sp_[:, :, :, 0], -8.0, None, ALU.mult), reads=[sp_], writes=[sp_])
            x = kb.sb([128, TOT]); xc = kb.sb([128, TOT]); ug = kb.sb([128, TOT])
            ra = [kb.sb([128, TOT]) for _ in range(2)]; iu = [kb.sb([128, TOT]) for _ in range(2)]
            s_ = kb.sb([128, TOT])
            wts = kb.sb([128, 2, 2, 128])
            pg = [kb.ps([128, 512]) for _ in range(4)]
            for j in range(2):
                ld(x, 0 + j)
                ld(ug, 2 + j)
                kb.dma("sp", wts[:], wbd[j].rearrange("d g k m -> k d g m"), writes=[wts])
                W = lambda i, j=j: cws[:, j, i:i+1]
                kb.op("dve", lambda e, W=W: e.tensor_scalar(xc[:], x[:], W(2), W(4), ALU.mult, ALU.add), reads=[x, cws], writes=[xc])
                for (a, b) in ((0, NLAT), (NLAT, TOT)):
                    kb.op("dve", lambda e, a=a, b=b, W=W: e.scalar_tensor_tensor(xc[:, a+2:b], x[:, a:b-2], W(0), xc[:, a+2:b], ALU.mult, ALU.add), reads=[x, xc, cws], writes=[xc])
                    kb.op("dve", lambda e, a=a, b=b, W=W: e.scalar_tensor_tensor(xc[:, a+1:b], x[:, a:b-1], W(1), xc[:, a+1:b], ALU.mult, ALU.add), reads=[x, xc, cws], writes=[xc])
                    kb.op("dve", lambda e, a=a, b=b, W=W: e.scalar_tensor_tensor(xc[:, a:b-1], x[:, a+1:b], W(3), xc[:, a:b-1], ALU.mult, ALU.add), reads=[x, xc, cws], writes=[xc])
                pi = 0
                for d in range(2):
                    for g in range(2):
                        dst = ra[d] if g == 0 else iu[d]
                        for (c0, n) in CH:
                            p_ = pg[pi % 4]; pi += 1
                            kb.op("pe", lambda e, d=d, g=g, p_=p_, c0=c0, n=n: e.matmul(p_[:, 0:n], wts[:, d, g, :], xc[:, c0:c0+n], start=True, stop=True), reads=[wts, xc], writes=[p_])
                            kb.op("act", lambda e, d=d, g=g, p_=p_, c0=c0, n=n, dst=dst, j=j: e.activation(dst[:, c0:c0+n], p_[:, 0:n], AF.Sigmoid, bias=lps[:, j, d, g:g+1]), reads=[p_, lps], writes=[dst])
                for d in range(2):
                    kb.op("act", lambda e, d=d, j=j: e.activation(ra[d][:], ra[d][:], AF.Exp, scale=sp_[:, j, d, :]), reads=[ra[d], sp_], writes=[ra[d]])
                for d in range(2):
                    kb.op("pool", lambda e, d=d: e.tensor_tensor(s_[:], ra[d][:], ra[d][:], ALU.mult), reads=[ra[d]], writes=[s_])
                    kb.op("act", lambda e: e.activation(s_[:], s_[:], AF.Sqrt, scale=-1.0, bias=1.0), reads=[s_], writes=[s_])
                    kb.op("pool", lambda e, d=d: e.tensor_tensor(iu[d][:], iu[d][:], xc[:], ALU.mult), reads=[iu[d], xc], writes=[iu[d]])
                    kb.op("dve", lambda e, d=d: e.tensor_tensor(iu[d][:], iu[d][:], s_[:], ALU.mult), reads=[iu[d], s_], writes=[iu[d]])
                hf = x; hb = xc
                kb.op("dve", lambda e: e.tensor_tensor_scan(hf[:, NLAT:TOT], ra[0][:, NLAT:TOT], iu[0][:, NLAT:TOT], 0.0, ALU.mult, ALU.add), reads=[ra[0], iu[0]], writes=[hf])
                kb.op("dve", lambda e: e.tensor_tensor_scan(hf[:, 0:NLAT], ra[0][:, 0:NLAT], iu[0][:, 0:NLAT], hf[:, TOT-1:TOT], ALU.mult, ALU.add), reads=[ra[0], iu[0], hf], writes=[hf])
                kb.op("dve", lambda e: e.tensor_tensor_scan(hb[:, TOT-1:NLAT-1:-1], ra[1][:, TOT-1:NLAT-1:-1], iu[1][:, TOT-1:NLAT-1:-1], 0.0, ALU.mult, ALU.add), reads=[ra[1], iu[1]], writes=[hb])
                kb.op("dve", lambda e: e.tensor_tensor_scan(hb[:, NLAT-1::-1], ra[1][:, NLAT-1::-1], iu[1][:, NLAT-1::-1], hb[:, NLAT:NLAT+1], ALU.mult, ALU.add), reads=[ra[1], iu[1], hb], writes=[hb])
                kb.op("pool", lambda e: e.tensor_tensor(hf[:], hf[:], hb[:], ALU.add), reads=[hf, hb], writes=[hf])
                t1 = ra[0]; t2 = ra[1]
                kb.op("pool", lambda e: e.tensor_tensor(t1[:], ug[:], ug[:], ALU.mult), reads=[ug], writes=[t1])
                kb.op("dve", lambda e: e.tensor_scalar(t1[:], t1[:], 0.044715, 1.0, ALU.mult, ALU.add), reads=[t1], writes=[t1])
                kb.op("pool", lambda e: e.tensor_tensor(t1[:], t1[:], ug[:], ALU.mult), reads=[t1, ug], writes=[t1])
                kb.op("act", lambda e: e.activation(t2[:], t1[:], AF.Sigmoid, scale=1.5957691216057308), reads=[t1], writes=[t2])
                kb.op("dve", lambda e: e.tensor_tensor(t2[:], t2[:], ug[:], ALU.mult), reads=[t2, ug], writes=[t2])
                kb.op("pool", lambda e: e.tensor_tensor(t2[:], t2[:], hf[:], ALU.mult), reads=[t2, hf], writes=[t2])
                st(j, t2)
        with kb.scope():
            bones = kb.sb([128, 128], BF16)
            kb.op("pool", lambda e: e.memset(bones[:], 0.0), writes=[bones])
            kb.op("pool", lambda e: e.memset(bones[0:64, 0:64], 1.0), writes=[bones])
            kb.op("pool", lambda e: e.memset(bones[64:128, 64:128], 1.0), writes=[bones])
            qf = kb.sb([128, TOT]); kf = kb.sb([128, TOT])
            sq = kb.sb([128, 512], BF16); rs_ = kb.sb([128, 512])
            Qbd = kb.sb([128, 68, 2, 64], BF16); Kn = kb.sb([128, TOT], BF16)
            vst = kb.sb([128, TOT]); V0 = kb.sb([128, 34, 128], BF16); V1 = kb.sb([128, 33, 128], BF16)
            bs = kb.sb([128, 8, 512])
            on = kb.sb([128, TOT])
            gsc = kb.sb([128, 2])
            kb.op("dve", lambda e: e.tensor_scalar(gsc[:, 0:1], qkgs[:, 0:1], 0.125, None, ALU.mult), reads=[qkgs], writes=[gsc])
            kb.op("dve", lambda e: e.tensor_copy(gsc[:, 1:2], qkgs[:, 1:2]), reads=[qkgs], writes=[gsc])
            pss = kb.ps([128, 512])
            psw = [kb.ps([128, 512]) for _ in range(2)]; psc = [kb.ps([128, 512]) for _ in range(2)]
            ppt = kb.ps([128, 1024], BF16) ; pso = kb.ps([128, 512]); pot = kb.ps([128, 512]); ptr = [pss, pso]
            sb = [kb.sb([128, 768]) for _ in range(2)]; P = [kb.sb([128, 768], BF16) for _ in range(2)]; Osb = [kb.sb([128, 128]) for _ in range(2)]
            ptb = [kb.sb([128, 768], BF16) for _ in range(2)]; Dm = [kb.sb([128, 128]) for _ in range(2)]
            sm = [kb.sb([128, 4]) for _ in range(3)]
            for j in range(2):
                ld(qf, 4 + j)
                ld(kf, 6 + j)
                kb.dma("sp", bs[:], bias[j], writes=[bs])
                ld(vst, 8 + j)
                for bi in range(34):
                    p_ = ptr[bi % 2]
                    kb.op("pe", lambda e, bi=bi, p_=p_: e.transpose(p_[:, 0:128], vst[:, bi*128:(bi+1)*128], ident[:]), reads=[vst, ident], writes=[p_])
                    kb.op("act", lambda e, bi=bi, p_=p_: e.activation(V0[:, bi, :], p_[:, 0:128], AF.Copy), reads=[p_], writes=[V0])
                for bi in range(33):
                    p_ = ptr[bi % 2]
                    kb.op("pe", lambda e, bi=bi, p_=p_: e.transpose(p_[:, 0:128], vst[:, 64+bi*128:64+(bi+1)*128], ident[:]), reads=[vst, ident], writes=[p_])
                    kb.op("dve", lambda e, bi=bi, p_=p_: e.tensor_copy(V1[:, bi, :], p_[:, 0:128]), reads=[p_], writes=[V1])
                kb.op("pool", lambda e: e.memset(Qbd[:], 0.0), writes=[Qbd])
                for which, src in ((0, qf), (1, kf)):
                    for (c0, n) in CH:
                        kb.op("act", lambda e, src=src, c0=c0, n=n: e.activation(sq[:, 0:n], src[:, c0:c0+n], AF.Square), reads=[src], writes=[sq])
                        kb.op("pe", lambda e, n=n: e.matmul(pss[:, 0:n], bones[:], sq[:, 0:n], start=True, stop=True), reads=[bones, sq], writes=[pss])
                        kb.op("act", lambda e, n=n: e.activation(rs_[:, 0:n], pss[:, 0:n], AF.Sqrt, scale=1.0 / 64, bias=1e-6), reads=[pss], writes=[rs_])
                        kb.op("dve", lambda e, n=n: e.reciprocal(rs_[:, 0:n], rs_[:, 0:n]), reads=[rs_], writes=[rs_])
                        if which == 1:
                            kb.op("dve", lambda e, c0=c0, n=n: e.scalar_tensor_tensor(Kn[:, c0:c0+n], kf[:, c0:c0+n], gsc[:, 1:2], rs_[:, 0:n], ALU.mult, ALU.mult), reads=[kf, gsc, rs_], writes=[Kn])
                        else:
                            r0 = c0 // 64; nr = n // 64
                            for hh in range(2):
                                pa, pb = hh * 64, hh * 64 + 64
                                kb.op("dve", lambda e, c0=c0, n=n, r0=r0, nr=nr, hh=hh, pa=pa, pb=pb: e.scalar_tensor_tensor(
                                    Qbd[pa:pb, r0:r0+nr, hh, :], qf[pa:pb, c0:c0+n].rearrange("p (r c) -> p r c", c=64), gsc[pa:pb, 0:1],
                                    rs_[pa:pb, 0:n].rearrange("p (r c) -> p r c", c=64), ALU.mult, ALU.mult), reads=[qf, gsc, rs_], writes=[Qbd])
                def stS(r):
                        b_ = r % 2
                        sb_, P_, ptb_, D_, sm_ = sb[b_], P[b_], ptb[b_], Dm[b_], sm[r % 3]
                        pw, pc = psw[b_], psc[b_]
                        lhs = Qbd[:, r, :, :].rearrange("p a c -> p (a c)")
                        if r < 64:
                            rs = min(max(r - 4, 0), 56)
                            cls = r if r < 4 else (4 if r <= 60 else r - 56)
                            kb.op("pe", lambda e, lhs=lhs, rs=rs, pw=pw: e.matmul(pw[:], lhs, Kn[:, rs*64:rs*64+512], start=True, stop=True), reads=[Qbd, Kn], writes=[pw])
                            kb.op("dve", lambda e, pw=pw, sb_=sb_, cls=cls: e.tensor_tensor(sb_[:, 0:512], pw[:], bs[:, cls, :], ALU.add), reads=[pw, bs], writes=[sb_])
                            w0 = 0; nb = 6
                        else:
                            w0 = 512; nb = 2
                        kb.op("pe", lambda e, lhs=lhs, pc=pc: e.matmul(pc[:, 0:256], lhs, Kn[:, NLAT:TOT], start=True, stop=True), reads=[Qbd, Kn], writes=[pc])
                        kb.op("act", lambda e, pc=pc, sb_=sb_: e.activation(sb_[:, 512:768], pc[:, 0:256], AF.Copy), reads=[pc], writes=[sb_])
                        kb.op("dve", lambda e, sb_=sb_, sm_=sm_, w0=w0: e.reduce_max(sm_[:, 0:1], sb_[:, w0:768], AX.X, negate=True), reads=[sb_], writes=[sm_])
                        kb.op("act", lambda e, sb_=sb_, sm_=sm_, P_=P_, w0=w0: e.activation(P_[:, w0:768], sb_[:, w0:768], AF.Exp, bias=sm_[:, 0:1], accum_out=sm_[:, 1:2]), reads=[sb_, sm_], writes=[P_, sm_])
                        kb.op("dve", lambda e, sm_=sm_: e.reciprocal(sm_[:, 2:3], sm_[:, 1:2]), reads=[sm_], writes=[sm_])
                def stB(r):
                        b_ = r % 2
                        sb_, P_, ptb_, D_, sm_ = sb[b_], P[b_], ptb[b_], Dm[b_], sm[r % 3]
                        if r < 64:
                            rs = min(max(r - 4, 0), 56)
                            w0 = 0; nb = 6
                        else:
                            w0 = 512; nb = 2
                        for bi in range(nb):
                            o0 = w0 + bi * 128
                            kb.op("pe", lambda e, o0=o0, P_=P_: e.transpose(ppt[:, o0:o0+128], P_[:, o0:o0+128], identb[:]), reads=[P_, identb], writes=[ppt])
                        if nb == 6:
                            kb.op("act", lambda e, ptb_=ptb_: e.activation(ptb_[:, 0:384], ppt[:, 0:384], AF.Copy), reads=[ppt], writes=[ptb_])
                            kb.op("dve", lambda e, ptb_=ptb_: e.tensor_copy(ptb_[:, 384:768], ppt[:, 384:768]), reads=[ppt], writes=[ptb_])
                        else:
                            kb.op("act", lambda e, ptb_=ptb_: e.activation(ptb_[:, 512:768], ppt[:, 512:768], AF.Copy), reads=[ppt], writes=[ptb_])
                def stC(r):
                        b_ = r % 2
                        sb_, P_, ptb_, D_, sm_ = sb[b_], P[b_], ptb[b_], Dm[b_], sm[r % 3]
                        if r < 64:
                            rs = min(max(r - 4, 0), 56)
                            w0 = 0; nb = 6
                        else:
                            w0 = 512; nb = 2
                        for bi in range(nb):
                            o0 = w0 + bi * 128
                            if o0 < 512:
                                Vt = (V0, rs // 2 + bi) if rs % 2 == 0 else (V1, (rs - 1) // 2 + bi)
                            else:
                                Vt = (V0, 32 + (o0 - 512) // 128)
                            kb.op("pe", lambda e, o0=o0, ptb_=ptb_, Vt=Vt, bi=bi, nb=nb: e.matmul(pso[:, 0:128], ptb_[:, o0:o0+128], Vt[0][:, Vt[1], :], start=(bi == 0), stop=(bi == nb - 1)), reads=[Vt[0], ptb_], writes=[pso])
                        O_ = Osb[b_]
                        kb.op("act", lambda e, O_=O_, sm_=sm_: e.activation(O_[:], pso[:, 0:128], AF.Copy, scale=sm_[:, 2:3]), reads=[pso, sm_], writes=[O_])
                        kb.op("pe", lambda e, O_=O_: e.transpose(pot[:, 0:128], O_[:], ident[:]), reads=[O_, ident], writes=[pot])
                        kb.op("act", lambda e, r=r: e.activation(on[0:64, r*64:(r+1)*64], pot[0:64, 0:64], AF.Copy), reads=[pot], writes=[on])
                        kb.op("dve", lambda e, r=r: e.tensor_copy(on[64:128, r*64:(r+1)*64], pot[64:128, 64:128]), reads=[pot], writes=[on])

                stS(0); stS(1); stB(0)
                for r in range(68):
                    if r + 2 < 68:
                        stS(r + 2)
                    stC(r)
                    if r + 1 < 68:
                        stB(r + 1)
                st(2 + j, on)

def na_bias(rpb):
    reps = [0, 1, 2, 3, 30, 61, 62, 63]
    cq = np.arange(64); ck = np.arange(64)
    cs = np.clip(cq - 8, 0, 48)
    valid = (ck[None, :] >= cs[:, None]) & (ck[None, :] < cs[:, None] + 16)
    dcol = np.clip(ck[None, :] - cq[:, None] + 15, 0, 30)
    out = np.full((8, 8, 64, 8, 64), -1e30, np.float32)
    for ci, r in enumerate(reps):
        rs = min(max(r - 4, 0), 56)
        for j in range(8):
            drow = rs + j - r + 7
            g = rpb[:, drow][:, dcol]
            out[:, ci, :, j, :] = np.where(valid[None], g, np.float32(-1e30))
    return out.reshape(8, 8, 64, 512)

def blockdiag2(wblk):
    o = np.zeros((128, 128), np.float32)
    o[0:64, 0:64] = wblk[0]; o[64:128, 64:128] = wblk[1]
    return o

def mixe_inputs(uT, hf, P):
    c0 = hf * 256
    ux = uT[c0:c0+256]; ug = uT[512+c0:512+c0+256]
    q = uT[1024+c0:1024+c0+256]; k = uT[1536+c0:1536+c0+256]; v = uT[2048+c0:2048+c0+256]
    cw = np.zeros((128, 2, 5), np.float32)
    for j in range(2):
        cw[:, j, 0:4] = P['lru_conv_w'][:, c0+j*128:c0+(j+1)*128].T
        cw[:, j, 4] = P['lru_conv_b'][c0+j*128:c0+(j+1)*128]
    wbd = np.zeros((2, 2, 2, 128, 128), np.float32)
    lp = np.zeros((128, 2, 2, 3), np.float32)
    for j in range(2):
        for d in range(2):
            b0 = hf * 4 + j * 2
            wbd[j, d, 0] = blockdiag2(P['lru_wa'][d, b0:b0+2]); wbd[j, d, 1] = blockdiag2(P['lru_wx'][d, b0:b0+2])
            sl = slice(c0+j*128, c0+(j+1)*128)
            lp[:, j, d, 0] = P['lru_ba'][d, sl]; lp[:, j, d, 1] = P['lru_bx'][d, sl]; lp[:, j, d, 2] = P['lru_lam'][d, sl]
    qkg = np.stack([np.tile(P['na_q_g'], 2), np.tile(P['na_k_g'], 2)], 1).astype(np.float32)
    nb = na_bias(P['na_rpb'])
    bias = np.zeros((2, 128, 8, 512), np.float32)
    for j in range(2):
        h0 = hf * 4 + j * 2
        bias[j, 0:64] = nb[h0].transpose(1, 0, 2); bias[j, 64:128] = nb[h0+1].transpose(1, 0, 2)
    C = np.ascontiguousarray
    return {"uxT": C(ux), "ugT": C(ug), "qT": C(q), "kT": C(k), "vT": C(v), "cw": cw, "wbd": wbd, "lp": lp, "qkg": qkg, "bias": bias}


TOT = 4352; NLAT = 4096
NCH = 34
ORDER = {0: [32, 33] + list(range(32)), 1: [33, 32] + list(range(31, -1, -1))}

def conv_silu(kb, src, dst, cp, ti, tmp, func=AF.Silu):
    W = lambda i: cp[:, ti, i:i+1]
    kb.op("dve", lambda e: e.tensor_scalar(tmp[:], src[:], W(2), W(4), ALU.mult, ALU.add), reads=[src, cp], writes=[tmp])
    for (a, b) in ((0, NLAT), (NLAT, TOT)):
        kb.op("dve", lambda e, a=a, b=b: e.scalar_tensor_tensor(tmp[:, a+2:b], src[:, a:b-2], W(0), tmp[:, a+2:b], ALU.mult, ALU.add), reads=[src, tmp, cp], writes=[tmp])
        kb.op("dve", lambda e, a=a, b=b: e.scalar_tensor_tensor(tmp[:, a+1:b], src[:, a:b-1], W(1), tmp[:, a+1:b], ALU.mult, ALU.add), reads=[src, tmp, cp], writes=[tmp])
        kb.op("dve", lambda e, a=a, b=b: e.scalar_tensor_tensor(tmp[:, a:b-1], src[:, a+1:b], W(3), tmp[:, a:b-1], ALU.mult, ALU.add), reads=[src, tmp, cp], writes=[tmp])
    kb.op("act", lambda e: e.activation(dst, tmp[:], func), reads=[tmp], writes=[dst.buf] if hasattr(dst, "buf") else [])

def emit_mixo(kb, ld, ld_rows, rowp, convp, chp, st):
    if True:
        ident = kb.ident(F32)
        identb = kb.sb([128, 128], BF16)
        kb.op("dve", lambda e: e.tensor_copy(identb[:], ident[:]), reads=[ident], writes=[identb])
        onesf = kb.sb([128, 128], F32); onesb = kb.sb([128, 128], BF16)
        kb.op("pool", lambda e: e.memset(onesf[:], 1.0), writes=[onesf])
        kb.op("pool", lambda e: e.memset(onesb[:], 1.0), writes=[onesb])
        maskf = kb.sb([128, 128], F32); maskb = kb.sb([128, 128], F32)
        kb.op("pool", lambda e: e.affine_select(maskf[:], onesf[:], [[1, 128]], ALU.is_ge, 0.0, base=0, channel_multiplier=-1), reads=[onesf], writes=[maskf])
        kb.op("pool", lambda e: e.affine_select(maskb[:], onesf[:], [[-1, 128]], ALU.is_ge, 0.0, base=0, channel_multiplier=1), reads=[onesf], writes=[maskb])
        masks = [maskf, maskb]
        cps = kb.sb([128, 8, 5]); chs = kb.sb([128, 2, 3]); rps = kb.sb([128, 2])
        kb.dma("sp", cps[:], convp, writes=[cps]); kb.dma("sp", chs[:], chp, writes=[chs]); kb.dma("sp", rps[:], rowp, writes=[rps])
        RC = kb.sb([48, TOT], F32)
        cols = kb.sb([128, NCH, 48], F32)
        DC = kb.sb([128, NCH, 4, 12], F32)
        sel = kb.sb([48, 12, 128], F32)
        with kb.scope():
            rs = kb.sb([128, TOT]); t1 = kb.sb([128, TOT]); t2 = kb.sb([128, TOT]); t3 = kb.sb([128, TOT])
            Tg = kb.sb([128, TOT]); Tl = kb.sb([128, TOT]); Gf = kb.sb([128, TOT]); Gb = kb.sb([128, TOT])
            ld_rows(rs)
            acol = kb.sb([128, 1])
            kb.op("act", lambda e: e.activation(acol[0:8, :], rps[0:8, 1:2], AF.Exp), reads=[rps], writes=[acol])
            kb.op("dve", lambda e: e.tensor_scalar(acol[0:8, :], acol[0:8, :], -1.0, None, ALU.mult), reads=[acol], writes=[acol])
            for (p0, p1) in ((0, 8), (32, 36)):
                S = slice(p0, p1)
                kb.op("dve", lambda e, S=S: e.tensor_scalar(t1[S, :], rs[S, :], rps[S, 0:1], None, ALU.add), reads=[rs, rps], writes=[t1])
                kb.op("dve", lambda e, S=S: e.scalar_tensor_tensor(t2[S, :], t1[S, :], -1.0, t1[S, :], ALU.mult, ALU.max), reads=[t1], writes=[t2])
                kb.op("act", lambda e, S=S: e.activation(t2[S, :], t2[S, :], AF.Exp, scale=-1.0), reads=[t2], writes=[t2])
                kb.op("act", lambda e, S=S: e.activation(t2[S, :], t2[S, :], AF.Ln, bias=1.0), reads=[t2], writes=[t2])
            S = slice(0, 8)
            kb.op("dve", lambda e: e.scalar_tensor_tensor(t3[S, :], t1[S, :], 0.0, t2[S, :], ALU.max, ALU.add), reads=[t1, t2], writes=[t3])
            kb.op("dve", lambda e: e.tensor_scalar(Tg[S, :], t3[S, :], acol[S, 0:1], None, ALU.mult), reads=[t3, acol], writes=[Tg])
            kb.op("act", lambda e: e.activation(Tl[S, :], t3[S, :], AF.Ln), reads=[t3], writes=[Tl])
            S2 = slice(32, 36)
            kb.op("dve", lambda e: e.scalar_tensor_tensor(Tg[S2, :], t1[S2, :], 0.0, t2[S2, :], ALU.min, ALU.subtract), reads=[t1, t2], writes=[Tg])
            S3 = slice(96, 100)
            kb.op("dve", lambda e: e.tensor_scalar(Tl[S3, :], rs[S3, :], rps[S3, 0:1], None, ALU.add), reads=[rs, rps], writes=[Tl])
            for S_ in (S, S2):
                for ci in range(NCH):
                    a, b = ci * 128, ci * 128 + 128
                    kb.op("dve", lambda e, S_=S_, a=a, b=b: e.tensor_tensor_scan(Gf[S_, a:b], onesf[S_, :], Tg[S_, a:b], 0.0, ALU.mult, ALU.add), reads=[onesf, Tg], writes=[Gf])
                    kb.op("dve", lambda e, S_=S_, a=a, b=b: e.tensor_tensor_scan(Gb[S_, b-1:(a-1 if a > 0 else None):-1], onesf[S_, :], Tg[S_, b-1:(a-1 if a > 0 else None):-1], 0.0, ALU.mult, ALU.add), reads=[onesf, Tg], writes=[Gb])
            for (dst0, src, p0, n) in ((0, Tg, 0, 8), (8, Tg, 32, 4), (12, Tl, 0, 8), (20, Tl, 96, 4), (24, Gf, 0, 8), (32, Gf, 32, 4), (36, Gb, 0, 8), (44, Gb, 32, 4)):
                kb.dma("sp", RC[dst0:dst0+n, :], src[p0:p0+n, :], reads=[src], writes=[RC])
            pt = [kb.ps([128, 512]) for _ in range(2)]
            for ci in range(NCH):
                p_ = pt[ci % 2]
                kb.op("pe", lambda e, ci=ci, p_=p_: e.transpose(p_[:, 0:48], RC[0:48, ci*128:(ci+1)*128], ident[0:48, 0:48]), reads=[RC, ident], writes=[p_])
                kb.op("act", lambda e, ci=ci, p_=p_: e.activation(cols[:, ci, :], p_[:, 0:48], AF.Copy), reads=[p_], writes=[cols])
            pg = kb.ps([128, 512])
            kb.op("pe", lambda e: e.matmul(pg[:, 0:NCH*12].rearrange("p (c j) -> p c j", j=12), onesf[:], cols[:, :, 0:12], start=True, stop=True), reads=[onesf, cols], writes=[pg])
            kb.op("act", lambda e: e.activation(DC[:, :, 3, :], pg[:, 0:NCH*12].rearrange("p (c j) -> p c j", j=12), AF.Copy), reads=[pg], writes=[DC])
            for (j0, j1, d) in ((0, 4, 0), (4, 8, 1), (8, 10, 0), (10, 12, 1)):
                g0 = 24 if d == 0 else 36
                kb.op("dve", lambda e, j0=j0, j1=j1, g0=g0: e.tensor_scalar(DC[:, :, 0, j0:j1], cols[:, :, g0+j0:g0+j1], -1.0, None, ALU.mult), reads=[cols], writes=[DC])
                kb.op("dve", lambda e, j0=j0, j1=j1: e.tensor_tensor(DC[:, :, 1, j0:j1], DC[:, :, 0, j0:j1], cols[:, :, 12+j0:12+j1], ALU.add), reads=[DC, cols], writes=[DC])
                kb.op("dve", lambda e, j0=j0, j1=j1: e.tensor_tensor(DC[:, :, 1, j0:j1], DC[:, :, 1, j0:j1], DC[:, :, 3, j0:j1], ALU.add), reads=[DC], writes=[DC])
            kb.op("act", lambda e: e.activation(DC[:, :, 1, :], DC[:, :, 1, :], AF.Exp), reads=[DC], writes=[DC])
            kb.op("act", lambda e: e.activation(DC[:, :, 2, :], DC[:, :, 3, :], AF.Exp), reads=[DC], writes=[DC])
            for j in range(12):
                d = 0 if (j < 4 or j in (8, 9)) else 1
                r = (24 if d == 0 else 36) + j
                kb.op("dve", lambda e, j=j, r=r: e.tensor_scalar(sel[:, j, :], onesf[0:48, :], ident[0:48, r:r+1], None, ALU.mult), reads=[onesf, ident], writes=[sel])

        def gla(units, Qf, Kf, Ktok, Vtok, dv, ysum, is_ml):
            pqk = kb.ps([128, 512]); pbc = [kb.ps([128, 512]) for _ in range(2)]
            pys = [kb.ps([128, 512]) for _ in range(2)]
            if is_ml:
                pden = kb.ps([128, 512]); pdss = [kb.ps([128, 512])]; pdn = kb.ps([128, 512])
            else:
                pdss = [kb.ps([128, 512]) for _ in range(2)]
            QKm2 = [kb.sb([128, 128]) for _ in range(2)]; dcl = [kb.sb([128, 128]) for _ in range(2)]; Wd = [kb.sb([128, 128]) for _ in range(2)]
            AT = [kb.sb([128, 128], BF16) for _ in range(2)]; Ebc = [kb.sb([128, 128]) for _ in range(2)]
            Qp = [kb.sb([128, 128], BF16) for _ in range(2)]; Kp = [kb.sb([128, 128], BF16) for _ in range(2)]
            rden = [kb.sb([128, 128]) for _ in range(2)]
            nu = len(units)
            Sst2 = [[kb.sb([128, dv]) for _ in range(nu)] for _ in range(2)]; Sb2 = [[kb.sb([128, dv], BF16) for _ in range(nu)] for _ in range(2)]
            if is_ml:
                nrep2 = [[kb.sb([128, 128]) for _ in range(nu)] for _ in range(2)]; nrb2 = [[kb.sb([128, 128], BF16) for _ in range(nu)] for _ in range(2)]
            it = 0
            touched = set()
            for d in range(2):
                for u in range(nu):
                    kb.op("pool", lambda e, u=u, d=d: e.memset(Sst2[d][u][:], 0.0), writes=[Sst2[d][u]])
                    kb.op("pool", lambda e, u=u, d=d: e.memset(Sb2[d][u][:], 0.0), writes=[Sb2[d][u]])
                    if is_ml:
                        kb.op("pool", lambda e, u=u, d=d: e.memset(nrep2[d][u][:], 0.0), writes=[nrep2[d][u]])
                        kb.op("pool", lambda e, u=u, d=d: e.memset(nrb2[d][u][:], 0.0), writes=[nrb2[d][u]])
            def mk_iter(d, ci, u, unit, b_, do_qk, first):
                jf, jb, qkg, yt, yp0 = unit
                j = jf if d == 0 else jb
                Sst = Sst2[d]; Sb = Sb2[d]; QKm = QKm2[d]
                nrep = nrep2[d] if is_ml else None; nrb = nrb2[d] if is_ml else None
                lat = ci < 32
                c0 = ci * 128
                py = pys[b_]; pds = pdss[b_ % len(pdss)]
                bc = pbc[b_]
                yo = py[yp0:yp0+dv, 0:128]
                ydst = yt[yp0:yp0+dv, c0:c0+128] if lat else None
                def P():
                    if lat:
                        if do_qk:
                            kb.op("pe", lambda e: e.matmul(pqk[:, 0:128], Kf[1](qkg, ci), Qf[1](qkg, ci), start=True, stop=True), reads=[Kf[0], Qf[0]], writes=[pqk])
                            kb.op("dve", lambda e: e.tensor_tensor(QKm[:], pqk[:, 0:128], masks[d][:], ALU.mult), reads=[pqk, masks[d]], writes=[QKm])
                        kb.op("pe", lambda e: e.matmul(bc[:, 0:128], sel[:, j, :], RC[0:48, c0:c0+128], start=True, stop=True), reads=[sel, RC], writes=[bc])
                        kb.op("dve", lambda e: e.tensor_scalar(dcl[b_][:], bc[:, 0:128], DC[:, ci, 0, j:j+1], 0.0, ALU.add, ALU.min), reads=[bc, DC], writes=[dcl[b_]])
                        kb.op("act", lambda e: e.activation(Wd[b_][:], dcl[b_][:], AF.Exp, bias=cols[:, ci, 12+j:13+j]), reads=[dcl[b_], cols], writes=[Wd[b_]])
                        kb.op("dve", lambda e: e.tensor_tensor(AT[b_][:], QKm[:], Wd[b_][:], ALU.mult), reads=[QKm, Wd[b_]], writes=[AT[b_]])
                        kb.op("act", lambda e: e.activation(Ebc[b_][:], bc[:, 0:128], AF.Exp), reads=[bc], writes=[Ebc[b_]])
                        kb.op("pool", lambda e: e.tensor_tensor(Qp[b_][:], Qf[1](qkg, ci), Ebc[b_][:], ALU.mult), reads=[Qf[0], Ebc[b_]], writes=[Qp[b_]])
                    kb.op("pool", lambda e: e.tensor_scalar(Kp[b_][:], Ktok[1](qkg, ci), DC[:, ci, 1, j:j+1], 1.0, ALU.mult, ALU.mult), reads=[Ktok[0], DC], writes=[Kp[b_]])
                def Q():
                    if lat:
                        kb.op("pe", lambda e: e.matmul(yo, Vtok[1](u, ci), AT[b_][:], start=True, stop=False), reads=[Vtok[0], AT[b_]], writes=[py])
                        kb.op("pe", lambda e: e.matmul(yo, Sb[u][:], Qp[b_][:], start=False, stop=True), reads=[Sb[u], Qp[b_]], writes=[py])
                        if is_ml:
                            kb.op("pe", lambda e: e.matmul(pden[:, 0:128], onesb[:], AT[b_][:], start=True, stop=False), reads=[onesb, AT[b_]], writes=[pden])
                            kb.op("pe", lambda e: e.matmul(pden[:, 0:128], nrb[u][:], Qp[b_][:], start=False, stop=True), reads=[nrb[u], Qp[b_]], writes=[pden])
                            kb.op("act", lambda e: e.activation(rden[b_][:], pden[:, 0:128], AF.Abs), reads=[pden], writes=[rden[b_]])
                            kb.op("dve", lambda e: e.tensor_scalar(rden[b_][:], rden[b_][:], 1.0, None, ALU.max), reads=[rden[b_]], writes=[rden[b_]])
                            kb.op("dve", lambda e: e.reciprocal(rden[b_][:], rden[b_][:]), reads=[rden[b_]], writes=[rden[b_]])
                            if first:
                                kb.op("dve", lambda e: e.tensor_tensor(ydst, yo, rden[b_][:], ALU.mult), reads=[py, rden[b_]], writes=[yt])
                            else:
                                kb.op("dve", lambda e: e.tensor_tensor(rden[b_][:], yo, rden[b_][:], ALU.mult), reads=[py, rden[b_]], writes=[rden[b_]])
                                kb.op("pool", lambda e: e.tensor_tensor(ydst, ydst, rden[b_][:], ALU.add), reads=[yt, rden[b_]], writes=[yt])
                        else:
                            if first:
                                kb.op("act", lambda e: e.activation(ydst, yo, AF.Copy), reads=[py], writes=[yt])
                            else:
                                kb.op("dve", lambda e: e.tensor_tensor(ydst, yo, ydst, ALU.add), reads=[py, yt], writes=[yt])
                    kb.op("pe", lambda e: e.matmul(pds[:, 0:dv], Kp[b_][:], Vtok[1](u, ci), start=True, stop=True), reads=[Kp[b_], Vtok[0]], writes=[pds])
                    kb.op("dve", lambda e: e.scalar_tensor_tensor(Sst[u][:], Sst[u][:], DC[:, ci, 2, j:j+1], pds[:, 0:dv], ALU.mult, ALU.add), reads=[Sst[u], DC, pds], writes=[Sst[u]])
                    kb.op("act", lambda e: e.activation(Sb[u][:], Sst[u][:], AF.Copy), reads=[Sst[u]], writes=[Sb[u]])
                    if is_ml:
                        kb.op("pe", lambda e: e.matmul(pdn[:, 0:128], Kp[b_][:], onesb[:], start=True, stop=True), reads=[Kp[b_], onesb], writes=[pdn])
                        kb.op("dve", lambda e: e.scalar_tensor_tensor(nrep[u][:], nrep[u][:], DC[:, ci, 2, j:j+1], pdn[:, 0:128], ALU.mult, ALU.add), reads=[nrep[u], DC, pdn], writes=[nrep[u]])
                        kb.op("act", lambda e: e.activation(nrb[u][:], nrep[u][:], AF.Copy), reads=[nrep[u]], writes=[nrb[u]])
                return P, Q
            its = []
            for step in range(NCH):
                for d in range(2):
                    ci = ORDER[d][step]
                    qk_done = {}
                    for u, unit in enumerate(units):
                        qkg = unit[2]; yt = unit[3]; yp0 = unit[4]
                        b_ = it % 2; it += 1
                        do_qk = (ci < 32) and (qkg not in qk_done)
                        qk_done[qkg] = 1
                        first = False
                        if ci < 32:
                            first = (id(yt), yp0, ci) not in touched; touched.add((id(yt), yp0, ci))
                        its.append(mk_iter(d, ci, u, unit, b_, do_qk, first))
            its[0][0]()
            for k in range(len(its)):
                if k + 1 < len(its):
                    its[k + 1][0]()
                its[k][1]()

        def to_tok(src_bf, dst_fn, ptr, idn=None):
            idn = idn or identb
            for ci in range(NCH):
                p_ = ptr[ci % 2]
                kb.op("pe", lambda e, ci=ci, p_=p_: e.transpose(p_[:, 0:128], src_bf[:, ci*128:(ci+1)*128], idn[:]), reads=[src_bf, idn], writes=[p_])
                dst, buf = dst_fn(ci)
                if ci % 2 == 0:
                    kb.op("act", lambda e, p_=p_, dst=dst: e.activation(dst, p_[:, 0:128], AF.Copy), reads=[p_], writes=[buf])
                else:
                    kb.op("dve", lambda e, p_=p_, dst=dst: e.tensor_copy(dst, p_[:, 0:128]), reads=[p_], writes=[buf])

        def norm_gate_out(ysrc, gate_src_dram, gfunc, gcol, row0, ntile_sum, stg, tmpb):
            pass

        with kb.scope():
            xsc = [kb.sb([128, TOT]) for _ in range(2)]
            Bf = kb.sb([128, TOT], BF16); Cf = kb.sb([128, TOT], BF16)
            Xtok = kb.sb([128, NCH, 256], BF16); Btok = kb.sb([128, NCH, 128], BF16)
            ys = [kb.sb([128, NLAT]) for _ in range(2)]
            with kb.scope():
                stg = kb.sb([128, TOT]); tmp = kb.sb([128, TOT])
                ptr = [kb.ps([128, 128], BF16) for _ in range(2)]
                ptrf = [kb.ps([128, 128], F32) for _ in range(2)]
                for t in range(2):
                    ld(stg, 2 + t)
                    W = lambda i, t=t: cps[:, t, i:i+1]
                    conv_silu_(kb, stg, xsc[t], cps, t, tmp)
                    to_tok(xsc[t], lambda ci, t=t: (Xtok[:, ci, t*128:(t+1)*128], Xtok), ptrf, idn=ident)
                ld(stg, 4)
                conv_silu_(kb, stg, Bf, cps, 2, tmp)
                to_tok(Bf, lambda ci: (Btok[:, ci, :], Btok), ptr)
                ld(stg, 5)
                conv_silu_(kb, stg, Cf, cps, 3, tmp)
            with kb.scope():
                units = [(hl, 4 + hl, 0, ys[hl // 2], (hl % 2) * 64) for hl in range(4)]
                gla(units, (Cf, lambda g, ci: Cf[:, ci*128:(ci+1)*128]), (Bf, lambda g, ci: Bf[:, ci*128:(ci+1)*128]),
                    (Btok, lambda g, ci: Btok[:, ci, :]), (Xtok, lambda u, ci: Xtok[:, ci, u*64:(u+1)*64]), 64, None, False)
            with kb.scope():
                stgm = kb.sb([128, TOT]); tmpm = kb.sb([128, TOT])
                pss = kb.ps([128, 512])
                sqb = [kb.sb([128, 512], BF16) for _ in range(2)]
                rstd = kb.sb([128, 512])
                for t in range(2):
                    ld(stgm, 0 + t)
                    kb.op("act", lambda e: e.activation(tmpm[:, 0:NLAT], stgm[:, 0:NLAT], AF.Silu), reads=[stgm], writes=[tmpm])
                    kb.op("dve", lambda e, t=t: e.scalar_tensor_tensor(ys[t][:], xsc[t][:, 0:NLAT], chs[:, t, 0:1], ys[t][:], ALU.mult, ALU.add), reads=[xsc[t], chs, ys[t]], writes=[ys[t]])
                    kb.op("pool", lambda e, t=t: e.tensor_tensor(ys[t][:], ys[t][:], tmpm[:, 0:NLAT], ALU.mult), reads=[ys[t], tmpm], writes=[ys[t]])
                for c in range(8):
                    a, b = c * 512, c * 512 + 512
                    for t in range(2):
                        kb.op("act", lambda e, t=t, a=a, b=b: e.activation(sqb[t][:], ys[t][:, a:b], AF.Square), reads=[ys[t]], writes=[sqb[t]])
                        kb.op("pe", lambda e, t=t: e.matmul(pss[:], onesb[:], sqb[t][:], start=(t == 0), stop=(t == 1)), reads=[onesb, sqb[t]], writes=[pss])
                    kb.op("act", lambda e: e.activation(rstd[:], pss[:], AF.Sqrt, scale=1.0 / 256, bias=1e-6), reads=[pss], writes=[rstd])
                    kb.op("dve", lambda e: e.reciprocal(rstd[:], rstd[:]), reads=[rstd], writes=[rstd])
                    for t in range(2):
                        kb.op("dve", lambda e, t=t, a=a, b=b: e.scalar_tensor_tensor(ys[t][:, a:b], ys[t][:, a:b], chs[:, t, 1:2], rstd[:], ALU.mult, ALU.mult), reads=[ys[t], chs, rstd], writes=[ys[t]])
                for t in range(2):
                    st(t, ys[t])
        with kb.scope():
            Qf = kb.sb([128, 2, TOT], BF16); Kf = kb.sb([128, 2, TOT], BF16); vb = kb.sb([128, TOT], BF16)
            Ktok = kb.sb([128, NCH, 2, 128], BF16); Vtok = kb.sb([128, NCH, 2, 128], BF16)
            hs = [kb.sb([128, NLAT]) for _ in range(2)]
            with kb.scope():
                stg2 = kb.sb([128, TOT]); tmp2 = kb.sb([128, TOT])
                ptr2 = [kb.ps([128, 128], BF16) for _ in range(2)]
                for t in range(2):
                    ld(stg2, 6 + t)
                    conv_silu_(kb, stg2, kb.view(Qf.t, "Qf"), cps, 4 + t, tmp2, dst_ap=Qf[:, t, :], dst_buf=Qf)
                    ld(stg2, 8 + t)
                    conv_silu_(kb, stg2, None, cps, 6 + t, tmp2, dst_ap=stg2[:], dst_buf=stg2)
                    kb.op("dve", lambda e, t=t: e.tensor_scalar(Kf[:, t, :], stg2[:], float(128 ** -0.5), None, ALU.mult), reads=[stg2], writes=[Kf])
                    kf_t = kb.view(Kf.t, "kf")
                    for ci in range(NCH):
                        p_ = ptr2[ci % 2]
                        kb.op("pe", lambda e, ci=ci, p_=p_, t=t: e.transpose(p_[:, 0:128], Kf[:, t, ci*128:(ci+1)*128], identb[:]), reads=[Kf, identb], writes=[p_])
                        kb.op("act", lambda e, ci=ci, p_=p_, t=t: e.activation(Ktok[:, ci, t, :], p_[:, 0:128], AF.Copy), reads=[p_], writes=[Ktok])
                    ld(stg2, 10 + t)
                    kb.op("pool", lambda e: e.tensor_copy(vb[:], stg2[:]), reads=[stg2], writes=[vb])
                    to_tok(vb, lambda ci, t=t: (Vtok[:, ci, t, :], Vtok), ptr2)
            with kb.scope():
                units2 = [(8 + hl, 10 + hl, hl, hs[hl], 0) for hl in range(2)]
                gla(units2, (Qf, lambda g, ci: Qf[:, g, ci*128:(ci+1)*128]), (Kf, lambda g, ci: Kf[:, g, ci*128:(ci+1)*128]),
                    (Ktok, lambda g, ci: Ktok[:, ci, g, :]), (Vtok, lambda u, ci: Vtok[:, ci, u, :]), 128, None, True)
            with kb.scope():
                stg3 = kb.sb([128, TOT]); tmp3 = kb.sb([128, TOT])
                pss2 = kb.ps([128, 512]); sqb2 = kb.sb([128, 512], BF16); rstd2 = kb.sb([128, 512])
                for t in range(2):
                    ld(stg3, 12 + t)
                    kb.op("act", lambda e: e.activation(tmp3[:, 0:NLAT], stg3[:, 0:NLAT], AF.Sigmoid), reads=[stg3], writes=[tmp3])
                    for c in range(8):
                        a, b = c * 512, c * 512 + 512
                        kb.op("act", lambda e, t=t, a=a, b=b: e.activation(sqb2[:], hs[t][:, a:b], AF.Square), reads=[hs[t]], writes=[sqb2])
                        kb.op("pe", lambda e: e.matmul(pss2[:], onesb[:], sqb2[:], start=True, stop=True), reads=[onesb, sqb2], writes=[pss2])
                        kb.op("act", lambda e: e.activation(rstd2[:], pss2[:], AF.Sqrt, scale=1.0 / 128, bias=1e-6), reads=[pss2], writes=[rstd2])
                        kb.op("dve", lambda e: e.reciprocal(rstd2[:], rstd2[:]), reads=[rstd2], writes=[rstd2])
                        kb.op("dve", lambda e, t=t, a=a, b=b: e.scalar_tensor_tensor(hs[t][:, a:b], hs[t][:, a:b], chs[:, t, 2:3], rstd2[:], ALU.mult, ALU.mult), reads=[hs[t], chs, rstd2], writes=[hs[t]])
                    kb.op("pool", lambda e, t=t: e.tensor_tensor(hs[t][:], hs[t][:], tmp3[:, 0:NLAT], ALU.mult), reads=[hs[t], tmp3], writes=[hs[t]])
                    st(2 + t, hs[t])

def conv_silu_(kb, src, dst, cp, ti, tmp, dst_ap=None, dst_buf=None, func=AF.Silu):
    if dst_ap is None:
        dst_ap = dst[:]; dst_buf = dst
    W = lambda i: cp[:, ti, i:i+1]
    kb.op("dve", lambda e: e.tensor_scalar(tmp[:], src[:], W(2), W(4), ALU.mult, ALU.add), reads=[src, cp], writes=[tmp])
    for (a, b) in ((0, NLAT), (NLAT, TOT)):
        kb.op("dve", lambda e, a=a, b=b: e.scalar_tensor_tensor(tmp[:, a+2:b], src[:, a:b-2], W(0), tmp[:, a+2:b], ALU.mult, ALU.add), reads=[src, tmp, cp], writes=[tmp])
        kb.op("dve", lambda e, a=a, b=b: e.scalar_tensor_tensor(tmp[:, a+1:b], src[:, a:b-1], W(1), tmp[:, a+1:b], ALU.mult, ALU.add), reads=[src, tmp, cp], writes=[tmp])
        kb.op("dve", lambda e, a=a, b=b: e.scalar_tensor_tensor(tmp[:, a:b-1], src[:, a+1:b], W(3), tmp[:, a:b-1], ALU.mult, ALU.add), reads=[src, tmp, cp], writes=[tmp])
    kb.op("act", lambda e: e.activation(dst_ap, tmp[:], func), reads=[tmp], writes=[dst_buf])

def mixo_inputs(uT, hf, P):
    C = np.ascontiguousarray
    c0 = hf * 256
    z = uT[c0:c0+256]; xs = uT[512+c0:512+c0+256]
    Bm = uT[512+512+hf*128:512+512+hf*128+128]; Cm = uT[512+768+hf*128:512+768+hf*128+128]
    dtr = uT[1536:1552]; mq = uT[1552+c0:1552+c0+256]; mk = uT[2064+c0:2064+c0+256]; mv = uT[2576+c0:2576+c0+256]; mo = uT[3088+c0:3088+c0+256]
    mg = uT[3600:3616]
    rows = np.zeros((128, TOT), np.float32); rowp = np.zeros((128, 2), np.float32)
    for d in range(2):
        for hl in range(4):
            h = hf * 4 + hl
            rows[d*4+hl] = dtr[d*8+h]; rowp[d*4+hl, 0] = P['ssd_dt_bias'][d, h]; rowp[d*4+hl, 1] = P['ssd_a_log'][d, h]
        for hl in range(2):
            h = hf * 2 + hl
            rows[32+d*2+hl] = mg[d*8+4+h]; rowp[32+d*2+hl, 0] = P['ml_gate_b'][d, 1, h]
            rows[96+d*2+hl] = mg[d*8+h]; rowp[96+d*2+hl, 0] = P['ml_gate_b'][d, 0, h]
    convp = np.zeros((128, 8, 5), np.float32)
    def cv(w, b, ch0):
        o = np.zeros((128, 5), np.float32); o[:, 0:4] = w[:, ch0:ch0+128].T; o[:, 4] = b[ch0:ch0+128]; return o
    convp[:, 0] = cv(P['ssd_conv_w'], P['ssd_conv_b'], c0); convp[:, 1] = cv(P['ssd_conv_w'], P['ssd_conv_b'], c0+128)
    convp[:, 2] = cv(P['ssd_conv_w'], P['ssd_conv_b'], 512+hf*128); convp[:, 3] = cv(P['ssd_conv_w'], P['ssd_conv_b'], 768+hf*128)
    convp[:, 4] = cv(P['ml_conv_w'], P['ml_conv_b'], c0); convp[:, 5] = cv(P['ml_conv_w'], P['ml_conv_b'], c0+128)
    convp[:, 6] = cv(P['ml_conv_w'], P['ml_conv_b'], 512+c0); convp[:, 7] = cv(P['ml_conv_w'], P['ml_conv_b'], 512+c0+128)
    chp = np.zeros((128, 2, 3), np.float32)
    for t in range(2):
        chp[:, t, 0] = np.repeat(P['ssd_d'][hf*4+t*2:hf*4+t*2+2], 64)
        chp[:, t, 1] = P['ssd_norm_g'][c0+t*128:c0+(t+1)*128]
        chp[:, t, 2] = P['ml_norm_g'][c0+t*128:c0+(t+1)*128]
    return {"zT": C(z), "xsT": C(xs), "BT": C(Bm), "CT": C(Cm), "mqT": C(mq), "mkT": C(mk), "mvT": C(mv), "moT": C(mo),
            "rows": rows, "rowp": rowp, "convp": convp, "chp": chp}


PAIRS = [[0, 1], [2, 3], [4, 5], [6, 7]]

def emit_cc(kb, in_ap, out_ap, reads, writes):
    e = "pool"
    kb._deps(e, reads, writes)
    kb.ccn += 1
    sem = kb.ccsem
    ev = (sem, kb.ccn, "cc")
    kb.prog[e].append(lambda eng, i=in_ap, o=out_ap, sem=sem: eng.collective_compute("AllGather", ALU.bypass, replica_groups=PAIRS, ins=[i], outs=[o]).then_inc(sem))
    kb._commit(ev, reads, writes)

def emit_mod(kb, cT, mod_w, mod_bl, mods):
    with kb.scope():
        cs = kb.sb([128, 8, 2]); sc = kb.sb([128, 8, 2]); bs = kb.sb([128, 2, 48])
        ident2 = kb.ident(F32)
        kb.dma("sp", cs[:], cT, writes=[cs]); kb.dma("sp", bs[:], mod_bl, writes=[bs])
        kb.op("act", lambda e: e.activation(sc[:], cs[:], AF.Silu), reads=[cs], writes=[sc])
        ws = [kb.sb([128, 8, 1536]) for _ in range(2)]
        pg = [kb.ps([128, 512]) for _ in range(3)]
        pt = [kb.ps([128, 512]) for _ in range(2)]
        msb = [kb.sb([2, 1536]) for _ in range(2)]
        ci = 0
        for l in range(2):
            wv = mod_w[l].rearrange("(kt p) m -> p kt m", p=128)
            for q in range(4):
                w_ = ws[ci % 2]; m_ = msb[ci % 2]; ci += 1
                for kt in range(8):
                    kb.dma("sp", w_[:, kt, :], wv[:, kt, q*1536:(q+1)*1536], writes=[w_])
                for g in range(3):
                    for kt in range(8):
                        kb.op("pe", lambda e, g=g, kt=kt, w_=w_: e.matmul(pg[g][0:2, 0:512], sc[:, kt, :], w_[:, kt, g*512:(g+1)*512], start=(kt == 0), stop=(kt == 7)), reads=[w_, sc], writes=[pg[g]])
                    kb.op("act", lambda e, g=g, m_=m_: e.activation(m_[:, g*512:(g+1)*512], pg[g][0:2, 0:512], AF.Copy), reads=[pg[g]], writes=[m_])
                for mm in range(12):
                    m = q * 12 + mm
                    j, kto = divmod(m, 8)
                    p_ = pt[mm % 2]
                    kb.op("pe", lambda e, mm=mm, p_=p_, m_=m_: e.transpose(p_[:, 0:2], m_[0:2, mm*128:(mm+1)*128], ident2[0:2, 0:2]), reads=[m_, ident2], writes=[p_])
                    kb.op("dve", lambda e, l=l, m=m, j=j, kto=kto, p_=p_: e.tensor_scalar(mods[l][:, kto, j:12:6], p_[:, 0:2], bs[:, l, m:m+1], None, ALU.add), reads=[p_, bs], writes=[mods[l]])

def emit_pre(kb, xT, xbuf, w, NOUT, g1col, g1buf, mod, U_loc, ubuf, NTOK, NLAT, ones, stages, after_tile=None):
    MT = (NOUT + 127) // 128
    with kb.scope():
        stages = [kb.sb([128, 2048], F32) for _ in range(2)]
        wbf = kb.sb([128, 8, NOUT], BF16)
        A = kb.sb([128, 8, 2], F32); Bc = kb.sb([128, 8, 2], F32)
        for s in range(2):
            kb.op("dve", lambda e, s=s: e.scalar_tensor_tensor(A[:, :, s:s+1], mod[:, :, s*6+1:s*6+2], 1.0, g1col, ALU.add, ALU.mult), reads=[mod, g1buf], writes=[A])
            kb.op("dve", lambda e, s=s: e.tensor_copy(Bc[:, :, s:s+1], mod[:, :, s*6:s*6+1]), reads=[mod], writes=[Bc])
        load_cast_weight(kb, w, 1024, NOUT, wbf, stages)
        xs = [kb.sb([128, 8, 512], F32) for _ in range(2)]
        sq = [kb.sb([128, 8, 512], BF16) for _ in range(2)]
        hall = kb.sb([128, 8, NTOK], BF16)
        tmp = [kb.sb([128, 512], F32) for _ in range(2)]
        rstd = [kb.sb([128, 512], F32) for _ in range(2)]
        ss = kb.ps([128, 512], F32)
        pso = [kb.ps([128, 512], F32) for _ in range(4)]
        ob = [kb.sb([128, 512], F32) for _ in range(4)]
        xv = xT.rearrange("(kt p) n -> p kt n", p=128)
        tiles = [(i * 512, 512, 0) for i in range(NLAT // 512)] + ([(NLAT, NTOK - NLAT, 1)] if NTOK > NLAT else [])
        hv = [kb.view(hall.t, "hall%d" % ti) for ti in range(len(tiles))]
        for ti, (t0, n, s) in enumerate(tiles):
            x_ = xs[ti % 2]; sq_ = sq[ti % 2]; rs_ = rstd[ti % 2]; h_ = hv[ti]
            kb.dma("sp", x_[:, :, 0:n], xv[:, :, t0:t0 + n], reads=[xbuf], writes=[x_])
            kb.op("act", lambda e, x_=x_, sq_=sq_, n=n: e.activation(sq_[:, :, 0:n], x_[:, :, 0:n], AF.Square), reads=[x_], writes=[sq_])
            for kt in range(8):
                kb.op("pe", lambda e, kt=kt, sq_=sq_, n=n: e.matmul(ss[:, 0:n], ones[:], sq_[:, kt, 0:n], start=(kt == 0), stop=(kt == 7)), reads=[ones, sq_], writes=[ss])
            kb.op("act", lambda e, rs_=rs_, n=n: e.activation(rs_[:, 0:n], ss[:, 0:n], AF.Sqrt, scale=1.0 / 1024, bias=1e-6), reads=[ss], writes=[rs_])
            kb.op("dve", lambda e, rs_=rs_, n=n: e.reciprocal(rs_[:, 0:n], rs_[:, 0:n]), reads=[rs_], writes=[rs_])
            for kt in range(8):
                t_ = tmp[kt % 2]
                kb.op("dve", lambda e, kt=kt, t_=t_, x_=x_, rs_=rs_, n=n, s=s: e.scalar_tensor_tensor(t_[:, 0:n], x_[:, kt, 0:n], A[:, kt, s:s+1], rs_[:, 0:n], ALU.mult, ALU.mult), reads=[x_, A, rs_], writes=[t_])
                kb.op("pool", lambda e, kt=kt, t_=t_, n=n, s=s, t0=t0: e.tensor_scalar(hall[:, kt, t0:t0+n], t_[:, 0:n], Bc[:, kt, s:s+1], 1.0, ALU.add, ALU.mult), reads=[t_, Bc], writes=[h_])
        oi = 0
        for m in range(MT):
            mw = min(128, NOUT - m * 128)
            for ti, (t0, n, s) in enumerate(tiles):
                p_ = pso[oi % 4]; o_ = ob[oi % 4]; h_ = hv[ti]
                for kt in range(8):
                    kb.op("pe", lambda e, kt=kt, m=m, mw=mw, p_=p_, n=n, t0=t0: e.matmul(p_[0:mw, 0:n], wbf[:, kt, m*128:m*128+mw], hall[:, kt, t0:t0+n], start=(kt == 0), stop=(kt == 7)), reads=[wbf, h_], writes=[p_])
                if oi % 2 == 0:
                    kb.op("act", lambda e, p_=p_, o_=o_, mw=mw, n=n: e.activation(o_[0:mw, 0:n], p_[0:mw, 0:n], AF.Copy), reads=[p_], writes=[o_])
                else:
                    kb.op("dve", lambda e, p_=p_, o_=o_, mw=mw, n=n: e.tensor_copy(o_[0:mw, 0:n], p_[0:mw, 0:n]), reads=[p_], writes=[o_])
                kb.dma("sp", U_loc[m*128:m*128+mw, t0:t0+n], o_[0:mw, 0:n], reads=[o_], writes=[ubuf[m]])
                oi += 1
            if after_tile is not None:
                after_tile(m)

def emit_post(kb, xT, xbuf, mk_ldmix, w_out, g2col, g2buf, mod, wr, w1, w3, w2, xoT, xo_buf, NTOK, NLAT, ones, ident, stages, NEXP=16):
    tiles = [(i * 512, 512, 0) for i in range(NLAT // 512)] + ([(NLAT, NTOK - NLAT, 1)] if NTOK > NLAT else [])
    xv = xT.rearrange("(kt p) n -> p kt n", p=128)
    ov = xoT.rearrange("(kt p) n -> p kt n", p=128)
    with kb.scope():
        stages = [kb.sb([128, 2048], F32) for _ in range(2)]
        A = kb.sb([128, 8, 2], F32); Bc = kb.sb([128, 8, 2], F32)
        G1 = kb.sb([128, 8, 2], F32); G5 = kb.sb([128, 8, 2], F32)
        for s in range(2):
            kb.op("dve", lambda e, s=s: e.scalar_tensor_tensor(A[:, :, s:s+1], mod[:, :, s*6+4:s*6+5], 1.0, g2col, ALU.add, ALU.mult), reads=[mod, g2buf], writes=[A])
            kb.op("dve", lambda e, s=s: e.tensor_copy(Bc[:, :, s:s+1], mod[:, :, s*6+3:s*6+4]), reads=[mod], writes=[Bc])
            kb.op("dve", lambda e, s=s: e.tensor_copy(G1[:, :, s:s+1], mod[:, :, s*6+2:s*6+3]), reads=[mod], writes=[G1])
            kb.op("dve", lambda e, s=s: e.tensor_copy(G5[:, :, s:s+1], mod[:, :, s*6+5:s*6+6]), reads=[mod], writes=[G5])
        h2all = kb.sb([128, 8, NTOK], BF16)
        gateT = kb.sb([16, NTOK], F32)
        wrs = kb.sb([128, 8, 20], F32)
        kb.dma("sp", wrs[:], wr.rearrange("(kt p) m -> p kt m", p=128), writes=[wrs])
        with kb.scope():
            wob = kb.sb([128, 8, 1024], BF16)
            load_cast_weight(kb, w_out, 1024, 1024, wob, stages)
            xs = [kb.sb([128, 8, 512], F32) for _ in range(2)]
            ms = [kb.sb([128, 8, 512], F32) for _ in range(2)]
            mb = kb.sb([128, 8, 512], BF16)
            sq = kb.sb([128, 8, 512], BF16)
            h2f = kb.sb([128, 8, 512], F32)
            tmp = [kb.sb([128, 512], F32) for _ in range(2)]
            rstd = kb.sb([128, 512], F32)
            ss = kb.ps([128, 512], F32)
            psy = [kb.ps([128, 512], F32) for _ in range(2)]
            psl = kb.ps([128, 512], F32)
            pst = kb.ps([128, 512], F32)
            L = kb.sb([128, 4, 20], F32)
            sm = kb.sb([128, 4, 16], F32)
            oh = kb.sb([128, 4, 4], F32); pen = kb.sb([128, 4, 4], F32)
            em = kb.sb([128, 4, 16], F32); em2 = kb.sb([128, 4, 16], F32)
            eq1 = kb.sb([128, 4, 16], F32); eq2 = kb.sb([128, 4, 16], F32); gate = kb.sb([128, 4, 16], F32)
            junk = kb.sb([128, 4, 4], F32)
            def route(t0, n):
                ns = n // 128
                for c in range(ns):
                    c0 = c * 128
                    for kt in range(8):
                        kb.op("pe", lambda e, kt=kt, c0=c0, c=c: e.matmul(psl[:, c*20:(c+1)*20], h2f[:, kt, c0:c0+128], wrs[:, kt, :], start=(kt == 0), stop=(kt == 7)), reads=[h2f, wrs], writes=[psl])
                kb.op("act", lambda e, ns=ns: e.activation(L[:, 0:ns, :], psl[:, 0:ns*20].rearrange("p (s c) -> p s c", c=20), AF.Copy), reads=[psl], writes=[L])
                D = lambda fn, r, w: kb.op("dve", fn, reads=r, writes=w)
                S_ = slice(0, ns)
                Lg = L[:, S_, 0:4]; Le = L[:, S_, 4:20]
                bc = lambda col, w: sm[:, S_, col:col+1].broadcast_to([128, ns, w])
                D(lambda e: e.tensor_reduce(sm[:, S_, 10:11], Lg, AX.X, ALU.max), [L], [sm])
                D(lambda e: e.tensor_tensor(oh[:, S_, :], Lg, bc(10, 4), ALU.is_equal), [L, sm], [oh])
                D(lambda e: e.tensor_tensor(junk[:, S_, :], Lg, bc(10, 4), ALU.subtract), [L, sm], [junk])
                kb.op("act", lambda e: e.activation(junk[:, S_, :], junk[:, S_, :], AF.Exp), reads=[junk], writes=[junk])
                D(lambda e: e.tensor_reduce(sm[:, S_, 1:2], junk[:, S_, :], AX.X, ALU.add), [junk], [sm])
                D(lambda e: e.reciprocal(sm[:, S_, 2:3], sm[:, S_, 1:2]), [sm], [sm])
                D(lambda e: e.tensor_scalar(pen[:, S_, :], oh[:, S_, :], 1e30, -1e30, ALU.mult, ALU.add), [oh], [pen])
                D(lambda e: e.tensor_tensor(em[:, S_, :].rearrange("p s (g e) -> p s g e", e=4), Le.rearrange("p s (g e) -> p s g e", e=4),
                                            pen[:, S_, :].rearrange("p s (g o) -> p s g o", o=1).broadcast_to([128, ns, 4, 4]), ALU.add), [L, pen], [em])
                D(lambda e: e.tensor_reduce(sm[:, S_, 3:4], em[:, S_, :], AX.X, ALU.max), [em], [sm])
                D(lambda e: e.tensor_tensor(eq1[:, S_, :], em[:, S_, :], bc(3, 16), ALU.is_equal), [em, sm], [eq1])
                D(lambda e: e.scalar_tensor_tensor(em2[:, S_, :], eq1[:, S_, :], -1e30, em[:, S_, :], ALU.mult, ALU.add), [eq1, em], [em2])
                D(lambda e: e.tensor_reduce(sm[:, S_, 4:5], em2[:, S_, :], AX.X, ALU.max), [em2], [sm])
                D(lambda e: e.tensor_tensor(eq2[:, S_, :], em2[:, S_, :], bc(4, 16), ALU.is_equal), [em2, sm], [eq2])
                D(lambda e: e.tensor_tensor(sm[:, S_, 5:6], sm[:, S_, 3:4], sm[:, S_, 4:5], ALU.subtract), [sm], [sm])
                kb.op("act", lambda e: e.activation(sm[:, S_, 6:7], sm[:, S_, 5:6], AF.Sigmoid), reads=[sm], writes=[sm])
                D(lambda e: e.tensor_scalar(sm[:, S_, 7:8], sm[:, S_, 6:7], -1.0, 1.0, ALU.mult, ALU.add), [sm], [sm])
                D(lambda e: e.tensor_tensor(sm[:, S_, 8:10], sm[:, S_, 6:8], bc(2, 2), ALU.mult), [sm], [sm])
                D(lambda e: e.tensor_tensor(gate[:, S_, :], eq1[:, S_, :], bc(8, 16), ALU.mult), [eq1, sm], [gate])
                D(lambda e: e.tensor_tensor(eq2[:, S_, :], eq2[:, S_, :], bc(9, 16), ALU.mult), [eq2, sm], [eq2])
                D(lambda e: e.tensor_tensor(gate[:, S_, :], gate[:, S_, :], eq2[:, S_, :], ALU.add), [gate, eq2], [gate])
                for c in range(ns):
                    kb.op("pe", lambda e, c=c: e.transpose(pst[0:16, c*128:(c+1)*128], gate[:, c, :], ident[:]), reads=[gate, ident], writes=[pst])
                kb.op("act", lambda e, t0=t0, n=n: e.activation(gateT[:, t0:t0+n], pst[0:16, 0:n], AF.Copy), reads=[pst], writes=[gateT])

            ldmix = mk_ldmix()
            def load(ti):
                t0, n, s = tiles[ti]
                kb.dma("sp", xs[ti % 2][:, :, 0:n], xv[:, :, t0:t0+n], reads=[xbuf], writes=[xs[ti % 2]])
                ldmix(ms[ti % 2], t0, n)
            mbs = [mb, kb.sb([128, 8, 512], BF16)]
            def partA(ti):
                t0, n, s = tiles[ti]
                x_ = xs[ti % 2]; m_ = ms[ti % 2]; mb_ = mbs[ti % 2]
                kb.op("pool", lambda e, m_=m_, n=n, mb_=mb_: e.tensor_copy(mb_[:, :, 0:n], m_[:, :, 0:n]), reads=[m_], writes=[mb_])
                for m in range(8):
                    p_ = psy[m % 2]
                    for kt in range(8):
                        kb.op("pe", lambda e, kt=kt, m=m, p_=p_, n=n, mb_=mb_: e.matmul(p_[:, 0:n], wob[:, kt, m*128:(m+1)*128], mb_[:, kt, 0:n], start=(kt == 0), stop=(kt == 7)), reads=[wob, mb_], writes=[p_])
                    kb.op("dve", lambda e, m=m, p_=p_, x_=x_, n=n, s=s: e.scalar_tensor_tensor(x_[:, m, 0:n], p_[:, 0:n], G1[:, m, s:s+1], x_[:, m, 0:n], ALU.mult, ALU.add), reads=[p_, G1, x_], writes=[x_])
                kb.dma("sp", ov[:, :, t0:t0+n], x_[:, :, 0:n], reads=[x_], writes=[xo_buf])
            def partB(ti):
                t0, n, s = tiles[ti]
                x_ = xs[ti % 2]
                kb.op("act", lambda e, x_=x_, n=n: e.activation(sq[:, :, 0:n], x_[:, :, 0:n], AF.Square), reads=[x_], writes=[sq])
                for kt in range(8):
                    kb.op("pe", lambda e, kt=kt, n=n: e.matmul(ss[:, 0:n], ones[:], sq[:, kt, 0:n], start=(kt == 0), stop=(kt == 7)), reads=[ones, sq], writes=[ss])
                kb.op("act", lambda e, n=n: e.activation(rstd[:, 0:n], ss[:, 0:n], AF.Sqrt, scale=1.0 / 1024, bias=1e-6), reads=[ss], writes=[rstd])
                kb.op("dve", lambda e, n=n: e.reciprocal(rstd[:, 0:n], rstd[:, 0:n]), reads=[rstd], writes=[rstd])
                for kt in range(8):
                    t_ = tmp[kt % 2]
                    kb.op("dve", lambda e, kt=kt, t_=t_, x_=x_, n=n, s=s: e.scalar_tensor_tensor(t_[:, 0:n], x_[:, kt, 0:n], A[:, kt, s:s+1], rstd[:, 0:n], ALU.mult, ALU.mult), reads=[x_, A, rstd], writes=[t_])
                    kb.op("pool", lambda e, kt=kt, t_=t_, n=n, s=s: e.tensor_scalar(h2f[:, kt, 0:n], t_[:, 0:n], Bc[:, kt, s:s+1], 1.0, ALU.add, ALU.mult), reads=[t_, Bc], writes=[h2f])
                kb.op("act", lambda e, n=n, t0=t0: e.activation(h2all[:, :, t0:t0+n], h2f[:, :, 0:n], AF.Copy), reads=[h2f], writes=[h2all])
                route(t0, n)
            load(0)
            if len(tiles) > 1:
                load(1)
            partA(0)
            for ti in range(len(tiles)):
                if ti + 1 < len(tiles):
                    partA(ti + 1)
                partB(ti)
                if ti + 2 < len(tiles):
                    load(ti + 2)
        with kb.scope():
            yacc = kb.sb([128, 8, NTOK], F32)
            es2 = kb.scope(); es2.__enter__()
            sel = kb.sb([16, NEXP, 128], F32)
            kb.op("pool", lambda e: e.memset(sel[:], 1.0), writes=[sel])
            kb.op("pool", lambda e: e.affine_select(sel[:], sel[:], [[-1, NEXP], [0, 128]], ALU.is_equal, 0.0, base=0, channel_multiplier=1), reads=[sel], writes=[sel])
            wb = [dict(w1=kb.sb([128, 8, 512], BF16), w3=kb.sb([128, 8, 512], BF16), w2=kb.sb([128, 4, 1024], BF16)) for _ in range(2)]
            gb_ps = kb.ps([128, 512], F32)
            gb = kb.sb([128, 512], F32)
            pa = [kb.ps([128, 512], F32) for _ in range(2)]
            pb = [kb.ps([128, 512], F32) for _ in range(2)]
            po = [kb.ps([128, 512], F32) for _ in range(2)]
            sa = [kb.sb([128, 512], F32) for _ in range(2)]
            tt = [kb.sb([128, 512], F32) for _ in range(2)]
            hid = [kb.sb([128, 4, 512], BF16) for _ in range(2)]
            st4 = [kb.view(stages[i // 2].t[:, (i % 2) * 1024:(i % 2 + 1) * 1024], "st4_%d" % i) for i in range(4)]
            def chunks(e_):
                d_ = wb[e_ % 2]
                out = []
                for nm, src, K, M in (("w1", w1[e_], 1024, 512), ("w3", w3[e_], 1024, 512), ("w2", w2[e_], 512, 1024)):
                    wv = src.rearrange("(kt p) m -> p kt m", p=128)
                    for kt in range(K // 128):
                        out.append((wv[:, kt, :], d_[nm], kt, M))
                return out
            allch = [c for e_ in range(NEXP) for c in chunks(e_)]
            NCHK = 20
            ptr_ = {"dma": 0, "cast": 0}
            def emit_dma():
                i = ptr_["dma"]
                if i >= len(allch): return
                src, dbuf, kt, M = allch[i]
                st = st4[i % 4]
                kb.dma("sp", st[:, 0:M], src, writes=[st])
                ptr_["dma"] += 1
            def emit_cast():
                i = ptr_["cast"]
                if i >= ptr_["dma"]: return
                src, dbuf, kt, M = allch[i]
                st = st4[i % 4]
                kb.op("pool", lambda e, st=st, dbuf=dbuf, kt=kt, M=M: e.tensor_copy(dbuf[:, kt, 0:M], st[:, 0:M]), reads=[st], writes=[dbuf])
                ptr_["cast"] += 1
            def advance(upto):
                while ptr_["dma"] < min(upto, len(allch)):
                    emit_dma()
                    if ptr_["dma"] - ptr_["cast"] > 2:
                        emit_cast()
            def flush(upto):
                while ptr_["cast"] < min(upto, ptr_["dma"]):
                    emit_cast()
            gbs = [gb, kb.sb([128, 512], F32)]
            items = [(e_, ti) for e_ in range(NEXP) for ti in range(len(tiles))]
            def stageA(k):
                e_, ti = items[k]
                t0, n, s = tiles[ti]
                d_ = wb[e_ % 2]; h_ = hid[k % 2]; g_ = gbs[k % 2]
                kb.op("pe", lambda e, e_=e_, t0=t0, n=n: e.matmul(gb_ps[:, 0:n], sel[:, e_, :], gateT[:, t0:t0+n], start=True, stop=True), reads=[sel, gateT], writes=[gb_ps])
                kb.op("act", lambda e, n=n, g_=g_: e.activation(g_[:, 0:n], gb_ps[:, 0:n], AF.Copy), reads=[gb_ps], writes=[g_])
                for f in range(4):
                    a_ = pa[f % 2]; b_ = pb[f % 2]; sa_ = sa[f % 2]; t_ = tt[f % 2]
                    for kt in range(8):
                        kb.op("pe", lambda e, kt=kt, f=f, a_=a_, d_=d_, t0=t0, n=n: e.matmul(a_[:, 0:n], d_["w1"][:, kt, f*128:(f+1)*128], h2all[:, kt, t0:t0+n], start=(kt == 0), stop=(kt == 7)), reads=[d_["w1"], h2all], writes=[a_])
                    for kt in range(8):
                        kb.op("pe", lambda e, kt=kt, f=f, b_=b_, d_=d_, t0=t0, n=n: e.matmul(b_[:, 0:n], d_["w3"][:, kt, f*128:(f+1)*128], h2all[:, kt, t0:t0+n], start=(kt == 0), stop=(kt == 7)), reads=[d_["w3"], h2all], writes=[b_])
                    kb.op("act", lambda e, a_=a_, sa_=sa_, n=n: e.activation(sa_[:, 0:n], a_[:, 0:n], AF.Silu), reads=[a_], writes=[sa_])
                    kb.op("dve", lambda e, b_=b_, sa_=sa_, t_=t_, n=n: e.tensor_tensor(t_[:, 0:n], b_[:, 0:n], sa_[:, 0:n], ALU.mult), reads=[b_, sa_], writes=[t_])
                    kb.op("pool", lambda e, t_=t_, h_=h_, f=f, n=n, g_=g_: e.tensor_tensor(h_[:, f, 0:n], t_[:, 0:n], g_[:, 0:n], ALU.mult), reads=[t_, g_], writes=[h_])
            def stageB(k):
                e_, ti = items[k]
                t0, n, s = tiles[ti]
                d_ = wb[e_ % 2]; h_ = hid[k % 2]
                for m in range(8):
                    o_ = po[m % 2]
                    for f in range(4):
                        kb.op("pe", lambda e, f=f, m=m, o_=o_, h_=h_, d_=d_, n=n: e.matmul(o_[:, 0:n], d_["w2"][:, f, m*128:(m+1)*128], h_[:, f, 0:n], start=(f == 0), stop=(f == 3)), reads=[d_["w2"], h_], writes=[o_])
                    if e_ == 0:
                        kb.op("dve", lambda e, m=m, o_=o_, t0=t0, n=n: e.tensor_copy(yacc[:, m, t0:t0+n], o_[:, 0:n]), reads=[o_], writes=[yacc])
                    else:
                        kb.op("dve", lambda e, m=m, o_=o_, t0=t0, n=n: e.tensor_tensor(yacc[:, m, t0:t0+n], o_[:, 0:n], yacc[:, m, t0:t0+n], ALU.add), reads=[o_, yacc], writes=[yacc])
            advance(2 * NCHK); flush(2 * NCHK)
            nt_ = len(tiles)
            per_it = -(-NCHK // max(1, nt_ - 1))
            stageA(0)
            for k in range(len(items)):
                if k + 1 < len(items):
                    e_n, ti_n = items[k + 1]
                    if ti_n == 0:
                        flush((e_n + 1) * NCHK)
                    stageA(k + 1)
                stageB(k)
                e_k, ti_k = items[k]
                if e_k >= 1 and e_k + 1 < NEXP and ti_k < nt_ - 1:
                    advance((e_k + 1) * NCHK + min(NCHK, (ti_k + 1) * per_it))
            es2.__exit__(None, None, None)
            xm = [kb.sb([128, 8, 512], F32) for _ in range(2)]
            for ti, (t0, n, s) in enumerate(tiles):
                x_ = xm[ti % 2]
                kb.dma("sp", x_[:, :, 0:n], ov[:, :, t0:t0+n], reads=[xo_buf], writes=[x_])
                for m in range(8):
                    kb.op("dve", lambda e, m=m, x_=x_, t0=t0, n=n, s=s: e.scalar_tensor_tensor(x_[:, m, 0:n], yacc[:, m, t0:t0+n], G5[:, m, s:s+1], x_[:, m, 0:n], ALU.mult, ALU.add), reads=[yacc, G5, x_], writes=[x_])
                kb.dma("sp", ov[:, :, t0:t0+n], x_[:, :, 0:n], reads=[x_], writes=[xo_buf])

TOT = 4352
def build_fused():
    nc = bass.Bass("TRN2", target_bir_lowering=False)
    I = lambda n, s: nc.dram_tensor(n, s, F32, kind="ExternalInput").ap()
    N = lambda n, s: nc.dram_tensor(n, s, F32, kind="Internal").ap()
    xT0 = I("xT0", [1024, 2176]); cT = I("cT", [128, 8, 2]); mod_w = I("mod_w", [2, 1024, 6144]); mod_bl = I("mod_bl", [128, 2, 48])
    g12 = I("g12", [128, 4, 8])
    rsel_d = I("rsel", [128, 2])
    w_in = [I("w_in0", [1024, 2560]), I("w_in1", [1024, 3616])]
    w_out = [I("w_out0", [1024, 1024]), I("w_out1", [1024, 1024])]
    cw = I("cw", [128, 2, 5]); wbd = I("wbd", [2, 2, 2, 128, 128]); lp = I("lp", [128, 2, 2, 3]); qkg = I("qkg", [128, 2]); bias = I("bias", [2, 128, 8, 512])
    rowp = I("rowp", [128, 2]); convp = I("convp", [128, 8, 5]); chp = I("chp", [128, 2, 3])
    wr = I("wr", [2, 1024, 20]); w1 = I("w1", [2, 16, 1024, 512]); w3 = I("w3", [2, 16, 1024, 512]); w2 = I("w2", [2, 16, 512, 1024])
    xoT = nc.dram_tensor("xoT", [1024, 2048], F32, kind="ExternalOutput").ap()
    NOUT = [2560, 3616]; T = [10, 14]; NT = [2176, 2048]
    U_loc = [N("U_loc%d" % l, [((NOUT[l] + 127) // 128) * 128, 2176]) for l in range(2)]
    U_g = [[N("U_g%d_%d" % (l, i), [256, 2176]) for i in range(T[l])] for l in range(2)]
    G_g = N("G_g", [64, 2176])
    M_send = [[N("M_send%d_%d" % (l, t), [128, NT[l]]) for t in range(4)] for l in range(2)]
    M_loc = [[N("M_loc%d_%d" % (l, t), [128, NT[l]]) for t in range(4)] for l in range(2)]
    M_g = [[N("M_g%d_%d" % (l, t), [256, NT[l]]) for t in range(4)] for l in range(2)]
    X1 = N("X1", [1024, 2176])
    with ExitStack() as es:
        kb = KB(nc, es)
        kb.ccsem = es.enter_context(nc.semaphore("ccsem")); kb.ccn = 0
        V = kb.view
        b_x0 = V(xT0); b_X1 = V(X1); b_xo = V(xoT)
        b_Ul = [V(U_loc[l]) for l in range(2)]; b_Ug = [[V(a) for a in U_g[l]] for l in range(2)]; b_Gg = V(G_g)
        b_Ms = [[V(a) for a in M_send[l]] for l in range(2)]; b_Ml = [[V(a) for a in M_loc[l]] for l in range(2)]; b_Mg = [[V(a) for a in M_g[l]] for l in range(2)]
        ones = kb.sb([128, 128], BF16)
        kb.op("pool", lambda e: e.memset(ones[:], 1.0), writes=[ones])
        ident = kb.ident(F32)
        gs = kb.sb([128, 4, 8]); rsel = kb.sb([128, 2])
        kb.dma("sp", gs[:], g12, writes=[gs]); kb.dma("sp", rsel[:], rsel_d, writes=[rsel])
        R0 = rsel[:, 0:1]; R1 = rsel[:, 1:2]
        mods = [kb.sb([128, 8, 12]) for _ in range(2)]
        stages = None
        emit_mod(kb, cT, mod_w, mod_bl, mods)

        def blend(dst_ap, a_ap, b_ap, ca, cb, t_ap, rd, wr_):
            kb.op("pool", lambda e: e.tensor_scalar(t_ap, a_ap, ca, 1.0, ALU.mult, ALU.mult), reads=rd + [rsel], writes=[wr_[1]])
            kb.op("dve", lambda e: e.scalar_tensor_tensor(dst_ap, b_ap, cb, t_ap, ALU.mult, ALU.add), reads=rd + [rsel, wr_[1]], writes=[wr_[0]])

        for l in range(2):
            xin, xbuf = (xT0, b_x0) if l == 0 else (X1, b_X1)
            xout, xobuf = (X1, b_X1) if l == 0 else (xoT, b_xo)
            NTOKp = 2176
            MTl = (NOUT[l] + 127) // 128
            ub_t = [V(U_loc[l], "ul%d_%d" % (l, m)) for m in range(MTl)]
            def after_tile(m, l=l, ub_t=ub_t):
                if m < T[l]:
                    emit_cc(kb, U_loc[l][m*128:(m+1)*128, :], U_g[l][m], [ub_t[m]], [b_Ug[l][m]])
                elif l == 1 and m == 28:
                    emit_cc(kb, U_loc[1][28*128:28*128+32, :], G_g, [ub_t[m]], [b_Gg])
            emit_pre(kb, xin, xbuf, w_in[l], NOUT[l], gs[:, l, :].rearrange("p (k o) -> p k o", o=1), gs, mods[l], U_loc[l], ub_t, NTOKp, 2048, ones, stages, after_tile)
            if l == 1:
                b_Gg.last_w = (kb.ccsem, kb.ccn, "cc")
            for i in range(T[l]):
                b_Ug[l][i].last_w = (kb.ccsem, kb.ccn, "cc")
            with kb.scope():
                CHK = [(0, 512), (512, 512), (1024, 512), (1536, 512), (2048, 128)]
                sets = [tuple(kb.sb([128, 512]) for _ in range(5)) for _ in range(2)]
                cnt = {"i": 0}
                def nxt():
                    cnt["i"] += 1
                    return sets[cnt["i"] % 2]
                def dcols(h, c0, cn):
                    return (h * 2048 + c0, cn) if c0 < 2048 else (4096 + h * 128, cn)
                def ld(dst, i, l=l):
                    for (c0, cn) in CHK:
                        sl, s0, s1, ta, tb = nxt()
                        kb.dma("sp", sl[:, 0:cn], U_loc[l][(T[l]+i)*128:(T[l]+i+1)*128, c0:c0+cn], reads=[b_Ul[l]], writes=[sl])
                        kb.dma("sp", s0[:, 0:cn], U_g[l][i][0:128, c0:c0+cn], reads=[b_Ug[l][i]], writes=[s0])
                        kb.dma("sp", s1[:, 0:cn], U_g[l][i][128:256, c0:c0+cn], reads=[b_Ug[l][i]], writes=[s1])
                        d0, _ = dcols(0, c0, cn); d1, _ = dcols(1, c0, cn)
                        blend(dst[:, d0:d0+cn], sl[:, 0:cn], s0[:, 0:cn], R0, R1, ta[:, 0:cn], [sl, s0], (dst, ta))
                        blend(dst[:, d1:d1+cn], sl[:, 0:cn], s1[:, 0:cn], R1, R0, tb[:, 0:cn], [sl, s1], (dst, tb))
                def st(t, src, l=l):
                    pieces = [(0, 512), (512, 512), (1024, 512), (1536, 512)] + ([(2048, 128)] if l == 0 else [])
                    for (c0, cn) in pieces:
                        sl, s0, s1, ta, tb = nxt()
                        if c0 < 2048:
                            a0 = src[:, c0:c0+cn]; a1 = src[:, 2048+c0:2048+c0+cn]
                        else:
                            a0 = src[:, 4096:4224]; a1 = src[:, 4224:4352]
                        blend(s0[:, 0:cn], a0, a1, R0, R1, ta[:, 0:cn], [src], (s0, ta))
                        kb.dma("sp", M_loc[l][t][:, c0:c0+cn], s0[:, 0:cn], reads=[s0], writes=[b_Ml[l][t]])
                        blend(s1[:, 0:cn], a0, a1, R1, R0, tb[:, 0:cn], [src], (s1, tb))
                        kb.dma("sp", M_send[l][t][:, c0:c0+cn], s1[:, 0:cn], reads=[s1], writes=[b_Ms[l][t]])
                    emit_cc(kb, M_send[l][t], M_g[l][t], [b_Ms[l][t]], [b_Mg[l][t]])
                if l == 0:
                    emit_mixe(kb, ld, cw, wbd, lp, qkg, bias, st)
                else:
                    def ld_rows(dst):
                        kb.op("pool", lambda e: e.memset(dst[:], 0.0), writes=[dst])
                        for (c0, cn) in CHK:
                            for h in range(2):
                                sA, sB, _s1, ta, _tb = nxt()
                                kb.op("pool", lambda e, sA=sA: e.memset(sA[:], 0.0), writes=[sA])
                                kb.op("pool", lambda e, sB=sB: e.memset(sB[:], 0.0), writes=[sB])
                                a_r0 = 0 if h == 0 else 16
                                b_r0 = 16 if h == 0 else 0
                                for (pb, ro, nr) in ((0, 0, 8), (32, 8, 4), (96, 12, 4)):
                                    kb.dma("sp", sA[pb:pb+nr, 0:cn], G_g[h*32+a_r0+ro:h*32+a_r0+ro+nr, c0:c0+cn], reads=[b_Gg], writes=[sA])
                                    kb.dma("sp", sB[pb:pb+nr, 0:cn], G_g[h*32+b_r0+ro:h*32+b_r0+ro+nr, c0:c0+cn], reads=[b_Gg], writes=[sB])
                                d0, _ = dcols(h, c0, cn)
                                blend(dst[:, d0:d0+cn], sA[:, 0:cn], sB[:, 0:cn], R0, R1, ta[:, 0:cn], [sA, sB], (dst, ta))
                    emit_mixo(kb, ld, ld_rows, rowp, convp, chp, st)
            NTOK = NT[l]
            if True:
              def mk_ldmix(l=l):
                sb0 = kb.sb([128, 4, 512]); sb1 = kb.sb([128, 4, 512])
                def ldmix(dst, t0, n, l=l):
                    for kt in range(4):
                        kb.dma("sp", dst[:, kt, 0:n], M_loc[l][kt][:, t0:t0+n], reads=[b_Ml[l][kt]], writes=[dst])
                        kb.dma("sp", sb0[:, kt, 0:n], M_g[l][kt][0:128, t0:t0+n], reads=[b_Mg[l][kt]], writes=[sb0])
                        kb.dma("sp", sb1[:, kt, 0:n], M_g[l][kt][128:256, t0:t0+n], reads=[b_Mg[l][kt]], writes=[sb1])
                    blend(dst[:, 4:8, 0:n], sb0[:, :, 0:n], sb1[:, :, 0:n], R1, R0, sb0[:, :, 0:n], [sb0, sb1], (dst, sb0))
                return ldmix
              emit_post(kb, xin, xbuf, mk_ldmix, w_out[l], gs[:, 2 + l, :].rearrange("p (k o) -> p k o", o=1), gs, mods[l], wr[l], w1[l], w3[l], w2[l],
                        xout if l == 1 else X1, xobuf, NTOK, 2048, ones, ident, stages)
        kb._wait("sp", (kb.ccsem, kb.ccn, "cc"))
        kb.finish()
    return nc


def needs_even(hf):
    c0 = hf * 256
    return np.concatenate([np.arange(b + c0, b + c0 + 256) for b in (0, 512, 1024, 1536, 2048)])

def needs_odd(hf):
    c0 = hf * 256
    return np.concatenate([np.arange(c0, c0 + 256), np.arange(512 + c0, 512 + c0 + 256), np.arange(1024 + hf*128, 1024 + hf*128 + 128),
                           np.arange(1280 + hf*128, 1280 + hf*128 + 128), np.arange(1552 + c0, 1552 + c0 + 256), np.arange(2064 + c0, 2064 + c0 + 256),
                           np.arange(2576 + c0, 2576 + c0 + 256), np.arange(3088 + c0, 3088 + c0 + 256)])

def gates_odd(hf):
    dt = [1536 + d*8 + hf*4 + hl for d in range(2) for hl in range(4)]
    fg = [3600 + d*8 + 4 + hf*2 + hl for d in range(2) for hl in range(2)]
    ig = [3600 + d*8 + hf*2 + hl for d in range(2) for hl in range(2)]
    return np.array(dt + fg + ig)

def mixset(hf):
    return np.concatenate([np.arange(hf*256, hf*256 + 256), np.arange(512 + hf*256, 512 + hf*256 + 256)])

def fused_inputs(b, hf, a):
    C = np.ascontiguousarray
    f = lambda k: np.asarray(a[k], dtype=np.float32)
    x = f('x'); ctx = f('ctx')
    d = {}
    d["xT0"] = C(np.concatenate([x[b, hf*2048:(hf+1)*2048], ctx[b, hf*128:(hf+1)*128]], 0).T)
    cc = np.stack([f('c')[b], f('c_ctx')], 0)
    d["cT"] = C(cc.T.reshape(8, 128, 2).transpose(1, 0, 2))
    d["mod_w"] = f('mod_w')
    d["mod_bl"] = C(f('mod_b').reshape(2, 48, 128).transpose(2, 0, 1))
    d["g12"] = C(np.stack([f('norm1_g')[0], f('norm1_g')[1], f('norm2_g')[0], f('norm2_g')[1]], 0).reshape(4, 8, 128).transpose(2, 0, 1))
    d["rsel"] = np.tile(np.array([[1.0 - hf, float(hf)]], np.float32), (128, 1))
    pe = np.concatenate([needs_even(1 - hf), needs_even(hf)])
    po = np.concatenate([needs_odd(1 - hf), needs_odd(hf), gates_odd(hf), gates_odd(1 - hf)])
    d["w_in0"] = C(f('even_w_in')[0][:, pe]); d["w_in1"] = C(f('odd_w_in')[0][:, po])
    pm = np.concatenate([mixset(hf), mixset(1 - hf)])
    d["w_out0"] = C(f('even_w_out')[0][pm]); d["w_out1"] = C(f('odd_w_out')[0][pm])
    Pe = {k: f(k)[0] for k in ['lru_conv_w','lru_conv_b','lru_wa','lru_ba','lru_wx','lru_bx','lru_lam','na_q_g','na_k_g','na_rpb']}
    Po = {k: f(k)[0] for k in ['ssd_conv_w','ssd_conv_b','ssd_dt_bias','ssd_a_log','ssd_d','ssd_norm_g','ml_conv_w','ml_conv_b','ml_gate_b','ml_norm_g']}
    me = mixe_inputs(np.zeros((2560, 1), np.float32), hf, Pe)
    for k in ("cw", "wbd", "lp", "qkg", "bias"):
        d[k] = me[k]
    mo = mixo_inputs(np.zeros((3616, TOT), np.float32), hf, Po)
    for k in ("rowp", "convp", "chp"):
        d[k] = mo[k]
    d["wr"] = C(np.concatenate([f('moe_router_g'), f('moe_router_e')], 2))
    d["w1"] = f('moe_w1'); d["w3"] = f('moe_w3'); d["w2"] = f('moe_w2')
    return d

_NC = {}
def kernel(**a):
    if "nc" not in _NC:
        _NC["nc"] = build_fused()
    cores = [(b, hf) for b in range(4) for hf in range(2)]
    maps = [fused_inputs(b, hf, a) for (b, hf) in cores]
    res = run_bass_kernel_spmd(_NC["nc"], maps, core_ids=list(range(8))).results
    out = np.zeros((4, 4096, 1024), np.float32)
    for i, (b, hf) in enumerate(cores):
        out[b, hf*2048:(hf+1)*2048] = res[i]["xoT"].T
    return out
```
